# Optimizing a Trainium2 kernel written in Bass

```python
import math
import jax
import jax.numpy as jnp
from jax import lax
import numpy as np


D_MODEL = 1024
BATCH = 16
SEQ = 2048
DEPTH = 1

GRID_W = 64
CTX_LEN = 256
DA_HEAD_DIM = 64
DA_V_DIM = 2 * DA_HEAD_DIM
DA_WIDTH = D_MODEL // 2
DA_HEADS = DA_WIDTH // DA_V_DIM
QK_COLS = DA_HEADS * DA_HEAD_DIM
SG_CHUNK = 128
SG_GROUP_DIM = 128
SG_WIDTH = D_MODEL - DA_WIDTH
SG_GROUPS = SG_WIDTH // SG_GROUP_DIM
MIX_WIDTH = DA_WIDTH + SG_WIDTH
KV_LO = 2 * QK_COLS
KV_HI = 4 * QK_COLS + DA_WIDTH
IN_COLS = KV_HI + 2 * SG_WIDTH
Q_BLOCK = 128
ROPE_THETA = 10000.0
MOE_GROUPS = 4
MOE_EXPERTS_PER_GROUP = 8
N_EXPERTS = MOE_GROUPS * MOE_EXPERTS_PER_GROUP
MOE_TOP_K = 2
D_EXPERT = D_MODEL
MOE_BLOCK = 128
EPS = 1e-5
DEEPNORM_ALPHA = (2.0 * DEPTH) ** 0.25
DEEPNORM_BETA = (8.0 * DEPTH) ** -0.25

kernel_name = 'hybrid_diffattn_sgmlp_hmoe_dit_block'


def _layer_norm(x, g, b):
    xf = x.astype(jnp.float32)
    mu = jnp.mean(xf, -1, keepdims=True)
    var = jnp.mean(jnp.square(xf - mu), -1, keepdims=True)
    y = (xf - mu) * lax.rsqrt(var + EPS) * g.astype(jnp.float32) + b.astype(jnp.float32)
    return y.astype(x.dtype)


def _modulate(h, shift, scale):
    return h * (1 + scale[..., None, :]) + shift[..., None, :]


def _heads(t, dh):
    return t.reshape(t.shape[:-1] + (DA_HEADS, dh))


def _rope_1d(x, pos):
    half = x.shape[-1] // 2
    inv = ROPE_THETA ** (-jnp.arange(half, dtype=jnp.float32) / half)
    ang = pos[:, None] * inv[None, :]
    cos = jnp.cos(ang)[None, :, None, :].astype(x.dtype)
    sin = jnp.sin(ang)[None, :, None, :].astype(x.dtype)
    x1, x2 = x[..., :half], x[..., half:]
    return jnp.concatenate([x1 * cos - x2 * sin, x2 * cos + x1 * sin], -1)


def _axial_rope(x, rows, cols):
    a = x.shape[-1] // 2
    return jnp.concatenate([_rope_1d(x[..., :a], rows), _rope_1d(x[..., a:], cols)], -1)


def _diff_attend(q1, q2, k1, k2, v, lam):
    s1 = jnp.einsum('bqhd,bkhd->bhqk', q1, k1).astype(jnp.float32)
    s2 = jnp.einsum('bqhd,bkhd->bhqk', q2, k2).astype(jnp.float32)
    a = jax.nn.softmax(s1, axis=-1) - lam * jax.nn.softmax(s2, axis=-1)
    return jnp.einsum('bhqk,bkhe->bqhe', a.astype(v.dtype), v)


def _diff_attend_blocked(q1, q2, k1, k2, v, lam):
    B, L, H, d = q1.shape
    nb = L // Q_BLOCK

    def to_blocks(q):
        return q.reshape(B, nb, Q_BLOCK, H, d).swapaxes(0, 1)

    o = lax.map(lambda qs: _diff_attend(qs[0], qs[1], k1, k2, v, lam),
                (to_blocks(q1), to_blocks(q2)))
    return o.swapaxes(0, 1).reshape(B, L, H, v.shape[-1])


def _diff_head_out(o, g, lam_init):
    of = o.astype(jnp.float32)
    of = of * lax.rsqrt(jnp.mean(of * of, -1, keepdims=True) + EPS) * g.astype(jnp.float32)
    of = of * (1.0 - lam_init)
    return of.astype(o.dtype).reshape(o.shape[:-2] + (DA_WIDTH,))


def _spatial_gate(z, ln_g, ln_b, w_s, b_s):
    z = jax.nn.gelu(z, approximate=False)
    u, v = z[..., :SG_WIDTH], z[..., SG_WIDTH:]
    v = _layer_norm(v, ln_g, ln_b)
    B, L, _ = v.shape
    v = v.reshape(B, L // SG_CHUNK, SG_CHUNK, SG_GROUPS, SG_GROUP_DIM)
    s = jnp.einsum('gpq,bnqgc->bnpgc', w_s, v) + b_s.T[None, None, :, :, None]
    return u * s.reshape(B, L, SG_WIDTH)


def _expert_dispatch(t, eid, w, w_gate, w_up, w_down):
    N, D = t.shape
    S = N * MOE_TOP_K
    flat_e = eid.reshape(S)
    flat_w = w.reshape(S)
    order = jnp.argsort(flat_e)
    sorted_e = flat_e[order]
    counts = jnp.zeros((N_EXPERTS,), jnp.int32).at[flat_e].add(1)
    padded = (counts + MOE_BLOCK - 1) // MOE_BLOCK * MOE_BLOCK
    start = jnp.cumsum(counts) - counts
    pad_end = jnp.cumsum(padded)
    pad_start = pad_end - padded
    dest = pad_start[sorted_e] + (jnp.arange(S, dtype=jnp.int32) - start[sorted_e])
    n_blocks = -(-S // MOE_BLOCK) + N_EXPERTS
    P = n_blocks * MOE_BLOCK
    buf_tok = jnp.zeros((P,), jnp.int32).at[dest].set((order // MOE_TOP_K).astype(jnp.int32))
    buf_w = jnp.zeros((P,), t.dtype).at[dest].set(flat_w[order])
    blk_start = jnp.arange(n_blocks, dtype=jnp.int32) * MOE_BLOCK
    blk_e = jnp.minimum(jnp.searchsorted(pad_end, blk_start, side='right'), N_EXPERTS - 1)
    xb = t[buf_tok].reshape(n_blocks, MOE_BLOCK, D)

    def run(args):
        xe, e = args
        hid = jax.nn.silu(xe @ w_gate[e]) * (xe @ w_up[e])
        return hid @ w_down[e]

    yb = lax.map(run, (xb, blk_e)).reshape(P, D)
    return jnp.zeros_like(t).at[buf_tok].add(yb * buf_w[:, None])


def _hier_moe(h, wg, bg, we, be, w_gate, w_up, w_down):
    B, L, D = h.shape
    t = h.reshape(B * L, D)
    N = t.shape[0]
    g_logits = (t @ wg).astype(jnp.float32) + bg.astype(jnp.float32)
    g_prob = jax.nn.softmax(g_logits, axis=-1)
    g_sel = jnp.argmax(g_logits, axis=-1).astype(jnp.int32)
    g_w = jnp.take_along_axis(g_prob, g_sel[:, None], axis=-1)
    e_logits = ((t @ we).astype(jnp.float32) + be.astype(jnp.float32)).reshape(
        N, MOE_GROUPS, MOE_EXPERTS_PER_GROUP)
    e_logits = jnp.take_along_axis(e_logits, g_sel[:, None, None], axis=1)[:, 0]
    top_v, top_i = lax.top_k(e_logits, MOE_TOP_K)
    w = jax.nn.softmax(top_v, axis=-1) * g_w
    eid = g_sel[:, None] * MOE_EXPERTS_PER_GROUP + top_i.astype(jnp.int32)
    out = _expert_dispatch(t, eid, w.astype(t.dtype), w_gate, w_up, w_down)
    return out.reshape(B, L, D)


def setup_inputs(seed: int = 0) -> dict:
    key = jax.random.key(seed)
    ks = jax.random.split(key, 28)

    def nrm(k, shape, scale):
        return jax.random.normal(k, shape, jnp.float32) * scale

    return {
        'x': nrm(ks[0], (BATCH, SEQ, D_MODEL), 1.0),
        'c': nrm(ks[1], (BATCH, D_MODEL), 1.0),
        'ctx': nrm(ks[2], (BATCH, CTX_LEN, D_MODEL), 1.0),
        'c_ctx': nrm(ks[3], (D_MODEL,), 1.0),
        'w_mod': nrm(ks[4], (DEPTH, D_MODEL, 6 * D_MODEL), 0.5 * D_MODEL ** -0.5),
        'b_mod': nrm(ks[5], (DEPTH, 6 * D_MODEL), 0.01),
        'w_in': nrm(ks[6], (DEPTH, D_MODEL, IN_COLS), D_MODEL ** -0.5),
        'lam_q1': nrm(ks[7], (DEPTH, DA_HEAD_DIM), 0.1),
        'lam_k1': nrm(ks[8], (DEPTH, DA_HEAD_DIM), 0.1),
        'lam_q2': nrm(ks[9], (DEPTH, DA_HEAD_DIM), 0.1),
        'lam_k2': nrm(ks[10], (DEPTH, DA_HEAD_DIM), 0.1),
        'subln_g': 1.0 + nrm(ks[11], (DEPTH, DA_V_DIM), 0.01),
        'sg_ln_g': 1.0 + nrm(ks[12], (DEPTH, SG_WIDTH), 0.01),
        'sg_ln_b': nrm(ks[13], (DEPTH, SG_WIDTH), 0.01),
        'sg_w': nrm(ks[14], (DEPTH, SG_GROUPS, SG_CHUNK, SG_CHUNK), SG_CHUNK ** -0.5),
        'sg_b': 1.0 + nrm(ks[15], (DEPTH, SG_GROUPS, SG_CHUNK), 0.01),
        'w_out': nrm(ks[16], (DEPTH, MIX_WIDTH, D_MODEL), DEEPNORM_BETA * MIX_WIDTH ** -0.5),
        'ln1_g': 1.0 + nrm(ks[17], (DEPTH, D_MODEL), 0.01),
        'ln1_b': nrm(ks[18], (DEPTH, D_MODEL), 0.01),
        'router_group_w': nrm(ks[19], (DEPTH, D_MODEL, MOE_GROUPS), D_MODEL ** -0.5),
        'router_group_b': nrm(ks[20], (DEPTH, MOE_GROUPS), 0.01),
        'router_expert_w': nrm(ks[21], (DEPTH, D_MODEL, N_EXPERTS), D_MODEL ** -0.5),
        'router_expert_b': nrm(ks[22], (DEPTH, N_EXPERTS), 0.01),
        'exp_w_gate': nrm(ks[23], (DEPTH, N_EXPERTS, D_MODEL, D_EXPERT), D_MODEL ** -0.5),
        'exp_w_up': nrm(ks[24], (DEPTH, N_EXPERTS, D_MODEL, D_EXPERT), D_MODEL ** -0.5),
        'exp_w_down': nrm(ks[25], (DEPTH, N_EXPERTS, D_EXPERT, D_MODEL), DEEPNORM_BETA * D_EXPERT ** -0.5),
        'ln2_g': 1.0 + nrm(ks[26], (DEPTH, D_MODEL), 0.01),
        'ln2_b': nrm(ks[27], (DEPTH, D_MODEL), 0.01),
    }


def reference(x, c, ctx, c_ctx, w_mod, b_mod, w_in, lam_q1, lam_k1, lam_q2, lam_k2,
              subln_g, sg_ln_g, sg_ln_b, sg_w, sg_b, w_out, ln1_g, ln1_b,
              router_group_w, router_group_b, router_expert_w, router_expert_b,
              exp_w_gate, exp_w_up, exp_w_down, ln2_g, ln2_b):
    B, L, D = x.shape
    rows_n = L // GRID_W
    rows = jnp.repeat(jnp.arange(rows_n, dtype=jnp.float32), GRID_W)
    cols = jnp.tile(jnp.arange(GRID_W, dtype=jnp.float32), rows_n)
    q_scale = DA_HEAD_DIM ** -0.5
    for l in range(DEPTH):
        last = l == DEPTH - 1
        wi = w_in[l]
        mod = jnp.einsum('bd,de->be', jax.nn.silu(c), w_mod[l]) + b_mod[l]
        mod_c = jax.nn.silu(c_ctx) @ w_mod[l] + b_mod[l]
        sh1, sc1, g1, sh2, sc2, g2 = jnp.split(mod, 6, axis=-1)
        csh1, csc1, cg1, csh2, csc2, cg2 = jnp.split(mod_c, 6, axis=-1)
        lam_init = 0.8 - 0.6 * math.exp(-0.3 * l)
        lam = (jnp.exp(jnp.sum(lam_q1[l].astype(jnp.float32) * lam_k1[l].astype(jnp.float32)))
               - jnp.exp(jnp.sum(lam_q2[l].astype(jnp.float32) * lam_k2[l].astype(jnp.float32)))
               + lam_init)

        h_c = _modulate(ctx, csh1, csc1)
        kv_c = jnp.einsum('bld,dc->blc', h_c, wi[:, KV_LO:KV_HI])
        k1_c = _heads(kv_c[..., :QK_COLS], DA_HEAD_DIM)
        k2_c = _heads(kv_c[..., QK_COLS:2 * QK_COLS], DA_HEAD_DIM)
        v_c = _heads(kv_c[..., 2 * QK_COLS:], DA_V_DIM)

        h = _modulate(x, sh1, sc1)
        p = jnp.einsum('bld,dc->blc', h, wi)
        q1 = _axial_rope(_heads(p[..., :QK_COLS], DA_HEAD_DIM) * q_scale, rows, cols)
        q2 = _axial_rope(_heads(p[..., QK_COLS:KV_LO], DA_HEAD_DIM) * q_scale, rows, cols)
        k1 = _axial_rope(_heads(p[..., KV_LO:KV_LO + QK_COLS], DA_HEAD_DIM), rows, cols)
        k2 = _axial_rope(_heads(p[..., KV_LO + QK_COLS:2 * KV_LO], DA_HEAD_DIM), rows, cols)
        v = _heads(p[..., 2 * KV_LO:KV_HI], DA_V_DIM)
        k1_all = jnp.concatenate([k1, k1_c], axis=1)
        k2_all = jnp.concatenate([k2, k2_c], axis=1)
        v_all = jnp.concatenate([v, v_c], axis=1)
        da = _diff_head_out(_diff_attend_blocked(q1, q2, k1_all, k2_all, v_all, lam),
                            subln_g[l], lam_init)
        sg = _spatial_gate(p[..., KV_HI:], sg_ln_g[l], sg_ln_b[l], sg_w[l], sg_b[l])
        y = jnp.einsum('blc,cd->bld', jnp.concatenate([da, sg], axis=-1), w_out[l])
        x = _layer_norm(DEEPNORM_ALPHA * x + g1[:, None, :] * y, ln1_g[l], ln1_b[l])

        f = _hier_moe(_modulate(x, sh2, sc2), router_group_w[l], router_group_b[l],
                      router_expert_w[l], router_expert_b[l],
                      exp_w_gate[l], exp_w_up[l], exp_w_down[l])
        x = _layer_norm(DEEPNORM_ALPHA * x + g2[:, None, :] * f, ln2_g[l], ln2_b[l])

        if not last:
            qc = jnp.einsum('bld,dc->blc', h_c, wi[:, :KV_LO])
            q1_c = _heads(qc[..., :QK_COLS], DA_HEAD_DIM) * q_scale
            q2_c = _heads(qc[..., QK_COLS:], DA_HEAD_DIM) * q_scale
            da_c = _diff_head_out(_diff_attend(q1_c, q2_c, k1_c, k2_c, v_c, lam), subln_g[l], lam_init)
            sg_c = _spatial_gate(jnp.einsum('bld,dc->blc', h_c, wi[:, KV_HI:]),
                                 sg_ln_g[l], sg_ln_b[l], sg_w[l], sg_b[l])
            y_c = jnp.einsum('blc,cd->bld', jnp.concatenate([da_c, sg_c], axis=-1), w_out[l])
            ctx = _layer_norm(DEEPNORM_ALPHA * ctx + cg1 * y_c, ln1_g[l], ln1_b[l])
            f_c = _hier_moe(_modulate(ctx, csh2, csc2), router_group_w[l], router_group_b[l],
                            router_expert_w[l], router_expert_b[l],
                            exp_w_gate[l], exp_w_up[l], exp_w_down[l])
            ctx = _layer_norm(DEEPNORM_ALPHA * ctx + cg2 * f_c, ln2_g[l], ln2_b[l])
    return x
```

```python
import math
from contextlib import ExitStack

import numpy as np
import concourse.bass as bass
import concourse.mybir as mybir
from concourse.bass_utils import run_bass_kernel_spmd

F32 = mybir.dt.float32
BF16 = mybir.dt.bfloat16
I32 = mybir.dt.int32
AF = mybir.ActivationFunctionType
ALU = mybir.AluOpType
AX = mybir.AxisListType

NCORES = 8
D = 1024
L = 2048
CTXL = 256
NKT = 18
NB = 2
NT = 16
NE = 32
CAP = 512
R_OV = 8
CAPO = 256
NMAIN = NE * CAP
ZROW = NMAIN + R_OV * CAPO
NSLOT = ZROW
ALPHA = 2.0 ** 0.25
LAM_INIT = 0.8 - 0.6 * math.exp(0.0)
EPS = 1e-5
BIG = 1.0e4

COMPUTE = ("pe", "act", "dve", "pool")
QUEUES = ("sp", "act", "pool")


class Buf:
    __slots__ = ("name", "lw", "rd")

    def __init__(self, name):
        self.name = name
        self.lw = None
        self.rd = []


class Op:
    __slots__ = ("eng", "fn", "deps", "signal", "sigval", "is_dma", "slot", "use", "cidx")


class _Rec:
    def __init__(self):
        self.call = None

    def __getattr__(self, name):
        def f(*a, **k):
            self.call = (name, a, k)
            return self
        return f


class Late:
    def __init__(self, builder):
        self.builder = builder


class Sched:
    def __init__(self, nslots=12):
        self.ops = {e: [] for e in ("pe", "act", "dve", "pool", "sp")}
        self.ccount = {e: 0 for e in self.ops}
        self.dcount = {e: 0 for e in self.ops}
        self.nslots = nslots
        self.slot_last = {}

    def _add(self, eng, fn, reads, writes, is_dma):
        op = Op()
        if fn is not None and not isinstance(fn, Late):
            rec = _Rec()
            fn(rec)
            fn = rec.call
        op.eng, op.fn, op.is_dma = eng, fn, is_dma
        op.signal, op.sigval, op.slot, op.use = False, None, None, None
        deps = []
        for b in reads:
            if b.lw is not None:
                deps.append(b.lw)
        for b in writes:
            if b.lw is not None:
                deps.append(b.lw)
            deps.extend(b.rd)
        op.cidx = self.ccount[eng]
        if is_dma:
            j = self.dcount[eng]
            self.dcount[eng] += 1
            op.slot = j % self.nslots
            op.use = j // self.nslots + 1
            prev = self.slot_last.get((eng, op.slot))
            if prev is not None:
                deps.append(prev)
            self.slot_last[(eng, op.slot)] = op
        else:
            self.ccount[eng] += 1
        seen = set()
        op.deps = []
        for d in deps:
            if d is op or id(d) in seen:
                continue
            seen.add(id(d))
            op.deps.append(d)
        for b in reads:
            b.rd.append(op)
        for b in writes:
            b.lw = op
            b.rd = []
        self.ops[eng].append(op)
        return op

    def op(self, eng, fn, reads=(), writes=()):
        return self._add(eng, fn, list(reads), list(writes), False)

    def dma(self, eng, fn, reads=(), writes=()):
        return self._add(eng, fn, list(reads), list(writes), True)

    def wait_all(self, eng, bufs):
        return self._add(eng, None, list(bufs), [], False)

    @staticmethod
    def alias(new, olds):
        for o in olds:
            if o.lw is not None:
                new.rd.append(o.lw)
            new.rd.extend(o.rd)

    @staticmethod
    def _needs_wait(op, d):
        if d.is_dma or d.eng != op.eng:
            return True
        if op.eng == "pe":
            return False
        if op.eng == "pool" or op.is_dma:
            return True
        return (op.cidx - d.cidx) < 2

    def emit(self, nc, stack):
        for lst in self.ops.values():
            for op in lst:
                for d in op.deps:
                    if not d.is_dma and self._needs_wait(op, d):
                        d.signal = True
        for lst in self.ops.values():
            c = 0
            for op in lst:
                if not op.is_dma and op.fn is not None and op.signal:
                    c += 1
                    op.sigval = c
        csem = {e: stack.enter_context(nc.semaphore("c_" + e)) for e in COMPUTE}
        dsem = {}
        for q in QUEUES:
            for s in range(min(self.nslots, self.dcount[q])):
                dsem[(q, s)] = stack.enter_context(nc.semaphore("d_%s%d" % (q, s)))
        sched = self

        def run(eng_obj, e):
            known = {}
            bc_reg = eng_obj.to_reg(NSLOT - 1) if e == "pool" else None
            bc_reg2 = eng_obj.to_reg(NSLOT) if e == "pool" else None
            for op in sched.ops[e]:
                for d in op.deps:
                    if not sched._needs_wait(op, d):
                        continue
                    if d.is_dma:
                        sem, val, key = dsem[(d.eng, d.slot)], 16 * d.use, ("d", d.eng, d.slot)
                    else:
                        sem, val, key = csem[d.eng], d.sigval, ("c", d.eng)
                    if known.get(key, 0) >= val:
                        continue
                    known[key] = val
                    eng_obj.wait_ge(sem, val)
                if op.fn is None:
                    continue
                if isinstance(op.fn, Late):
                    inst = op.fn.builder(eng_obj)
                    inst.then_inc(dsem[(e, op.slot)], 16)
                    continue
                try:
                    kw = op.fn[2]
                    if kw.get("bounds_check") == "BCREG":
                        kw = dict(kw)
                        kw["bounds_check"] = bc_reg
                    elif kw.get("bounds_check") == "BCREG2":
                        kw = dict(kw)
                        kw["bounds_check"] = bc_reg2
                    inst = getattr(eng_obj, op.fn[0])(*op.fn[1], **kw)
                except Exception:
                    ii = sched.ops[e].index(op)
                    print("PREV", [(o_.fn[0] if o_.fn else None) for o_ in sched.ops[e][max(0, ii - 12):ii]], ii, flush=True)
                    print("FAILED OP", e, op.fn[0], [getattr(a, "shape", a) for a in op.fn[1]],
                          {k: getattr(v, "shape", v) for k, v in op.fn[2].items()}, flush=True)
                    raise
                if op.is_dma:
                    inst.then_inc(dsem[(e, op.slot)], 16)
                elif op.signal:
                    inst.then_inc(csem[e], 1)

        with nc.Block() as block:
            @block.sync
            def _(eng):
                run(eng, "sp")

            @block.tensor
            def _(eng):
                run(eng, "pe")

            @block.scalar
            def _(eng):
                run(eng, "act")

            @block.vector
            def _(eng):
                run(eng, "dve")

            @block.gpsimd
            def _(eng):
                run(eng, "pool")


def build_nc(debug=False):
    nc = bass.Bass("TRN2", target_bir_lowering=False)

    def din(name, shape, dt=F32):
        return nc.dram_tensor(name, list(shape), dt, kind="ExternalInput").ap()

    x_d = din("x", [NB, L, D])
    ctx_d = din("ctx", [NB, CTXL, D])
    cT_d = din("cT", [128, 8, 3])
    wmod_d = din("w_mod", [D, 6 * D])
    bmod_d = din("b_mod", [1, 6 * D])
    win_d = din("w_in", [D, 2560])
    wout_d = din("w_out", [D, D])
    lamv_d = din("lamv", [1, 256])
    subg_d = din("subln_g", [1, 128])
    sglg_d = din("sg_ln_g", [1, 512])
    sglb_d = din("sg_ln_b", [1, 512])
    sgwT_d = din("sg_wT", [4, 128, 128])
    sgb_d = din("sg_b", [1, 512])
    ln1g_d = din("ln1_g", [1, D])
    ln1b_d = din("ln1_b", [1, D])
    ln2g_d = din("ln2_g", [1, D])
    ln2b_d = din("ln2_b", [1, D])
    rw_d = din("router_w", [D, 36])
    rb_d = din("router_b", [1, 36])
    wall_d = din("exp_w", [NE, 3, D, D])
    ident_d = din("ident", [128, 128])
    ustr_d = din("ustrict", [128, 128])
    ropec_d = din("rope_cos", [L, 64])
    ropes_d = din("rope_sin", [L, 64])
    econst_d = din("econst", [1, 5 * NE])
    out_d = nc.dram_tensor("out", [NB, L, D], F32, kind="ExternalOutput").ap()
    dbg_d = nc.dram_tensor("dbg", [NB * L, D], F32, kind="ExternalOutput").ap() if debug else None

    modd = nc.dram_tensor("modd", [3, 6 * D], F32, kind="Internal").ap()
    x1d = nc.dram_tensor("x1d", [NB * L, D], F32, kind="Internal").ap()
    tbd = nc.dram_tensor("tbd", [NB * L, D], BF16, kind="Internal").ap()
    Xd = nc.dram_tensor("Xd", [NSLOT, D], BF16, kind="Internal").ap()
    Yd = nc.dram_tensor("Yd", [NSLOT + 1, D], F32, kind="Internal").ap()
    b_modd = Buf("modd")
    B_x1d = [Buf("x1d%d" % i) for i in range(NB * NT)]
    B_Xd = [Buf("Xd%d" % i) for i in range(NB * NT * 2)]
    B_Yd = [Buf("Yd%d" % i) for i in range(NE * (CAP // 128) + R_OV * (CAPO // 128))]
    B_Xo = [Buf("Xo%d" % i) for i in range(NB * NT * 2)]
    B_vs = [Buf("vs%d" % i) for i in range(NB * NT)]
    B_perm, B_io = Buf("perm"), Buf("idxo")
    B_out = [Buf("out%d" % i) for i in range(NB * NT)]
    B_dbg = [Buf("dbg%d" % i) for i in range(NB * NT)]

    S = Sched()
    st = ExitStack()
    st.enter_context(nc.allow_low_precision("bf16 matmul operands, fp32 accumulation"))
    st.enter_context(nc.allow_non_contiguous_dma(reason="tiny setup gathers"))

    def sb(name, shape, dt):
        return st.enter_context(nc.sbuf_tensor(name, list(shape), dt))

    ident_f = sb("ident_f", [128, 128], F32)
    ident_b = sb("ident_b", [128, 128], BF16)
    ones_b = sb("ones_b", [128, 128], BF16)
    ustr_b = sb("ustr_b", [128, 128], BF16)
    rw_sb = sb("rw_sb", [128, 8, 36], F32)
    rb_b = sb("rb_b", [128, 36], F32)
    econst = sb("econst_sb", [128, 5, NE], F32)
    pslot = sb("pslot", [128, NB * NT, 2], F32)
    eslot = sb("eslot", [128, NB * NT, 2], F32)
    idxo_sb = sb("idxo_sb", [128, NB * NT, 2], I32)
    perm_i = sb("perm_i", [128, R_OV], I32)
    gvec = sb("gvec", [128, 128], F32)
    lamt = sb("lamt", [128, 256], F32)
    lams = sb("lams", [128, 8], F32)
    cT_sb = sb("cT_sb", [128, 8, 3], F32)
    silT = sb("silT", [128, 8, 3], BF16)
    modT = sb("modT", [128, 16, 3], F32)
    Mb = sb("Mb", [128, NB * NT, NE], BF16)
    idx_sb = sb("idx_sb", [128, NB * NT, 2], I32)
    wts_sb = sb("wts_sb", [128, NB * NT, 2], F32)
    idxg_sb = sb("idxg_sb", [128, NB * NT, 2], I32)
    B_const = Buf("const")
    B_lam = Buf("lam")
    B_modT = Buf("modT")
    B_Mb = [Buf("Mb%d" % i) for i in range(NB * NT)]
    B_idx = [Buf("idx%d" % i) for i in range(NB * NT)]
    B_wts = [Buf("wts%d" % i) for i in range(NB * NT)]

    P = [st.enter_context(nc.psum_tensor("P%d" % i, [128, 512], F32)) for i in range(8)]
    PB = [Buf("P%d" % i) for i in range(8)]

    ARENA_B = 199 * 1024
    arena = sb("arena", [128, ARENA_B // 2], BF16)

    def carve(off, nbytes, dt, pattern=None, **kw):
        assert off % 4 == 0 and off + nbytes <= ARENA_B, (off, nbytes)
        v = arena[:, off // 2:(off + nbytes) // 2]
        if dt == F32:
            v = v.bitcast(F32)
        elif dt == I32:
            v = v.bitcast(I32)
        if pattern:
            v = v.rearrange(pattern, **kw)
        return v

    o = 0
    wi = carve(o, 40960, BF16, "p (k n) -> p k n", k=8); o += 40960
    ropec = carve(o, 4096, F32, "p (n d) -> p n d", n=16); o += 4096
    ropes = carve(o, 4096, F32, "p (n d) -> p n d", n=16); o += 4096
    sglg = carve(o, 2048, F32); o += 2048
    sglb = carve(o, 2048, F32); o += 2048
    sgwT = carve(o, 1024, BF16, "p (g q) -> p g q", g=4); o += 1024
    sgb = carve(o, 1024, BF16); o += 1024
    ln1g = carve(o, 4096, F32); o += 4096
    ln1b = carve(o, 4096, F32); o += 4096
    g1b = carve(o, 4096, F32); o += 4096
    opsc2 = carve(o, 4096, F32); o += 4096
    sh2b = carve(o, 4096, F32); o += 4096
    hcT = carve(o, 4096, BF16, "p (k n) -> p k n", k=8)
    qtm2, ktm2, vln2 = carve(o, 1024, BF16), carve(o + 1024, 1024, BF16), carve(o + 2048, 1024, BF16)
    o += 4096
    mixT = carve(o, 32768, BF16, "p (k n) -> p k n", k=8); o += 32768
    O_ATT = o
    qT = carve(o, 16384, BF16, "p (k n) -> p k n", k=4); o += 16384
    kT = carve(o, 18432, BF16, "p (k n) -> p k n", k=4); o += 18432
    Vaug = carve(o, 18720, BF16, "p (t h e) -> p t h e", t=NKT, h=4); o += 18944
    O_A = o
    xl = [carve(o + i * 4096, 4096, F32) for i in range(2)]; o += 8192
    hTg = carve(o, 8192, BF16, "p (k n) -> p k n", k=8); o += 8192
    uT = carve(o, 4096, BF16, "p (k n) -> p k n", k=4); o += 4096
    gv = carve(o, 8192, F32, "p (j n) -> p j n", j=4); o += 8192
    r1 = carve(o, 2048, F32); o += 2048
    r2 = carve(o, 2048, F32); o += 2048
    qtm = carve(o, 1024, BF16); o += 1024
    ktm = carve(o, 1024, BF16); o += 1024
    vln = carve(o, 1024, BF16); o += 1024
    st6 = carve(o, 192, F32, "p (j s) -> p j s", j=8); o += 192
    mvs = carve(o, 64, F32, "p (j s) -> p j s", j=8); o += 64
    rst = carve(o, 64, F32); o += 64
    MIX_END = o
    assert MIX_END <= ARENA_B, MIX_END
    o = O_A
    PT = [carve(o + i * 1024, 1024, BF16) for i in range(4)]; o += 4096
    osb = [carve(o + i * 4224, 4128, F32, "p (m q e) -> p m q e", m=2, q=4) for i in range(2)]; o += 8448
    obuf = carve(o, 2048, F32, "p (q e) -> p q e", q=4); o += 2048
    sqb = carve(o, 2048, F32, "p (q e) -> p q e", q=4); o += 2048
    datm = carve(o, 1024, BF16, "p (q e) -> p q e", q=4); o += 1024
    rsb = carve(o, 64, F32); o += 64
    lnt = carve(o, 64, F32); o += 64
    assert o <= MIX_END
    ATT_TMP = 17792
    wout = carve(O_A + ATT_TMP, 16384, BF16, "p (k n) -> p k n", k=8)
    assert O_A + ATT_TMP + 16384 <= MIX_END
    RG = 8
    rtb = carve(O_A, 2240 * 4, F32)
    assert 2240 * 4 <= ATT_TMP
    o = O_ATT
    xr = [carve(o + i * 4096, 4096, F32) for i in range(2)]; o += 8192
    zt = [carve(o + i * 4096, 4096, F32) for i in range(3)]; o += 12288
    x1t = [carve(o + i * 4096, 4096, F32) for i in range(2)]; o += 8192
    tt = [carve(o + i * 4096, 4096, F32) for i in range(2)]; o += 8192
    tb = [carve(o + i * 2048, 2048, BF16) for i in range(2)]; o += 4096
    tT = carve(o, 4096, F32, "p (k n) -> p k n", k=8); o += 4096
    lgall = carve(o, NT * 36 * 4, F32, "p (n c) -> p n c", n=NT); o += NT * 36 * 4
    st6d = [carve(o + i * 64, 48, F32, "p (j s) -> p j s", j=2) for i in range(3)]; o += 192
    mvd = [carve(o + i * 64, 64, F32) for i in range(3)]; o += 192
    assert o <= O_A, o
    tbl = [mixT[:, c_, 0:1024] for c_ in range(4)]
    o = 0
    wgS = [carve(o + i * 16384, 16384, BF16, "p (k n) -> p k n", k=8) for i in range(2)]; o += 32768
    wuS = [carve(o + i * 16384, 16384, BF16, "p (k n) -> p k n", k=8) for i in range(2)]; o += 32768
    wdS = [carve(o + i * 16384, 16384, BF16, "p (k n) -> p k n", k=8) for i in range(2)]; o += 32768
    O_WOV = o
    wov = [carve(o, 49152, BF16, "p (k n) -> p k n", k=24)]; o += 49152
    XTm = [carve(o + i * 8192, 8192, BF16, "p (k n) -> p k n", k=8) for i in range(2)]; o += 16384
    XTo = carve(o, 4096, BF16, "p (k n) -> p k n", k=8); o += 4096
    hidT = carve(o, 8192, BF16, "p (k n) -> p k n", k=8); o += 8192
    xg = [carve(o + i * 2048, 2048, BF16) for i in range(4)]; o += 8192
    sgt = [carve(o + i * 2048, 2048, F32) for i in range(2)]; o += 4096
    ysb = [carve(o + i * 4096, 4096, F32) for i in range(2)]; o += 8192
    tbo = [carve(o + i * 2048, 2048, BF16) for i in range(2)]; o += 4096
    assert o <= ARENA_B, o
    o = 0
    g2bt = [carve(o + i * 4096, 4096, F32) for i in range(2)]; o += 8192
    ln2g = carve(o, 4096, F32); o += 4096
    ln2b = carve(o, 4096, F32); o += 4096
    Y1 = [carve(o + i * 4096, 4096, F32) for i in range(4)]; o += 16384
    Y2 = [carve(o + i * 4096, 4096, F32) for i in range(4)]; o += 16384
    x1r = [carve(o + i * 4096, 4096, F32) for i in range(4)]; o += 16384
    fst = [carve(o + i * 128, 128, F32) for i in range(2)]; o += 256
    assert o <= 98304, o

    B_wi, B_rope, B_sgc, B_ln1, B_bc, B_hcT = Buf("wi"), Buf("rope"), Buf("sgc"), Buf("ln1"), Buf("bc"), Buf("hcT")
    S.dma("sp", lambda e: e.dma_start(out=ident_f[:], in_=ident_d), writes=[B_const])
    S.dma("sp", lambda e: e.dma_start(out=cT_sb[:], in_=cT_d), writes=[B_const])
    S.dma("sp", lambda e: e.dma_start(out=rw_sb[:], in_=rw_d.rearrange("(k p) n -> p k n", p=128)), writes=[B_const])
    S.dma("sp", lambda e: e.dma_start(out=rb_b[:], in_=rb_d.partition_broadcast(128)), writes=[B_const])
    S.dma("sp", lambda e: e.dma_start(out=econst[:].rearrange("p a e -> p (a e)"), in_=econst_d.partition_broadcast(128)), writes=[B_const])
    S.dma("sp", lambda e: e.dma_start(out=gvec[:], in_=subg_d.partition_broadcast(128)), writes=[B_const])
    S.dma("sp", lambda e: e.dma_start(out=lamt[:], in_=lamv_d.partition_broadcast(128)), writes=[B_const])
    S.dma("pool", lambda e: e.dma_start(out=ustr_b[:], in_=ustr_d), writes=[B_const])
    S.op("dve", lambda e: e.tensor_copy(ident_b[:], ident_f[:]), reads=[B_const], writes=[B_const])
    S.op("dve", lambda e: e.memset(ones_b[:], 1.0), writes=[B_const])
    S.op("dve", lambda e: e.tensor_scalar_mul(gvec[:], gvec[:], 1.0 - LAM_INIT), reads=[B_const], writes=[B_const])
    S.op("dve", lambda e: e.tensor_tensor(lamt[:, 0:64], lamt[:, 0:64], lamt[:, 64:128], ALU.mult), reads=[B_const], writes=[B_lam])
    S.op("dve", lambda e: e.tensor_tensor(lamt[:, 128:192], lamt[:, 128:192], lamt[:, 192:256], ALU.mult), reads=[B_const, B_lam], writes=[B_lam])
    S.op("dve", lambda e: e.memset(lams[:], 0.0), writes=[B_lam])
    S.op("dve", lambda e: e.reduce_sum(lams[:, 0:1], lamt[:, 0:64], axis=AX.X), reads=[B_lam], writes=[B_lam])
    S.op("dve", lambda e: e.reduce_sum(lams[:, 1:2], lamt[:, 128:192], axis=AX.X), reads=[B_lam], writes=[B_lam])
    S.op("act", lambda e: e.activation(out=lams[:, 2:4], in_=lams[:, 0:2], func=AF.Exp), reads=[B_lam], writes=[B_lam])
    S.op("dve", lambda e: e.tensor_tensor(lams[:, 4:5], lams[:, 2:3], lams[:, 3:4], ALU.subtract), reads=[B_lam], writes=[B_lam])
    S.op("dve", lambda e: e.tensor_scalar(lams[:, 5:6], lams[:, 4:5], LAM_INIT, -1.0, ALU.add, ALU.mult), reads=[B_lam], writes=[B_lam])
    for hh in range(4):
        S.dma("pool", lambda e, hh=hh: e.dma_start(out=wi[:, :, hh * 640:(hh + 1) * 640], in_=win_d[:, hh * 640:(hh + 1) * 640].rearrange("(k p) n -> p k n", p=128)), writes=[B_wi])
    S.dma("sp", lambda e: e.dma_start(out=ropec, in_=ropec_d.rearrange("(n p) d -> p n d", p=128)), writes=[B_rope])
    S.dma("sp", lambda e: e.dma_start(out=ropes, in_=ropes_d.rearrange("(n p) d -> p n d", p=128)), writes=[B_rope])
    S.dma("sp", lambda e: e.dma_start(out=sglg, in_=sglg_d.partition_broadcast(128)), writes=[B_sgc])
    S.dma("sp", lambda e: e.dma_start(out=sglb, in_=sglb_d.partition_broadcast(128)), writes=[B_sgc])
    S.dma("pool", lambda e: e.dma_start(out=sgwT, in_=sgwT_d.rearrange("g q p -> q g p")), writes=[B_sgc])
    S.dma("pool", lambda e: e.dma_start(out=sgb[0:1, :], in_=sgb_d), writes=[B_sgc])
    S.dma("sp", lambda e: e.dma_start(out=ln1g, in_=ln1g_d.partition_broadcast(128)), writes=[B_ln1])
    S.dma("sp", lambda e: e.dma_start(out=ln1b, in_=ln1b_d.partition_broadcast(128)), writes=[B_ln1])

    S.op("act", lambda e: e.activation(out=silT[:], in_=cT_sb[:], func=AF.Silu), reads=[B_const], writes=[B_const])
    wm = [xl[0].bitcast(BF16).rearrange("p (k n) -> p k n", k=8)[:, :, 0:256],
          xl[1].bitcast(BF16).rearrange("p (k n) -> p k n", k=8)[:, :, 0:256]]
    B_wm = [Buf("wm0"), Buf("wm1")]
    bm3 = [gv[0:3, 0, 0:256], gv[0:3, 1, 0:256]]
    mrow = [gv[0:3, 2, 0:256], gv[0:3, 3, 0:256]]
    B_bm3 = [Buf("bm0"), Buf("bm1")]
    B_mrow = [Buf("mr0"), Buf("mr1")]
    def mod_dma(nb, wmv, bmv, Bw, Bb):
        cs = slice(nb * 256, (nb + 1) * 256)
        S.dma("pool", lambda e: e.dma_start(out=wmv, in_=wmod_d[:, cs].rearrange("(k p) n -> p k n", p=128)), writes=[Bw])
        S.dma("sp", lambda e: e.dma_start(out=bmv, in_=bmod_d[:, cs].partition_broadcast(3)), writes=[Bb])

    def mod_compute(nb, wmv, bmv, mrv, Bw, Bb, Bm, bank):
        cs = slice(nb * 256, (nb + 1) * 256)
        for k in range(8):
            S.op("pe", lambda e, k=k: e.matmul(P[bank][0:3, 0:256], lhsT=silT[:, k, :], rhs=wmv[:, k, :], start=(k == 0), stop=(k == 7)),
                 reads=[B_const, Bw], writes=[PB[bank]])
        S.op("dve", lambda e: e.tensor_tensor(mrv, P[bank][0:3, 0:256], bmv, ALU.add), reads=[PB[bank], Bb], writes=[Bm])
        S.dma("sp", lambda e: e.dma_start(out=modd[:, cs], in_=mrv), reads=[Bm], writes=[b_modd])

    def mod_block(nb, wmv, bmv, mrv, Bw, Bb, Bm, bank):
        mod_dma(nb, wmv, bmv, Bw, Bb)
        mod_compute(nb, wmv, bmv, mrv, Bw, Bb, Bm, bank)

    for nb in range(8):
        i = nb % 2
        mod_block(nb, wm[i], bm3[i], mrow[i], B_wm[i], B_bm3[i], B_mrow[i], i)
    B_xl = [Buf("xl%d" % i) for i in range(2)]
    for i in range(2):
        Sched.alias(B_xl[i], [B_wm[i]])
    B_hTg, B_uT, B_gv = Buf("hTg"), Buf("uT"), Buf("gv")
    Sched.alias(B_gv, B_bm3 + B_mrow)
    gvflat = gv.rearrange("p j n -> p (j n)")
    S.dma("sp", lambda e: e.dma_start(out=gvflat[0:3, :], in_=modd[:, 0:2048]), reads=[b_modd], writes=[B_gv])
    for j in range(16):
        S.op("pe", lambda e, j=j: e.transpose(P[0][:, j * 3:(j + 1) * 3], gvflat[0:3, j * 128:(j + 1) * 128], ident_f[0:3, 0:3]), reads=[B_gv, B_const], writes=[PB[0]])
    S.op("dve", lambda e: e.tensor_copy(modT[:], P[0][:, 0:48].rearrange("p (j v) -> p j v", j=16)), reads=[PB[0]], writes=[B_modT])
    S.op("dve", lambda e: e.tensor_scalar_add(modT[:, 8:16, :], modT[:, 8:16, :], 1.0), reads=[B_modT], writes=[B_modT])
    B_r1, B_r2, B_qtm, B_ktm, B_vln, B_st = Buf("r1"), Buf("r2"), Buf("qtm"), Buf("ktm"), Buf("vln"), Buf("st")
    B_mixT = [Buf("mixT%d" % i) for i in range(NT)]
    B_qT = [Buf("qT%d" % i) for i in range(NT)]
    B_kT = [Buf("kT%d" % i) for i in range(NKT)]
    B_V = [Buf("V%d" % i) for i in range(NKT)]
    B_qtm2 = [B_qtm, Buf("qtm2")]
    B_ktm2 = [B_ktm, Buf("ktm2")]
    B_vln2 = [B_vln, Buf("vln2")]
    B_regA = [B_xl[0], B_xl[1], B_hTg, B_uT, B_gv, B_r1, B_r2, B_qtm, B_ktm, B_vln, B_st]
    xl_ctr = [0]
    pb_ctr = [0]

    def next_bank(lo=2, n=6):
        i = lo + pb_ctr[0] % n
        pb_ctr[0] += 1
        return i

    P_bf = [P[i][:].bitcast(BF16) for i in range(8)]
    tile_ctr = [0]

    B_tbd_all = []
    B_wg, B_wu, B_wd = [Buf("wg0"), Buf("wg1")], [Buf("wu0"), Buf("wu1")], [Buf("wd0"), Buf("wd1")]
    early = set()
    for b in range(NB):
        def vcol(v):
            return (lambda k: modT[:, 8 + k, v:v + 1]), (lambda k: modT[:, k, v:v + 1])

        S.op("pool", lambda e: e.memset(Vaug[:, :, :, 128:130], 1.0), reads=[], writes=B_V)

        def proj_block(lhs_fn, lhs_bufs, cols, bank):
            for k in range(8):
                S.op("pe", lambda e, k=k: e.matmul(P[bank][:, :], lhsT=lhs_fn(k), rhs=wi[:, k, cols], start=(k == 0), stop=(k == 7)),
                     reads=lhs_bufs + [B_wi], writes=[PB[bank]])

        def rope_block(bank, n, dst, dstbuf):
            pv = P[bank][:, :]
            cosb = ropec[:, n, :].unsqueeze(1).to_broadcast([128, 8, 64])
            S.op("dve", lambda e: e.tensor_tensor(r1.rearrange("p (a d) -> p a d", a=8), pv.rearrange("p (a d) -> p a d", a=8), cosb, ALU.mult),
                 reads=[PB[bank], B_rope], writes=[B_r1])
            p4 = pv.rearrange("p (a x h) -> p a x h", a=8, x=2)
            r24 = r2.rearrange("p (a x h) -> p a x h", a=8, x=2)
            s4 = ropes[:, n, :].rearrange("p (x h) -> p x h", x=2).unsqueeze(1).to_broadcast([128, 8, 2, 32])
            S.op("dve", lambda e: e.tensor_tensor(r24[:, :, :, 0:16], p4[:, :, :, 16:32], s4[:, :, :, 0:16], ALU.mult),
                 reads=[PB[bank], B_rope], writes=[B_r2])
            S.op("dve", lambda e: e.tensor_tensor(r24[:, :, :, 16:32], p4[:, :, :, 0:16], s4[:, :, :, 16:32], ALU.mult),
                 reads=[PB[bank], B_rope], writes=[B_r2])
            S.op("dve", lambda e: e.tensor_tensor(dst, r1, r2, ALU.add), reads=[B_r1, B_r2], writes=[dstbuf])

        def to_featmajor(src, srcbuf, dstT, col0, dstbuf, scale):
            tb_ = next_bank()
            for c in range(4):
                S.op("pe", lambda e, c=c: e.transpose(P_bf[tb_][:, c * 128:(c + 1) * 128], src[:, c * 128:(c + 1) * 128], ident_b[:]),
                     reads=[srcbuf, B_const], writes=[PB[tb_]])
            S.op("act", lambda e: e.activation(out=dstT[:, :, col0:col0 + 128], in_=P_bf[tb_][:, 0:512].rearrange("p (c n) -> p c n", c=4),
                                               func=AF.Copy, scale=scale), reads=[PB[tb_]], writes=[dstbuf])

        Sched.alias(B_hcT, [B_qtm2[1], B_ktm2[1], B_vln2[1]])
        scf, shf = vcol(2)
        for j in range(2):
            xi = xl_ctr[0] % 2; xl_ctr[0] += 1
            S.dma("sp", lambda e, j=j, xi=xi: e.dma_start(out=xl[xi], in_=ctx_d[b, j * 128:(j + 1) * 128, :]), writes=[B_xl[xi]])
            for k in range(8):
                bank = k % 2
                S.op("pe", lambda e, k=k, xi=xi, bank=bank: e.transpose(P[bank][:, 0:128], xl[xi][:, k * 128:(k + 1) * 128], ident_f[:]),
                     reads=[B_xl[xi], B_const], writes=[PB[bank]])
                S.op("act", lambda e, k=k, j=j, bank=bank: e.activation(out=hcT[:, k, j * 128:(j + 1) * 128], in_=P[bank][:, 0:128], func=AF.Identity,
                                                                         scale=scf(k), bias=shf(k)), reads=[PB[bank], B_modT], writes=[B_hcT])
        for j in range(2):
            bank = next_bank()
            proj_block(lambda k, j=j: hcT[:, k, j * 128:(j + 1) * 128], [B_hcT], slice(512, 1024), bank)
            S.op("act", lambda e, bank=bank: e.activation(out=ktm, in_=P[bank][:, :], func=AF.Copy), reads=[PB[bank]], writes=[B_ktm])
            to_featmajor(ktm, B_ktm, kT, L + j * 128, B_kT[16 + j], 1.0)
            bank = next_bank()
            proj_block(lambda k, j=j: hcT[:, k, j * 128:(j + 1) * 128], [B_hcT], slice(1024, 1536), bank)
            S.op("act", lambda e, bank=bank, j=j: e.activation(out=Vaug[:, 16 + j, :, 0:128], in_=P[bank][:, :].rearrange("p (h e) -> p h e", h=4), func=AF.Copy),
                 reads=[PB[bank]], writes=[B_V[16 + j]])

        if b == 0:
            wm_l = [mixT[:, i, :].rearrange("p (k n) -> p k n", k=8) for i in range(3)]
            sm_l = [mixT[0:3, 3, i * 512:(i + 1) * 512].bitcast(F32) for i in range(3)]
            B_wml = [Buf("wml%d" % i) for i in range(3)]
            B_sml = [Buf("sml%d" % i) for i in range(3)]
            late = list(range(8, 24))
            late_dma = list(range(8, 24))

            def late_dma_next():
                if late_dma:
                    nb2 = late_dma.pop(0)
                    i2 = nb2 % 3
                    mod_dma(nb2, wm_l[i2], sm_l[i2], B_wml[i2], B_sml[i2])
            late_dma_next()
            late_dma_next()
        for nb_ in (B_qtm2[1], B_ktm2[1], B_vln2[1]):
            Sched.alias(nb_, [B_hcT])
        scf, shf = vcol(b)
        for g in range(4):
            xis = []
            for j in range(4):
                n = g * 4 + j
                xi = xl_ctr[0] % 2; xl_ctr[0] += 1
                xis.append(xi)
                S.dma("sp", lambda e, n=n, xi=xi: e.dma_start(out=xl[xi], in_=x_d[b, n * 128:(n + 1) * 128, :]), writes=[B_xl[xi]])
                for k in range(8):
                    bank = k // 4
                    S.op("pe", lambda e, k=k, xi=xi, bank=bank: e.transpose(P[bank][:, (k % 4) * 128:(k % 4 + 1) * 128], xl[xi][:, k * 128:(k + 1) * 128], ident_f[:]),
                         reads=[B_xl[xi], B_const], writes=[PB[bank]])
                    if k % 4 == 3:
                        for kk in range(k - 3, k + 1):
                            S.op("act", lambda e, kk=kk, j=j, bank=bank: e.activation(out=hTg[:, kk, j * 128:(j + 1) * 128], in_=P[bank][:, (kk % 4) * 128:(kk % 4 + 1) * 128],
                                                                                      func=AF.Identity, scale=scf(kk), bias=shf(kk)),
                                 reads=[PB[bank], B_modT], writes=[B_hTg])
            for c in range(4):
                bank = next_bank()
                for k in range(8):
                    S.op("pe", lambda e, k=k, c=c, bank=bank: e.matmul(P[bank][:, :], lhsT=wi[:, k, 1536 + c * 128:1536 + (c + 1) * 128], rhs=hTg[:, k, :],
                                                                       start=(k == 0), stop=(k == 7)), reads=[B_wi, B_hTg], writes=[PB[bank]])
                S.op("act", lambda e, c=c, bank=bank: e.activation(out=uT[:, c, :], in_=P[bank][:, :], func=AF.Gelu), reads=[PB[bank]], writes=[B_uT])
            for j in range(4):
                bank = next_bank()
                proj_block(lambda k, j=j: hTg[:, k, j * 128:(j + 1) * 128], [B_hTg], slice(2048, 2560), bank)
                S.op("act", lambda e, j=j, bank=bank: e.activation(out=gv[:, j, :], in_=P[bank][:, :], func=AF.Gelu), reads=[PB[bank]], writes=[B_gv])
                S.op("dve", lambda e, j=j: e.bn_stats(st6[:, j, :], gv[:, j, :]), reads=[B_gv], writes=[B_st])
                S.op("dve", lambda e, j=j: e.bn_aggr(mvs[:, j, :], st6[:, j, :]), reads=[B_st], writes=[B_st])
            S.op("act", lambda e: e.activation(out=rst[:, 0:4], in_=mvs[:, 0:4, 1], func=AF.Ln, bias=EPS), reads=[B_st], writes=[B_st])
            S.op("act", lambda e: e.activation(out=rst[:, 4:8], in_=rst[:, 0:4], func=AF.Exp, scale=-0.5), reads=[B_st], writes=[B_st])
            qtms, ktms, vlns = [qtm, qtm2], [ktm, ktm2], [vln, vln2]

            def stage1(j):
                n = g * 4 + j
                d = n % 2
                lhs = lambda k: hTg[:, k, j * 128:(j + 1) * 128]
                bq = next_bank()
                proj_block(lhs, [B_hTg], slice(0, 512), bq)
                bk = next_bank()
                proj_block(lhs, [B_hTg], slice(512, 1024), bk)
                bv = next_bank()
                proj_block(lhs, [B_hTg], slice(1024, 1536), bv)
                rope_block(bq, n, qtms[d], B_qtm2[d])
                rope_block(bk, n, ktms[d], B_ktm2[d])
                S.op("act", lambda e: e.activation(out=Vaug[:, n, :, 0:128], in_=P[bv][:, :].rearrange("p (h e) -> p h e", h=4), func=AF.Copy),
                     reads=[PB[bv]], writes=[B_V[n]])
                S.op("dve", lambda e: e.tensor_scalar(gv[:, j, :], gv[:, j, :], mvs[:, j, 0:1], rst[:, 4 + j:5 + j], ALU.subtract, ALU.mult),
                     reads=[B_gv, B_st], writes=[B_gv])
                S.op("pool", lambda e: e.tensor_tensor(gv[:, j, :], gv[:, j, :], sglg, ALU.mult), reads=[B_gv, B_sgc], writes=[B_gv])
                S.op("pool", lambda e: e.tensor_tensor(vlns[d], gv[:, j, :], sglb, ALU.add), reads=[B_gv, B_sgc], writes=[B_vln2[d]])

            def stage2(j):
                n = g * 4 + j
                d = n % 2
                to_featmajor(qtms[d], B_qtm2[d], qT, n * 128, B_qT[n], 0.125)
                to_featmajor(ktms[d], B_ktm2[d], kT, n * 128, B_kT[n], 1.0)
                bank = next_bank()
                for gg in range(4):
                    S.op("pe", lambda e, gg=gg: e.matmul(P[bank][:, gg * 128:(gg + 1) * 128], lhsT=vlns[d][:, gg * 128:(gg + 1) * 128], rhs=sgwT[:, gg, :],
                                                         start=True, stop=False), reads=[B_vln2[d], B_sgc], writes=[PB[bank]])
                    S.op("pe", lambda e, gg=gg: e.matmul(P[bank][:, gg * 128:(gg + 1) * 128], lhsT=ones_b[0:1, :], rhs=sgb[0:1, gg * 128:(gg + 1) * 128],
                                                         start=False, stop=True), reads=[B_const, B_sgc], writes=[PB[bank]])
                S.op("dve", lambda e: e.tensor_tensor(mixT[:, 4:8, n * 128:(n + 1) * 128], P[bank][:, :].rearrange("p (g q) -> p g q", g=4),
                                                      uT[:, :, j * 128:(j + 1) * 128], ALU.mult),
                     reads=[PB[bank], B_uT], writes=[B_mixT[n]])

            for j in range(4):
                stage1(j)
                if j > 0:
                    stage2(j - 1)
                if b == 0 and late:
                    nb = late.pop(0)
                    i = nb % 3
                    mod_compute(nb, wm_l[i], sm_l[i], sm_l[i], B_wml[i], B_sml[i], B_sml[i], next_bank())
                    late_dma_next()
            stage2(3)
            if b == 0 and g == 3:
                for mb_ in B_mixT:
                    Sched.alias(mb_, B_wml + B_sml)

        B_PT = [Buf("PT%d" % i) for i in range(4)]
        B_osb = [Buf("osb0"), Buf("osb1")]
        B_ob, B_sq, B_da, B_rs = Buf("obuf"), Buf("sqb"), Buf("datm"), Buf("rsb")
        for nb_ in B_PT + B_osb + [B_ob, B_sq, B_da, B_rs]:
            Sched.alias(nb_, B_regA)
        if b == NB - 1:
            Sched.alias(B_wg[0], [B_wi])
            Sched.alias(B_wg[1], [B_wi])
            Sched.alias(B_wu[0], [B_wi, B_rope])
            S.dma("pool", lambda e: e.dma_start(out=wgS[0], in_=wall_d[0, 0].rearrange("(k p) n -> p k n", p=128)), writes=[B_wg[0]])
            S.dma("pool", lambda e: e.dma_start(out=wuS[0], in_=wall_d[0, 1].rearrange("(k p) n -> p k n", p=128)), writes=[B_wu[0]])
            S.dma("pool", lambda e: e.dma_start(out=wgS[1], in_=wall_d[1, 0].rearrange("(k p) n -> p k n", p=128)), writes=[B_wg[1]])
            early.update([(0, 0), (0, 1), (1, 0)])
        B_wout = Buf("wout")
        Sched.alias(B_wout, B_regA)
        S.dma("pool", lambda e: e.dma_start(out=wout, in_=wout_d.rearrange("(k p) n -> p k n", p=128)), writes=[B_wout])
        pairs = [(hp, qb, m, kt) for hp in range(2) for qb in range(4) for m in range(2) for kt in range(NKT)]
        NI = len(pairs)
        SBK = [(0, 1), (6, 7)]

        def emit_S(i):
            hp, qb, m, kt = pairs[i]
            ch = m * 2 + hp
            qs0 = qb * 512
            for hh in range(2):
                r0 = hh * 64
                sb_ = SBK[i % 2][hh]
                S.op("pe", lambda e: e.matmul(P[sb_][:, :], lhsT=kT[r0:r0 + 64, ch, kt * 128:(kt + 1) * 128],
                                              rhs=qT[r0:r0 + 64, ch, qs0:qs0 + 512], start=True, stop=True),
                     reads=[B_kT[kt]] + B_qT[qb * 4:qb * 4 + 4], writes=[PB[sb_]])
            for hh in range(2):
                sb_ = SBK[i % 2][hh]
                pt = (i % 2) * 2 + hh
                S.op("act", lambda e: e.activation(out=PT[pt], in_=P[sb_][:, :], func=AF.Exp), reads=[PB[sb_]], writes=[B_PT[pt]])

        def emit_AV(i):
            hp, qb, m, kt = pairs[i]
            for hh in range(2):
                pt = (i % 2) * 2 + hh
                for qs in range(4):
                    ob = 2 + hh * 2 + qs // 2
                    S.op("pe", lambda e, qs=qs, ob=ob: e.matmul(P[ob][:, (qs % 2) * 129:(qs % 2) * 129 + 129], lhsT=PT[pt][:, qs * 128:(qs + 1) * 128],
                                                                rhs=Vaug[:, kt, 2 * hp + hh, 0:129], start=(kt == 0), stop=(kt == NKT - 1)),
                         reads=[B_PT[pt], B_V[kt]], writes=[PB[ob]])

        def evac(m):
            for hh in range(2):
                for hb in range(2):
                    ob = 2 + hh * 2 + hb
                    S.op("dve", lambda e, hh=hh, hb=hb, ob=ob: e.tensor_copy(osb[hh][:, m, hb * 2:hb * 2 + 2, :], P[ob][:, 0:258].rearrange("p (q e) -> p q e", q=2)),
                         reads=[PB[ob]], writes=[B_osb[hh]])

        def post_A(h, qb):
            oi = h % 2
            osv = osb[oi]
            S.op("dve", lambda e: e.reciprocal(rsb[:, 0:8].rearrange("p (m q) -> p m q", m=2), osv[:, :, :, 128]), reads=[B_osb[oi]], writes=[B_rs])
            S.op("dve", lambda e: e.tensor_scalar_mul(rsb[:, 8:12], rsb[:, 4:8], lams[:, 5:6]), reads=[B_rs, B_lam], writes=[B_rs])
            for qs in range(4):
                S.op("dve", lambda e, qs=qs: e.tensor_scalar_mul(obuf[:, qs, :], osv[:, 0, qs, 0:128], rsb[:, qs:qs + 1]), reads=[B_osb[oi], B_rs], writes=[B_ob])
                S.op("dve", lambda e, qs=qs: e.scalar_tensor_tensor(obuf[:, qs, :], osv[:, 1, qs, 0:128], rsb[:, 8 + qs:9 + qs], obuf[:, qs, :], ALU.mult, ALU.add),
                     reads=[B_osb[oi], B_rs, B_ob], writes=[B_ob])
            S.op("pool", lambda e: e.tensor_tensor(sqb, obuf, obuf, ALU.mult), reads=[B_ob], writes=[B_sq])
            S.op("dve", lambda e: e.reduce_sum(rsb[:, 12:16], sqb, axis=AX.X), reads=[B_sq], writes=[B_rs])

        def post_B(h, qb):
            S.op("act", lambda e: e.activation(out=lnt[:, 0:4], in_=rsb[:, 12:16], func=AF.Ln, scale=1.0 / 128.0, bias=EPS), reads=[B_rs], writes=[B_rs])
            S.op("act", lambda e: e.activation(out=lnt[:, 4:8], in_=lnt[:, 0:4], func=AF.Exp, scale=-0.5), reads=[B_rs], writes=[B_rs])
            for qs in range(4):
                S.op("dve", lambda e, qs=qs: e.scalar_tensor_tensor(datm[:, qs, :], obuf[:, qs, :], lnt[:, 4 + qs:5 + qs], gvec[:], ALU.mult, ALU.mult),
                     reads=[B_ob, B_rs, B_const], writes=[B_da])

        def post_C(h, qb):
            qs0 = qb * 512
            for qs in range(4):
                S.op("pe", lambda e, qs=qs: e.transpose(P_bf[6][:, qs * 128:(qs + 1) * 128], datm[:, qs, :], ident_b[:]), reads=[B_da, B_const], writes=[PB[6]])
            S.op("dve", lambda e: e.tensor_copy(mixT[:, h, qs0:qs0 + 512], P_bf[6][:, 0:512]), reads=[PB[6]], writes=B_mixT[qb * 4:qb * 4 + 4])

        pending = []
        emit_S(0)
        for i in range(NI):
            if i + 1 < NI:
                emit_S(i + 1)
            emit_AV(i)
            hp, qb, m, kt = pairs[i]
            if kt == NKT - 1:
                evac(m)
                if m == 1:
                    pending.append((i + 1, post_A, 2 * hp, qb))
                    pending.append((i + 4, post_B, 2 * hp, qb))
                    pending.append((i + 7, post_C, 2 * hp, qb))
                    pending.append((i + 8, post_A, 2 * hp + 1, qb))
                    pending.append((i + 11, post_B, 2 * hp + 1, qb))
                    pending.append((i + 14, post_C, 2 * hp + 1, qb))
            while pending and pending[0][0] <= i:
                _, fn_, h_, qb_ = pending.pop(0)
                fn_(h_, qb_)
        for _, fn_, h_, qb_ in pending:
            fn_(h_, qb_)

        B_tT, B_lg, B_rtb = Buf("tT"), Buf("lg"), Buf("rtb")
        B_std = [Buf("std%d" % i) for i in range(3)]
        B_xr, B_zt, B_x1t, B_tt, B_tb = ([Buf("xr%d" % i) for i in range(2)], [Buf("zt%d" % i) for i in range(3)],
                                         [Buf("x1t%d" % i) for i in range(2)], [Buf("tt%d" % i) for i in range(2)], [Buf("tb%d" % i) for i in range(2)])
        B_tbd = [Buf("tbd%d_%d" % (b, i)) for i in range(NT)]
        B_tbd_all.extend(B_tbd)
        B_tbl = [Buf("tbl%d" % i) for i in range(4)]
        old = B_qT + B_kT + B_V
        for nb_ in [B_tT, B_lg] + B_std + B_xr + B_zt + B_x1t + B_tt + B_tb:
            Sched.alias(nb_, old)
        Sched.alias(B_rtb, B_PT + B_osb + [B_ob, B_sq, B_da, B_rs])
        S.dma("sp", lambda e, b=b: e.dma_start(out=g1b, in_=modd[b:b + 1, 2048:3072].partition_broadcast(128)), reads=[b_modd], writes=[B_bc])
        S.dma("sp", lambda e, b=b: e.dma_start(out=sh2b, in_=modd[b:b + 1, 3072:4096].partition_broadcast(128)), reads=[b_modd], writes=[B_bc])
        S.dma("sp", lambda e, b=b: e.dma_start(out=opsc2, in_=modd[b:b + 1, 4096:5120].partition_broadcast(128)), reads=[b_modd], writes=[B_bc])
        S.op("pool", lambda e: e.tensor_scalar_add(opsc2, opsc2, 1.0), reads=[B_bc], writes=[B_bc])

        S.op("pool", lambda e: e.tensor_tensor(xr[0], ln1b, opsc2, ALU.mult), reads=[B_ln1, B_bc], writes=[B_xr[0]])
        S.op("pool", lambda e: e.tensor_tensor(sh2b, sh2b, xr[0], ALU.add), reads=[B_xr[0], B_bc], writes=[B_bc])
        S.op("pool", lambda e: e.tensor_tensor(opsc2, opsc2, ln1g, ALU.mult), reads=[B_ln1, B_bc], writes=[B_bc])

        obank = {}

        def stageA1pe(n):
            w = n % 2
            S.dma("sp", lambda e: e.dma_start(out=xr[w], in_=x_d[b, n * 128:(n + 1) * 128, :]), writes=[B_xr[w]])
            obank[n] = []
            for hf in range(2):
                bank = next_bank(0, 4)
                obank[n].append(bank)
                for c in range(8):
                    S.op("pe", lambda e, c=c: e.matmul(P[bank][:, :], lhsT=mixT[:, c, n * 128:(n + 1) * 128], rhs=wout[:, c, hf * 512:(hf + 1) * 512],
                                                       start=(c == 0), stop=(c == 7)), reads=[B_mixT[n], B_wout], writes=[PB[bank]])

        def stageA1dve(n):
            w = n % 2
            z = n % 3
            for hf in range(2):
                bank = obank[n][hf]
                S.op("dve", lambda e: e.tensor_tensor(zt[z][:, hf * 512:(hf + 1) * 512], P[bank][:, :], g1b[:, hf * 512:(hf + 1) * 512], ALU.mult),
                     reads=[PB[bank], B_bc], writes=[B_zt[z]])
            S.op("dve", lambda e: e.scalar_tensor_tensor(zt[z], xr[w], ALPHA, zt[z], ALU.mult, ALU.add), reads=[B_xr[w], B_zt[z]], writes=[B_zt[z]])
            for hf in range(2):
                S.op("dve", lambda e, hf=hf: e.bn_stats(st6d[z][:, hf, :], zt[z][:, hf * 512:(hf + 1) * 512]), reads=[B_zt[z]], writes=[B_std[z]])
            S.op("dve", lambda e: e.bn_aggr(mvd[z][:, 0:2], st6d[z][:, :, :]), reads=[B_std[z]], writes=[B_std[z]])
            S.op("act", lambda e: e.activation(out=mvd[z][:, 2:3], in_=mvd[z][:, 1:2], func=AF.Ln, bias=EPS), reads=[B_std[z]], writes=[B_std[z]])
            S.op("act", lambda e: e.activation(out=mvd[z][:, 3:4], in_=mvd[z][:, 2:3], func=AF.Exp, scale=-0.5), reads=[B_std[z]], writes=[B_std[z]])

        def stageA2(n):
            gi = b * NT + n
            w = n % 2
            z = n % 3
            S.op("dve", lambda e: e.tensor_scalar(mvd[z][:, 4:5], mvd[z][:, 0:1], mvd[z][:, 3:4], -1.0, ALU.mult, ALU.mult), reads=[B_std[z]], writes=[B_std[z]])
            S.op("act", lambda e: e.activation(out=zt[z], in_=zt[z], func=AF.Identity, scale=mvd[z][:, 3:4], bias=mvd[z][:, 4:5]), reads=[B_zt[z], B_std[z]], writes=[B_zt[z]])
            S.op("pool", lambda e: e.tensor_tensor(x1t[w], zt[z], ln1g, ALU.mult), reads=[B_zt[z], B_ln1], writes=[B_x1t[w]])
            S.op("pool", lambda e: e.tensor_tensor(x1t[w], x1t[w], ln1b, ALU.add), reads=[B_x1t[w], B_ln1], writes=[B_x1t[w]])
            S.dma("sp", lambda e: e.dma_start(out=x1d[gi * 128:(gi + 1) * 128, :], in_=x1t[w]), reads=[B_x1t[w]], writes=[B_x1d[gi]])
            S.op("dve", lambda e: e.tensor_tensor(tt[w], zt[z], opsc2, ALU.mult), reads=[B_zt[z], B_bc], writes=[B_tt[w]])
            S.op("dve", lambda e: e.tensor_tensor(tt[w], tt[w], sh2b, ALU.add), reads=[B_tt[w], B_bc], writes=[B_tt[w]])
            S.op("act", lambda e: e.activation(out=tb[w], in_=tt[w], func=AF.Copy), reads=[B_tt[w]], writes=[B_tb[w]])
            S.dma("act", lambda e: e.dma_start(out=tbd[gi * 128:(gi + 1) * 128, :], in_=tb[w]), reads=[B_tb[w]], writes=[B_tbd[n]])
            if debug:
                S.dma("sp", lambda e: e.dma_start(out=dbg_d[gi * 128:(gi + 1) * 128, :], in_=tt[w]), reads=[B_tt[w]], writes=[B_dbg[gi]])

        def stageB(n):
            w = n % 2
            for hf in range(2):
                bank = 4 + hf
                for k4 in range(4):
                    k = hf * 4 + k4
                    S.op("pe", lambda e, k=k, k4=k4: e.transpose(P[bank][:, k4 * 128:(k4 + 1) * 128], tt[w][:, k * 128:(k + 1) * 128], ident_f[:]),
                         reads=[B_tt[w], B_const], writes=[PB[bank]])
                S.op("act", lambda e: e.activation(out=tT[:, hf * 4:hf * 4 + 4, :], in_=P[bank][:, :].rearrange("p (k n) -> p k n", k=4), func=AF.Copy),
                     reads=[PB[bank]], writes=[B_tT])
            for k in range(8):
                S.op("pe", lambda e, k=k: e.matmul(P[6][:, (n % 2) * 64:(n % 2) * 64 + 36], lhsT=tT[:, k, :], rhs=rw_sb[:, k, :], start=(k == 0), stop=(k == 7)),
                     reads=[B_tT, B_const], writes=[PB[6]])

        def lgadd(n):
            S.op("dve", lambda e: e.tensor_tensor(lgall[:, n, :], P[6][:, (n % 2) * 64:(n % 2) * 64 + 36], rb_b[:], ALU.add), reads=[PB[6], B_const], writes=[B_lg])

        def route_chunk(c):
            G = RG
            t0 = c * G
            ts = slice(t0, t0 + G)
            gis = slice(b * NT + t0, b * NT + t0 + G)
            off = [0]

            def alloc(wd):
                v = rtb[:, off[0]:off[0] + G * wd].rearrange("p (g x) -> p g x", g=G)
                off[0] += G * wd
                return v
            gmax, gone, gex, gsum, gw, pen = alloc(1), alloc(4), alloc(4), alloc(1), alloc(1), alloc(4)
            elm, m1, mk1, elm2, m2, mk2 = alloc(32), alloc(1), alloc(32), alloc(32), alloc(1), alloc(32)
            dm, ex_, den, rr, ovf, dst, prod, dsum = alloc(1), alloc(1), alloc(1), alloc(1), alloc(32), alloc(32), alloc(64), alloc(2)
            assert off[0] <= 2240
            R = [B_rtb]
            gl = lgall[:, ts, 0:4]
            el = lgall[:, ts, 4:36]
            bc = lambda v, k: v.to_broadcast([128, G, k])
            S.op("dve", lambda e: e.reduce_max(gmax[:, :, 0], gl, axis=AX.X), reads=[B_lg], writes=R)
            S.op("dve", lambda e: e.tensor_tensor(gone, gl, bc(gmax, 4), ALU.is_ge), reads=[B_lg] + R, writes=R)
            S.op("dve", lambda e: e.tensor_tensor(gex, gl, bc(gmax, 4), ALU.subtract), reads=[B_lg] + R, writes=R)
            S.op("act", lambda e: e.activation(out=gex, in_=gex, func=AF.Exp), reads=R, writes=R)
            S.op("dve", lambda e: e.reduce_sum(gsum[:, :, 0], gex, axis=AX.X), reads=R, writes=R)
            S.op("dve", lambda e: e.reciprocal(gw, gsum), reads=R, writes=R)
            S.op("dve", lambda e: e.tensor_scalar(pen, gone, BIG, -BIG, ALU.mult, ALU.add), reads=R, writes=R)
            S.op("dve", lambda e: e.tensor_tensor(elm.rearrange("p g (a x) -> p g a x", a=4), el.rearrange("p g (a x) -> p g a x", a=4),
                                                  pen.unsqueeze(3).to_broadcast([128, G, 4, 8]), ALU.add), reads=[B_lg] + R, writes=R)
            S.op("dve", lambda e: e.reduce_max(m1[:, :, 0], elm, axis=AX.X), reads=R, writes=R)
            S.op("dve", lambda e: e.tensor_tensor(mk1, elm, bc(m1, 32), ALU.is_ge), reads=R, writes=R)
            S.op("dve", lambda e: e.scalar_tensor_tensor(elm2, mk1, -BIG, elm, ALU.mult, ALU.add), reads=R, writes=R)
            S.op("dve", lambda e: e.reduce_max(m2[:, :, 0], elm2, axis=AX.X), reads=R, writes=R)
            S.op("dve", lambda e: e.tensor_tensor(mk2, elm2, bc(m2, 32), ALU.is_ge), reads=R, writes=R)
            S.op("dve", lambda e: e.tensor_tensor(dm, m2, m1, ALU.subtract), reads=R, writes=R)
            S.op("act", lambda e: e.activation(out=ex_, in_=dm, func=AF.Exp), reads=R, writes=R)
            S.op("dve", lambda e: e.tensor_scalar_add(den, ex_, 1.0), reads=R, writes=R)
            S.op("dve", lambda e: e.reciprocal(rr, den), reads=R, writes=R)
            Wb = [B_wts[gi] for gi in range(b * NT + t0, b * NT + t0 + G)]
            S.op("dve", lambda e: e.tensor_tensor(wts_sb[:, gis, 0:1], rr, gw, ALU.mult), reads=R, writes=Wb)
            S.op("dve", lambda e: e.tensor_tensor(wts_sb[:, gis, 1:2], gw, wts_sb[:, gis, 0:1], ALU.subtract), reads=R + Wb, writes=Wb)
            Mbb = [B_Mb[gi] for gi in range(b * NT + t0, b * NT + t0 + G)]
            S.op("dve", lambda e: e.tensor_tensor(Mb[:, gis, :], mk1, mk2, ALU.add), reads=R, writes=Mbb)
            for il in range(G):
                gi = b * NT + t0 + il
                for jj in range(gi):
                    S.op("pe", lambda e, jj=jj, il=il: e.matmul(P[7][:, il * NE:(il + 1) * NE], lhsT=ones_b[:], rhs=Mb[:, jj, :], start=(jj == 0), stop=False),
                         reads=[B_Mb[jj], B_const], writes=[PB[7]])
                S.op("pe", lambda e, gi=gi, il=il: e.matmul(P[7][:, il * NE:(il + 1) * NE], lhsT=ustr_b[:], rhs=Mb[:, gi, :], start=(gi == 0), stop=True),
                     reads=[B_Mb[gi], B_const], writes=[PB[7]])
            pos = P[7][:, 0:G * NE].rearrange("p (g x) -> p g x", g=G)
            eb1024 = econst[:, 0, :].unsqueeze(1).to_broadcast([128, G, NE])
            ebcap = econst[:, 1, :].unsqueeze(1).to_broadcast([128, G, NE])
            S.op("dve", lambda e: e.tensor_scalar_min(dst, pos, 1023.0), reads=[PB[7]], writes=R)
            S.op("dve", lambda e: e.tensor_scalar(ovf, dst, float(CAP), 1.0e6, ALU.is_ge, ALU.mult), reads=R, writes=R)
            S.op("dve", lambda e: e.tensor_tensor(ovf, ovf, ebcap, ALU.add), reads=R + [B_const], writes=R)
            S.op("dve", lambda e: e.tensor_tensor(ovf, ovf, dst, ALU.add), reads=R, writes=R)
            Ib = [B_idx[gi] for gi in range(b * NT + t0, b * NT + t0 + G)]
            Vb = [B_vs[gi] for gi in range(b * NT + t0, b * NT + t0 + G)]
            S.op("dve", lambda e: e.tensor_tensor(prod[:, :, 0:32], ovf, mk1, ALU.mult), reads=R, writes=R)
            S.op("dve", lambda e: e.tensor_tensor(prod[:, :, 32:64], ovf, mk2, ALU.mult), reads=R, writes=R)
            S.op("dve", lambda e: e.reduce_sum(dsum.rearrange("p g a -> p (g a)"), prod.rearrange("p g (a x) -> p (g a) x", a=2), axis=AX.X), reads=R, writes=R)
            S.op("dve", lambda e: e.tensor_copy(idx_sb[:, gis, :], dsum), reads=R, writes=Ib)
            S.op("dve", lambda e: e.tensor_tensor(prod[:, :, 0:32], dst, mk1, ALU.mult), reads=R + Ib, writes=R)
            S.op("dve", lambda e: e.tensor_tensor(prod[:, :, 32:64], dst, mk2, ALU.mult), reads=R, writes=R)
            S.op("dve", lambda e: e.reduce_sum(pslot[:, gis, :].rearrange("p g a -> p (g a)"), prod.rearrange("p g (a x) -> p (g a) x", a=2), axis=AX.X), reads=R, writes=Vb)
            S.op("dve", lambda e: e.tensor_tensor(prod[:, :, 0:32], mk1, eb1024, ALU.mult), reads=R + Vb + [B_const], writes=R)
            S.op("dve", lambda e: e.tensor_tensor(prod[:, :, 32:64], mk2, eb1024, ALU.mult), reads=R + [B_const], writes=R)
            S.op("dve", lambda e: e.reduce_sum(eslot[:, gis, :].rearrange("p g a -> p (g a)"), prod.rearrange("p g (a x) -> p (g a) x", a=2), axis=AX.X), reads=R, writes=Vb)
            for nb_ in B_tbl:
                Sched.alias(nb_, B_mixT[0:8])
            for il in range(G):
                n = t0 + il
                gi = b * NT + n
                w = n % 4
                S.dma("sp", lambda e: e.dma_start(out=tbl[w], in_=tbd[gi * 128:(gi + 1) * 128, :]), reads=[B_tbd[n]], writes=[B_tbl[w]])
                for s_ in range(2):
                    S.dma("pool", lambda e, s_=s_: e.indirect_dma_start(
                        out=Xd, out_offset=bass.IndirectOffsetOnAxis(ap=idx_sb[:, gi, s_:s_ + 1], axis=0), in_=tbl[w], in_offset=None,
                        bounds_check="BCREG", oob_is_err=False), reads=[B_tbl[w], B_idx[gi]], writes=[B_Xd[gi * 2 + s_]])

        stageA1pe(0)
        stageA1pe(1)
        stageA1dve(0)
        for n in range(NT):
            if n >= 1:
                stageB(n - 1)
            if n + 2 < NT:
                stageA1pe(n + 2)
            stageA2(n)
            if n + 1 < NT:
                stageA1dve(n + 1)
            if n >= 2:
                lgadd(n - 2)
            if n == RG + 1:
                route_chunk(0)
        stageB(NT - 1)
        lgadd(NT - 2)
        lgadd(NT - 1)
        route_chunk(1)
        for mb_ in B_mixT:
            Sched.alias(mb_, B_tbl)
        prev_1d = [B_wout, B_tT, B_lg, B_rtb] + B_std + B_xr + B_zt + B_x1t + B_tt + B_tb + B_tbl
        for nb_ in B_regA + B_qT + B_kT + B_V:
            Sched.alias(nb_, prev_1d)

    mixer_all = prev_1d + B_regA + B_qT + B_kT + B_V + B_mixT + [B_qtm2[1], B_ktm2[1], B_vln2[1], B_wi, B_rope, B_sgc, B_ln1, B_bc, B_hcT]
    B_rk = Buf("rank")
    Sched.alias(B_rk, mixer_all)
    rk = carve(O_WOV, 2240 * 4, F32)
    T1 = carve(O_WOV + 8960, 4096, F32, "p (a c) -> p a c", a=32)
    T2 = carve(O_WOV + 8960 + 4096, 4096, F32, "p (a c) -> p a c", a=32)
    RK = [B_rk]
    cnt, cntu, rank_, permf = rk[:, 0:32], rk[:, 32:64], rk[:, 64:96], rk[:, 96:104]
    sv = lambda i: rk[:, 128 + i * 64:128 + (i + 1) * 64]
    pos_s, e1k, qs_s, ovp, vm, vo, mainf, ovi, t_a, t_b = [sv(i) for i in range(10)]
    pflat = pslot[:].rearrange("p g a -> p (g a)")
    eflat = eslot[:].rearrange("p g a -> p (g a)")
    def emit_ranking():
        for j in range(NB * NT):
            S.op("pe", lambda e, j=j: e.matmul(P[7][:, 0:NE], lhsT=ones_b[:], rhs=Mb[:, j, :], start=(j == 0), stop=(j == NB * NT - 1)),
                 reads=[B_Mb[j], B_const], writes=[PB[7]])
        S.op("dve", lambda e: e.tensor_tensor(cntu, P[7][:, 0:NE], econst[:, 2, :], ALU.add), reads=[PB[7], B_const], writes=RK)
        S.op("dve", lambda e: e.tensor_tensor(T1, cntu.unsqueeze(1).to_broadcast([128, NE, NE]), cntu.unsqueeze(2).to_broadcast([128, NE, NE]), ALU.is_gt), reads=RK, writes=RK)
        S.op("dve", lambda e: e.reduce_sum(rank_, T1, axis=AX.X), reads=RK, writes=RK)
        T2v = T2[:, 0:R_OV, :]
        S.op("dve", lambda e: e.tensor_tensor(T2v, rank_.unsqueeze(1).to_broadcast([128, R_OV, NE]), econst[:, 4, 0:R_OV].unsqueeze(2).to_broadcast([128, R_OV, NE]), ALU.is_equal),
             reads=RK + [B_const], writes=RK)
        S.op("dve", lambda e: e.tensor_tensor(T2v, T2v, econst[:, 3, :].unsqueeze(1).to_broadcast([128, R_OV, NE]), ALU.mult), reads=RK + [B_const], writes=RK)
        S.op("dve", lambda e: e.reduce_sum(permf, T2v, axis=AX.X), reads=RK, writes=RK)
        S.op("dve", lambda e: e.tensor_copy(perm_i[:], permf), reads=RK, writes=[B_perm])
        S.op("dve", lambda e: e.tensor_copy(pos_s, pflat), reads=B_vs, writes=RK)
        S.op("dve", lambda e: e.tensor_copy(e1k, eflat), reads=B_vs, writes=RK)
        for hh in range(2):
            S.op("dve", lambda e, hh=hh: e.tensor_tensor(T1, e1k[:, hh * 32:(hh + 1) * 32].unsqueeze(2).to_broadcast([128, 32, NE]),
                                                         econst[:, 0, :].unsqueeze(1).to_broadcast([128, 32, NE]), ALU.is_equal), reads=RK + [B_const], writes=RK)
            S.op("dve", lambda e: e.tensor_tensor(T1, T1, rank_.unsqueeze(1).to_broadcast([128, 32, NE]), ALU.mult), reads=RK, writes=RK)
            S.op("dve", lambda e, hh=hh: e.reduce_sum(qs_s[:, hh * 32:(hh + 1) * 32], T1, axis=AX.X), reads=RK, writes=RK)
        S.op("dve", lambda e: e.tensor_single_scalar(vm, pos_s, float(CAP), ALU.is_lt), reads=RK, writes=RK)
        S.op("dve", lambda e: e.tensor_scalar_add(ovp, pos_s, -float(CAP)), reads=RK, writes=RK)
        S.op("dve", lambda e: e.tensor_single_scalar(vo, ovp, float(CAPO), ALU.is_lt), reads=RK, writes=RK)
        S.op("dve", lambda e: e.tensor_single_scalar(t_a, ovp, 0.0, ALU.is_ge), reads=RK, writes=RK)
        S.op("dve", lambda e: e.tensor_tensor(vo, vo, t_a, ALU.mult), reads=RK, writes=RK)
        S.op("dve", lambda e: e.tensor_single_scalar(t_a, qs_s, float(R_OV), ALU.is_lt), reads=RK, writes=RK)
        S.op("dve", lambda e: e.tensor_tensor(vo, vo, t_a, ALU.mult), reads=RK, writes=RK)
        S.op("dve", lambda e: e.scalar_tensor_tensor(mainf, e1k, float(CAP) / 1024.0, pos_s, ALU.mult, ALU.add), reads=RK, writes=RK)
        S.op("dve", lambda e: e.scalar_tensor_tensor(ovi, qs_s, float(CAPO), ovp, ALU.mult, ALU.add), reads=RK, writes=RK)
        S.op("dve", lambda e: e.tensor_scalar_add(ovi, ovi, float(NMAIN)), reads=RK, writes=RK)
        S.op("dve", lambda e: e.tensor_scalar_add(t_a, mainf, -float(ZROW)), reads=RK, writes=RK)
        S.op("dve", lambda e: e.tensor_tensor(t_a, t_a, vm, ALU.mult), reads=RK, writes=RK)
        S.op("dve", lambda e: e.tensor_scalar_add(t_b, ovi, -float(ZROW)), reads=RK, writes=RK)
        S.op("dve", lambda e: e.tensor_tensor(t_b, t_b, vo, ALU.mult), reads=RK, writes=RK)
        S.op("dve", lambda e: e.tensor_tensor(t_a, t_a, t_b, ALU.add), reads=RK, writes=RK)
        S.op("dve", lambda e: e.tensor_scalar_add(t_a, t_a, float(ZROW)), reads=RK, writes=RK)
        S.op("dve", lambda e: e.tensor_copy(idxg_sb[:].rearrange("p g a -> p (g a)"), t_a), reads=RK, writes=B_idx)
        S.op("dve", lambda e: e.tensor_scalar_add(t_b, ovi, -1.0e6), reads=RK, writes=RK)
        S.op("dve", lambda e: e.tensor_tensor(t_b, t_b, vo, ALU.mult), reads=RK, writes=RK)
        S.op("dve", lambda e: e.tensor_scalar_add(t_b, t_b, 1.0e6), reads=RK, writes=RK)
        S.op("dve", lambda e: e.tensor_copy(idxo_sb[:].rearrange("p g a -> p (g a)"), t_b), reads=RK, writes=[B_io])


    B_XT, B_hid = [Buf("XT0"), Buf("XT1")], Buf("hid")
    B_xg, B_sgt, B_ysb = [Buf("xg%d" % i) for i in range(4)], [Buf("sgt0"), Buf("sgt1")], [Buf("ysb%d" % i) for i in range(2)]
    B_tbo = [Buf("tbo0"), Buf("tbo1")]
    moe_bufs = B_wg + B_wu + B_wd + B_XT + [B_hid] + B_xg + B_sgt + B_ysb + B_tbo
    for nb_ in moe_bufs:
        if nb_ in (B_wg[0], B_wg[1], B_wu[0]):
            continue
        Sched.alias(nb_, mixer_all)

    def load_expert(ex):
        i = ex % 2
        if (ex, 0) not in early:
            S.dma("pool", lambda e: e.dma_start(out=wgS[i], in_=wall_d[ex, 0].rearrange("(k p) n -> p k n", p=128)), writes=[B_wg[i]])
        if (ex, 1) not in early:
            S.dma("pool", lambda e: e.dma_start(out=wuS[i], in_=wall_d[ex, 1].rearrange("(k p) n -> p k n", p=128)), writes=[B_wu[i]])
        S.dma("pool", lambda e: e.dma_start(out=wdS[i], in_=wall_d[ex, 2].rearrange("(k p) n -> p k n", p=128)), writes=[B_wd[i]])

    ctr = {"xg": 0, "y": 0, "g": 0, "xt": 0}

    def prep_X(row0, ntile, xi_t, xreads, xt, Bxt):
        for s_ in range(ntile):
            xi = ctr["xg"] % 4
            ctr["xg"] += 1
            S.dma("sp", lambda e, s_=s_, xi=xi: e.dma_start(out=xg[xi], in_=Xd[row0 + s_ * 128: row0 + (s_ + 1) * 128, :]), reads=xreads, writes=[B_xg[xi]])
            for hf in range(2):
                bank = next_bank(0, 2)
                for k4 in range(4):
                    k = hf * 4 + k4
                    S.op("pe", lambda e, k=k, k4=k4, xi=xi, bank=bank: e.transpose(P_bf[bank][:, k4 * 128:(k4 + 1) * 128], xg[xi][:, k * 128:(k + 1) * 128], ident_b[:]),
                         reads=[B_xg[xi], B_const], writes=[PB[bank]])
                S.op("dve", lambda e, hf=hf, s_=s_, bank=bank: e.tensor_copy(xt[:, hf * 4:hf * 4 + 4, s_ * 128:(s_ + 1) * 128], P_bf[bank][:, 0:512].rearrange("p (k n) -> p k n", k=4)),
                     reads=[PB[bank]], writes=[Bxt])

    def compute(wgv, wuv, wdv, Bw, xt, Bxt, ncap, row0, yd0):
        for f in range(8):
            pg = 2 + (ctr["g"] % 2)
            pu = 4 + (ctr["g"] % 2)
            sg_i = ctr["g"] % 2
            ctr["g"] += 1
            for k in range(8):
                S.op("pe", lambda e, k=k: e.matmul(P[pg][:, 0:ncap], lhsT=wgv[:, k, f * 128:(f + 1) * 128], rhs=xt[:, k, 0:ncap],
                                                   start=(k == 0), stop=(k == 7)), reads=Bw + [Bxt], writes=[PB[pg]])
            for k in range(8):
                S.op("pe", lambda e, k=k: e.matmul(P[pu][:, 0:ncap], lhsT=wuv[:, k, f * 128:(f + 1) * 128], rhs=xt[:, k, 0:ncap],
                                                   start=(k == 0), stop=(k == 7)), reads=Bw + [Bxt], writes=[PB[pu]])
            S.op("act", lambda e: e.activation(out=sgt[sg_i][:, 0:ncap], in_=P[pg][:, 0:ncap], func=AF.Silu), reads=[PB[pg]], writes=[B_sgt[sg_i]])
            S.op("dve", lambda e: e.tensor_tensor(hidT[:, f, 0:ncap], P[pu][:, 0:ncap], sgt[sg_i][:, 0:ncap], ALU.mult),
                 reads=[PB[pu], B_sgt[sg_i]], writes=[B_hid])
        for s_ in range(ncap // 128):
            yi = ctr["y"] % 2
            ctr["y"] += 1
            for dh in range(2):
                pd = 6 + dh
                for f in range(8):
                    S.op("pe", lambda e, f=f: e.matmul(P[pd][:, :], lhsT=hidT[:, f, s_ * 128:(s_ + 1) * 128], rhs=wdv[:, f, dh * 512:(dh + 1) * 512],
                                                       start=(f == 0), stop=(f == 7)), reads=[B_hid] + Bw, writes=[PB[pd]])
                if dh == 0:
                    S.op("act", lambda e: e.activation(out=ysb[yi][:, 0:512], in_=P[pd][:, :], func=AF.Copy), reads=[PB[pd]], writes=[B_ysb[yi]])
                else:
                    S.op("dve", lambda e: e.tensor_copy(ysb[yi][:, 512:1024], P[pd][:, :]), reads=[PB[pd]], writes=[B_ysb[yi]])
            S.dma("act", lambda e: e.dma_start(out=Yd[row0 + s_ * 128: row0 + (s_ + 1) * 128, :], in_=ysb[yi]), reads=[B_ysb[yi]], writes=[B_Yd[yd0 + s_]])

    ov_tiles = list(range(NB * NT))

    def overflow_scatter(k):
        for _ in range(k):
            if not ov_tiles:
                return
            gi = ov_tiles.pop(0)
            w = gi % 2
            S.dma("sp", lambda e: e.dma_start(out=tbo[w], in_=tbd[gi * 128:(gi + 1) * 128, :]), reads=[B_tbd_all[gi]], writes=[B_tbo[w]])
            for s_ in range(2):
                S.dma("pool", lambda e, s_=s_: e.indirect_dma_start(
                    out=Xd, out_offset=bass.IndirectOffsetOnAxis(ap=idxo_sb[:, gi, s_:s_ + 1], axis=0), in_=tbo[w], in_offset=None,
                    bounds_check="BCREG", oob_is_err=False), reads=[B_tbo[w], B_io], writes=[B_Xo[gi * 2 + s_]])

    NS = CAP // 128
    NSO = CAPO // 128
    B_wov = [Buf("wov0")]
    B_XTo = Buf("XTo")
    for nb_ in B_wov + [B_XTo]:
        Sched.alias(nb_, mixer_all + [B_rk])

    def load_overflow(r):
        dst = wov[0]

        def bld(eng):
            reg = eng.alloc_register("pr%d" % r)
            eng.reg_load(reg, perm_i[0:1, r:r + 1])
            v = eng.snap(reg, donate=True, min_val=0, max_val=NE - 1)
            return eng.dma_start(out=dst, in_=wall_d[bass.ds(v, 1)].rearrange("o w (k p) n -> p (o w k) n", p=128))
        S.dma("pool", Late(bld), reads=[B_perm], writes=[B_wov[0]])

    ov_slot = {17 + 2 * r: r for r in range(R_OV)}
    load_expert(0)
    prep_X(0, NS, 0, B_Xd, XTm[0], B_XT[0])
    for ex in range(NE):
        i = ex % 2
        if ex + 1 < NE:
            load_expert(ex + 1)
            prep_X((ex + 1) * CAP, NS, (ex + 1) % 2, B_Xd, XTm[(ex + 1) % 2], B_XT[(ex + 1) % 2])
        if ex + 1 in ov_slot:
            load_overflow(ov_slot[ex + 1])
        if ex >= 1:
            overflow_scatter(2)
        compute(wgS[i], wuS[i], wdS[i], [B_wg[i], B_wu[i], B_wd[i]], XTm[i], B_XT[i], CAP, ex * CAP, ex * NS)
        if ex == 0:
            emit_ranking()
        if ex in ov_slot:
            r = ov_slot[ex]
            prep_X(NMAIN + r * CAPO, NSO, 0, B_Xo, XTo, B_XTo)
            wv = wov[0]
            compute(wv[:, 0:8, :], wv[:, 8:16, :], wv[:, 16:24, :], [B_wov[0]], XTo, B_XTo, CAPO, NMAIN + r * CAPO, NE * NS + r * NSO)
    assert not ov_tiles
    moe_bufs = moe_bufs + B_wov + [B_XTo]

    B_fin = Buf("fin")
    B_fs = [Buf("fst0"), Buf("fst1")]
    NBUF = 4
    B_Y1, B_Y2, B_x1r = [Buf("Y1%d" % i) for i in range(NBUF)], [Buf("Y2%d" % i) for i in range(NBUF)], [Buf("x1r%d" % i) for i in range(NBUF)]
    for nb_ in [B_fin] + B_fs + B_Y1 + B_Y2 + B_x1r:
        Sched.alias(nb_, moe_bufs)
    for b in range(NB):
        S.dma("sp", lambda e, b=b: e.dma_start(out=g2bt[b], in_=modd[b:b + 1, 5120:6144].partition_broadcast(128)), reads=[b_modd], writes=[B_fin])
    S.dma("sp", lambda e: e.dma_start(out=ln2g, in_=ln2g_d.partition_broadcast(128)), writes=[B_fin])
    S.dma("sp", lambda e: e.dma_start(out=ln2b, in_=ln2b_d.partition_broadcast(128)), writes=[B_fin])
    B_zrow = Buf("zrow")
    S.op("dve", lambda e: e.memset(x1r[0][0:1, :], 0.0), writes=[B_x1r[0]])
    S.dma("sp", lambda e: e.dma_start(out=Yd[ZROW:ZROW + 1, :], in_=x1r[0][0:1, :]), reads=[B_x1r[0]], writes=[B_zrow])
    YdAll = B_Yd + [B_zrow]

    def fetch(gi):
        w = gi % NBUF
        S.dma("pool", lambda e: e.indirect_dma_start(out=Y1[w], out_offset=None, in_=Yd, in_offset=bass.IndirectOffsetOnAxis(ap=idxg_sb[:, gi, 0:1], axis=0),
                                                   bounds_check="BCREG2", oob_is_err=False), reads=YdAll + [B_idx[gi]], writes=[B_Y1[w]])
        S.dma("pool", lambda e: e.indirect_dma_start(out=Y2[w], out_offset=None, in_=Yd, in_offset=bass.IndirectOffsetOnAxis(ap=idxg_sb[:, gi, 1:2], axis=0),
                                                   bounds_check="BCREG2", oob_is_err=False), reads=YdAll + [B_idx[gi]], writes=[B_Y2[w]])
        S.dma("sp", lambda e: e.dma_start(out=x1r[w], in_=x1d[gi * 128:(gi + 1) * 128, :]), reads=[B_x1d[gi]], writes=[B_x1r[w]])

    def comb1(gi):
        b = gi // NT
        w = gi % NBUF
        f_ = fst[gi % 2]
        Bf = B_fs[gi % 2]
        S.op("act", lambda e: e.activation(out=Y1[w], in_=Y1[w], func=AF.Identity, scale=wts_sb[:, gi, 0:1]), reads=[B_Y1[w], B_wts[gi]], writes=[B_Y1[w]])
        S.op("dve", lambda e: e.scalar_tensor_tensor(Y1[w], Y2[w], wts_sb[:, gi, 1:2], Y1[w], ALU.mult, ALU.add), reads=[B_Y1[w], B_Y2[w], B_wts[gi]], writes=[B_Y1[w]])
        S.op("dve", lambda e: e.tensor_tensor(Y1[w], Y1[w], g2bt[b], ALU.mult), reads=[B_Y1[w], B_fin], writes=[B_Y1[w]])
        S.op("dve", lambda e: e.scalar_tensor_tensor(Y1[w], x1r[w], ALPHA, Y1[w], ALU.mult, ALU.add), reads=[B_Y1[w], B_x1r[w]], writes=[B_Y1[w]])
        for hf in range(2):
            S.op("dve", lambda e, hf=hf: e.bn_stats(f_[:, hf * 6:hf * 6 + 6], Y1[w][:, hf * 512:(hf + 1) * 512]), reads=[B_Y1[w]], writes=[Bf])
        S.op("dve", lambda e: e.bn_aggr(f_[:, 12:14], f_[:, 0:12].rearrange("p (a s) -> p a s", a=2)), reads=[Bf], writes=[Bf])
        S.op("act", lambda e: e.activation(out=f_[:, 14:15], in_=f_[:, 13:14], func=AF.Ln, bias=EPS), reads=[Bf], writes=[Bf])
        S.op("act", lambda e: e.activation(out=f_[:, 15:16], in_=f_[:, 14:15], func=AF.Exp, scale=-0.5), reads=[Bf], writes=[Bf])

    def comb2(gi):
        b = gi // NT
        n = gi % NT
        w = gi % NBUF
        f_ = fst[gi % 2]
        Bf = B_fs[gi % 2]
        S.op("dve", lambda e: e.tensor_scalar(f_[:, 16:17], f_[:, 12:13], f_[:, 15:16], -1.0, ALU.mult, ALU.mult), reads=[Bf], writes=[Bf])
        S.op("act", lambda e: e.activation(out=Y1[w], in_=Y1[w], func=AF.Identity, scale=f_[:, 15:16], bias=f_[:, 16:17]), reads=[B_Y1[w], Bf], writes=[B_Y1[w]])
        S.op("dve", lambda e: e.tensor_tensor(Y1[w], Y1[w], ln2g, ALU.mult), reads=[B_Y1[w], B_fin], writes=[B_Y1[w]])
        S.op("pool", lambda e: e.tensor_tensor(Y2[w], Y1[w], ln2b, ALU.add), reads=[B_Y1[w], B_fin], writes=[B_Y2[w]])
        S.dma("sp", lambda e: e.dma_start(out=out_d[b, n * 128:(n + 1) * 128, :], in_=Y2[w]), reads=[B_Y2[w]], writes=[B_out[gi]])

    NTOT = NB * NT
    for gi in range(min(3, NTOT)):
        fetch(gi)
    comb1(0)
    for gi in range(NTOT):
        if gi + 3 < NTOT:
            fetch(gi + 3)
        if gi + 1 < NTOT:
            comb1(gi + 1)
        comb2(gi)
    S.wait_all("sp", B_out + (B_dbg if debug else []))
    S.emit(nc, st)
    st.close()
    return nc


def _rope_tables():
    half = 16
    inv = (10000.0 ** (-np.arange(half, dtype=np.float32) / half)).astype(np.float32)
    tok = np.arange(L)
    rows = (tok // 64).astype(np.float32)
    cols = (tok % 64).astype(np.float32)
    cos = np.zeros((L, 64), np.float32)
    sin = np.zeros((L, 64), np.float32)
    for base, pos in ((0, rows), (32, cols)):
        ang = (pos[:, None] * inv[None, :]).astype(np.float32)
        c, s = np.cos(ang).astype(np.float32), np.sin(ang).astype(np.float32)
        cos[:, base:base + 16] = c
        cos[:, base + 16:base + 32] = c
        sin[:, base:base + 16] = -s
        sin[:, base + 16:base + 32] = s
    return cos, sin


def make_in_maps(inputs, ncores=NCORES):
    f = lambda a: np.ascontiguousarray(np.asarray(a, dtype=np.float32))
    cos, sin = _rope_tables()
    shared = {
        "w_mod": f(inputs["w_mod"][0]), "b_mod": f(inputs["b_mod"][0]).reshape(1, -1),
        "w_in": f(inputs["w_in"][0]), "w_out": f(inputs["w_out"][0]),
        "lamv": np.concatenate([f(inputs[k][0]) for k in ("lam_q1", "lam_k1", "lam_q2", "lam_k2")]).reshape(1, 256),
        "subln_g": f(inputs["subln_g"][0]).reshape(1, -1),
        "sg_ln_g": f(inputs["sg_ln_g"][0]).reshape(1, -1), "sg_ln_b": f(inputs["sg_ln_b"][0]).reshape(1, -1),
        "sg_wT": np.ascontiguousarray(f(inputs["sg_w"][0]).transpose(0, 2, 1)),
        "sg_b": f(inputs["sg_b"][0]).reshape(1, -1),
        "ln1_g": f(inputs["ln1_g"][0]).reshape(1, -1), "ln1_b": f(inputs["ln1_b"][0]).reshape(1, -1),
        "ln2_g": f(inputs["ln2_g"][0]).reshape(1, -1), "ln2_b": f(inputs["ln2_b"][0]).reshape(1, -1),
        "router_w": np.ascontiguousarray(np.concatenate([f(inputs["router_group_w"][0]), f(inputs["router_expert_w"][0])], axis=1)),
        "router_b": np.concatenate([f(inputs["router_group_b"][0]), f(inputs["router_expert_b"][0])]).reshape(1, 36),
        "exp_w": np.ascontiguousarray(np.stack([f(inputs["exp_w_gate"][0]), f(inputs["exp_w_up"][0]), f(inputs["exp_w_down"][0])], axis=1)),
        "ident": np.eye(128, dtype=np.float32),
        "ustrict": np.triu(np.ones((128, 128), np.float32), 1),
        "rope_cos": cos, "rope_sin": sin,
        "econst": np.concatenate([np.arange(NE, dtype=np.float32) * 1024.0, np.arange(NE, dtype=np.float32) * CAP,
                                  np.arange(NE, dtype=np.float32) / 64.0, np.arange(NE, dtype=np.float32),
                                  np.arange(NE, dtype=np.float32)]).reshape(1, 5 * NE),
    }
    x = f(inputs["x"]); c = f(inputs["c"]); ctx = f(inputs["ctx"]); cc = f(inputs["c_ctx"])
    maps = []
    for i in range(ncores):
        sl = slice(i * NB, (i + 1) * NB)
        cv = np.stack([c[i * NB], c[i * NB + 1], cc], axis=1)
        cT = np.ascontiguousarray(cv.reshape(8, 128, 3).transpose(1, 0, 2))
        m = dict(shared)
        m.update({"x": np.ascontiguousarray(x[sl]), "ctx": np.ascontiguousarray(ctx[sl]), "cT": cT})
        maps.append(m)
    return maps


_NC_CACHE = {}


def kernel(**inputs):
    if "nc" not in _NC_CACHE:
        _NC_CACHE["nc"] = build_nc()
    nc = _NC_CACHE["nc"]
    in_maps = make_in_maps(inputs)
    res = run_bass_kernel_spmd(nc, in_maps, core_ids=list(range(NCORES)))
    out = np.concatenate([np.asarray(r["out"]) for r in res.results], axis=0)
    return out.astype(np.float32)
```

```python
import math
from contextlib import ExitStack

import numpy as np
import concourse.bass as bass
import concourse.mybir as mybir
from concourse.bass_utils import run_bass_kernel_spmd

F32 = mybir.dt.float32
BF16 = mybir.dt.bfloat16
I32 = mybir.dt.int32
AF = mybir.ActivationFunctionType
ALU = mybir.AluOpType
AX = mybir.AxisListType

NCORES = 8
D = 1024
L = 2048
CTXL = 256
NKT = 18
NB = 2
NT = 16
NE = 32
CAP = 512
R_OV = 8
CAPO = 256
NMAIN = NE * CAP
ZROW = NMAIN + R_OV * CAPO
NSLOT = ZROW
ALPHA = 2.0 ** 0.25
LAM_INIT = 0.8 - 0.6 * math.exp(0.0)
EPS = 1e-5
BIG = 1.0e4

COMPUTE = ("pe", "act", "dve", "pool")
QUEUES = ("sp", "act", "pool")


class Buf:
    __slots__ = ("name", "lw", "rd")

    def __init__(self, name):
        self.name = name
        self.lw = None
        self.rd = []


class Op:
    __slots__ = ("eng", "fn", "deps", "signal", "sigval", "is_dma", "slot", "use", "cidx")


class _Rec:
    def __init__(self):
        self.call = None

    def __getattr__(self, name):
        def f(*a, **k):
            self.call = (name, a, k)
            return self
        return f


class Late:
    def __init__(self, builder):
        self.builder = builder


class Sched:
    def __init__(self, nslots=12):
        self.ops = {e: [] for e in ("pe", "act", "dve", "pool", "sp")}
        self.ccount = {e: 0 for e in self.ops}
        self.dcount = {e: 0 for e in self.ops}
        self.nslots = nslots
        self.slot_last = {}

    def _add(self, eng, fn, reads, writes, is_dma):
        op = Op()
        if fn is not None and not isinstance(fn, Late):
            rec = _Rec()
            fn(rec)
            fn = rec.call
        op.eng, op.fn, op.is_dma = eng, fn, is_dma
        op.signal, op.sigval, op.slot, op.use = False, None, None, None
        deps = []
        for b in reads:
            if b.lw is not None:
                deps.append(b.lw)
        for b in writes:
            if b.lw is not None:
                deps.append(b.lw)
            deps.extend(b.rd)
        op.cidx = self.ccount[eng]
        if is_dma:
            j = self.dcount[eng]
            self.dcount[eng] += 1
            op.slot = j % self.nslots
            op.use = j // self.nslots + 1
            prev = self.slot_last.get((eng, op.slot))
            if prev is not None:
                deps.append(prev)
            self.slot_last[(eng, op.slot)] = op
        else:
            self.ccount[eng] += 1
        seen = set()
        op.deps = []
        for d in deps:
            if d is op or id(d) in seen:
                continue
            seen.add(id(d))
            op.deps.append(d)
        for b in reads:
            b.rd.append(op)
        for b in writes:
            b.lw = op
            b.rd = []
        self.ops[eng].append(op)
        return op

    def op(self, eng, fn, reads=(), writes=()):
        return self._add(eng, fn, list(reads), list(writes), False)

    def dma(self, eng, fn, reads=(), writes=()):
        return self._add(eng, fn, list(reads), list(writes), True)

    def wait_all(self, eng, bufs):
        return self._add(eng, None, list(bufs), [], False)

    @staticmethod
    def alias(new, olds):
        for o in olds:
            if o.lw is not None:
                new.rd.append(o.lw)
            new.rd.extend(o.rd)

    @staticmethod
    def _needs_wait(op, d):
        if d.is_dma or d.eng != op.eng:
            return True
        if op.eng == "pe":
            return False
        if op.eng == "pool" or op.is_dma:
            return True
        return (op.cidx - d.cidx) < 2

    def emit(self, nc, stack):
        for lst in self.ops.values():
            for op in lst:
                for d in op.deps:
                    if not d.is_dma and self._needs_wait(op, d):
                        d.signal = True
        for lst in self.ops.values():
            c = 0
            for op in lst:
                if not op.is_dma and op.fn is not None and op.signal:
                    c += 1
                    op.sigval = c
        csem = {e: stack.enter_context(nc.semaphore("c_" + e)) for e in COMPUTE}
        dsem = {}
        for q in QUEUES:
            for s in range(min(self.nslots, self.dcount[q])):
                dsem[(q, s)] = stack.enter_context(nc.semaphore("d_%s%d" % (q, s)))
        sched = self

        def run(eng_obj, e):
            known = {}
            bc_reg = eng_obj.to_reg(NSLOT - 1) if e == "pool" else None
            bc_reg2 = eng_obj.to_reg(NSLOT) if e == "pool" else None
            for op in sched.ops[e]:
                for d in op.deps:
                    if not sched._needs_wait(op, d):
                        continue
                    if d.is_dma:
                        sem, val, key = dsem[(d.eng, d.slot)], 16 * d.use, ("d", d.eng, d.slot)
                    else:
                        sem, val, key = csem[d.eng], d.sigval, ("c", d.eng)
                    if known.get(key, 0) >= val:
                        continue
                    known[key] = val
                    eng_obj.wait_ge(sem, val)
                if op.fn is None:
                    continue
                if isinstance(op.fn, Late):
                    inst = op.fn.builder(eng_obj)
                    inst.then_inc(dsem[(e, op.slot)], 16)
                    continue
                try:
                    kw = op.fn[2]
                    if kw.get("bounds_check") == "BCREG":
                        kw = dict(kw)
                        kw["bounds_check"] = bc_reg
                    elif kw.get("bounds_check") == "BCREG2":
                        kw = dict(kw)
                        kw["bounds_check"] = bc_reg2
                    inst = getattr(eng_obj, op.fn[0])(*op.fn[1], **kw)
                except Exception:
                    ii = sched.ops[e].index(op)
                    print("PREV", [(o_.fn[0] if o_.fn else None) for o_ in sched.ops[e][max(0, ii - 12):ii]], ii, flush=True)
                    print("FAILED OP", e, op.fn[0], [getattr(a, "shape", a) for a in op.fn[1]],
                          {k: getattr(v, "shape", v) for k, v in op.fn[2].items()}, flush=True)
                    raise
                if op.is_dma:
                    inst.then_inc(dsem[(e, op.slot)], 16)
                elif op.signal:
                    inst.then_inc(csem[e], 1)

        with nc.Block() as block:
            @block.sync
            def _(eng):
                run(eng, "sp")

            @block.tensor
            def _(eng):
                run(eng, "pe")

            @block.scalar
            def _(eng):
                run(eng, "act")

            @block.vector
            def _(eng):
                run(eng, "dve")

            @block.gpsimd
            def _(eng):
                run(eng, "pool")


def build_nc(debug=False):
    nc = bass.Bass("TRN2", target_bir_lowering=False)

    def din(name, shape, dt=F32):
        return nc.dram_tensor(name, list(shape), dt, kind="ExternalInput").ap()

    x_d = din("x", [NB, L, D])
    ctx_d = din("ctx", [NB, CTXL, D])
    cT_d = din("cT", [128, 8, 3])
    wmod_d = din("w_mod", [D, 6 * D])
    bmod_d = din("b_mod", [1, 6 * D])
    win_d = din("w_in", [D, 2560])
    wout_d = din("w_out", [D, D])
    lamv_d = din("lamv", [1, 256])
    subg_d = din("subln_g", [1, 128])
    sglg_d = din("sg_ln_g", [1, 512])
    sglb_d = din("sg_ln_b", [1, 512])
    sgwT_d = din("sg_wT", [4, 128, 128])
    sgb_d = din("sg_b", [1, 512])
    ln1g_d = din("ln1_g", [1, D])
    ln1b_d = din("ln1_b", [1, D])
    ln2g_d = din("ln2_g", [1, D])
    ln2b_d = din("ln2_b", [1, D])
    rw_d = din("router_w", [D, 36])
    rb_d = din("router_b", [1, 36])
    wall_d = din("exp_w", [NE, 3, D, D])
    ident_d = din("ident", [128, 128])
    ustr_d = din("ustrict", [128, 128])
    ropec_d = din("rope_cos", [L, 64])
    ropes_d = din("rope_sin", [L, 64])
    econst_d = din("econst", [1, 5 * NE])
    out_d = nc.dram_tensor("out", [NB, L, D], F32, kind="ExternalOutput").ap()
    dbg_d = nc.dram_tensor("dbg", [NB * L, D], F32, kind="ExternalOutput").ap() if debug else None

    modd = nc.dram_tensor("modd", [3, 6 * D], F32, kind="Internal").ap()
    x1d = nc.dram_tensor("x1d", [NB * L, D], F32, kind="Internal").ap()
    tbd = nc.dram_tensor("tbd", [NB * L, D], BF16, kind="Internal").ap()
    Xd = nc.dram_tensor("Xd", [NSLOT, D], BF16, kind="Internal").ap()
    Yd = nc.dram_tensor("Yd", [NSLOT + 1, D], F32, kind="Internal").ap()
    b_modd = Buf("modd")
    B_x1d = [Buf("x1d%d" % i) for i in range(NB * NT)]
    B_Xd = [Buf("Xd%d" % i) for i in range(NB * NT * 2)]
    B_Yd = [Buf("Yd%d" % i) for i in range(NE * (CAP // 128) + R_OV * (CAPO // 128))]
    B_Xo = [Buf("Xo%d" % i) for i in range(NB * NT * 2)]
    B_vs = [Buf("vs%d" % i) for i in range(NB * NT)]
    B_perm, B_io = Buf("perm"), Buf("idxo")
    B_out = [Buf("out%d" % i) for i in range(NB * NT)]
    B_dbg = [Buf("dbg%d" % i) for i in range(NB * NT)]

    S = Sched()
    st = ExitStack()
    st.enter_context(nc.allow_low_precision("bf16 matmul operands, fp32 accumulation"))
    st.enter_context(nc.allow_non_contiguous_dma(reason="tiny setup gathers"))

    def sb(name, shape, dt):
        return st.enter_context(nc.sbuf_tensor(name, list(shape), dt))

    ident_f = sb("ident_f", [128, 128], F32)
    ident_b = sb("ident_b", [128, 128], BF16)
    ones_b = sb("ones_b", [128, 128], BF16)
    ustr_b = sb("ustr_b", [128, 128], BF16)
    rw_sb = sb("rw_sb", [128, 8, 36], F32)
    rb_b = sb("rb_b", [128, 36], F32)
    econst = sb("econst_sb", [128, 5, NE], F32)
    pslot = sb("pslot", [128, NB * NT, 2], F32)
    eslot = sb("eslot", [128, NB * NT, 2], F32)
    idxo_sb = sb("idxo_sb", [128, NB * NT, 2], I32)
    perm_i = sb("perm_i", [128, R_OV], I32)
    gvec = sb("gvec", [128, 128], F32)
    lamt = sb("lamt", [128, 256], F32)
    lams = sb("lams", [128, 8], F32)
    cT_sb = sb("cT_sb", [128, 8, 3], F32)
    silT = sb("silT", [128, 8, 3], BF16)
    modT = sb("modT", [128, 16, 3], F32)
    Mb = sb("Mb", [128, NB * NT, NE], BF16)
    idx_sb = sb("idx_sb", [128, NB * NT, 2], I32)
    wts_sb = sb("wts_sb", [128, NB * NT, 2], F32)
    idxg_sb = sb("idxg_sb", [128, NB * NT, 2], I32)
    B_const = Buf("const")
    B_lam = Buf("lam")
    B_modT = Buf("modT")
    B_Mb = [Buf("Mb%d" % i) for i in range(NB * NT)]
    B_idx = [Buf("idx%d" % i) for i in range(NB * NT)]
    B_wts = [Buf("wts%d" % i) for i in range(NB * NT)]

    P = [st.enter_context(nc.psum_tensor("P%d" % i, [128, 512], F32)) for i in range(8)]
    PB = [Buf("P%d" % i) for i in range(8)]

    ARENA_B = 199 * 1024
    arena = sb("arena", [128, ARENA_B // 2], BF16)

    def carve(off, nbytes, dt, pattern=None, **kw):
        assert off % 4 == 0 and off + nbytes <= ARENA_B, (off, nbytes)
        v = arena[:, off // 2:(off + nbytes) // 2]
        if dt == F32:
            v = v.bitcast(F32)
        elif dt == I32:
            v = v.bitcast(I32)
        if pattern:
            v = v.rearrange(pattern, **kw)
        return v

    o = 0
    wi = carve(o, 40960, BF16, "p (k n) -> p k n", k=8); o += 40960
    ropec = carve(o, 4096, F32, "p (n d) -> p n d", n=16); o += 4096
    ropes = carve(o, 4096, F32, "p (n d) -> p n d", n=16); o += 4096
    sglg = carve(o, 2048, F32); o += 2048
    sglb = carve(o, 2048, F32); o += 2048
    sgwT = carve(o, 1024, BF16, "p (g q) -> p g q", g=4); o += 1024
    sgb = carve(o, 1024, BF16); o += 1024
    ln1g = carve(o, 4096, F32); o += 4096
    ln1b = carve(o, 4096, F32); o += 4096
    g1b = carve(o, 4096, F32); o += 4096
    opsc2 = carve(o, 4096, F32); o += 4096
    sh2b = carve(o, 4096, F32); o += 4096
    hcT = carve(o, 4096, BF16, "p (k n) -> p k n", k=8)
    qtm2, ktm2, vln2 = carve(o, 1024, BF16), carve(o + 1024, 1024, BF16), carve(o + 2048, 1024, BF16)
    o += 4096
    mixT = carve(o, 32768, BF16, "p (k n) -> p k n", k=8); o += 32768
    O_ATT = o
    qT = carve(o, 16384, BF16, "p (k n) -> p k n", k=4); o += 16384
    kT = carve(o, 18432, BF16, "p (k n) -> p k n", k=4); o += 18432
    Vaug = carve(o, 18720, BF16, "p (t h e) -> p t h e", t=NKT, h=4); o += 18944
    O_A = o
    xl = [carve(o + i * 4096, 4096, F32) for i in range(2)]; o += 8192
    hTg = carve(o, 8192, BF16, "p (k n) -> p k n", k=8); o += 8192
    uT = carve(o, 4096, BF16, "p (k n) -> p k n", k=4); o += 4096
    gv = carve(o, 8192, F32, "p (j n) -> p j n", j=4); o += 8192
    r1 = carve(o, 2048, F32); o += 2048
    r2 = carve(o, 2048, F32); o += 2048
    qtm = carve(o, 1024, BF16); o += 1024
    ktm = carve(o, 1024, BF16); o += 1024
    vln = carve(o, 1024, BF16); o += 1024
    st6 = carve(o, 192, F32, "p (j s) -> p j s", j=8); o += 192
    mvs = carve(o, 64, F32, "p (j s) -> p j s", j=8); o += 64
    rst = carve(o, 64, F32); o += 64
    MIX_END = o
    assert MIX_END <= ARENA_B, MIX_END
    o = O_A
    PT = [carve(o + i * 1024, 1024, BF16) for i in range(4)]; o += 4096
    osb = [carve(o + i * 4224, 4128, F32, "p (m q e) -> p m q e", m=2, q=4) for i in range(2)]; o += 8448
    obuf = carve(o, 2048, F32, "p (q e) -> p q e", q=4); o += 2048
    sqb = carve(o, 2048, F32, "p (q e) -> p q e", q=4); o += 2048
    datm = carve(o, 1024, BF16, "p (q e) -> p q e", q=4); o += 1024
    rsb = carve(o, 64, F32); o += 64
    lnt = carve(o, 64, F32); o += 64
    assert o <= MIX_END
    ATT_TMP = 17792
    wout = carve(O_A + ATT_TMP, 16384, BF16, "p (k n) -> p k n", k=8)
    assert O_A + ATT_TMP + 16384 <= MIX_END
    RG = 8
    rtb = carve(O_A, 2240 * 4, F32)
    assert 2240 * 4 <= ATT_TMP
    o = O_ATT
    xr = [carve(o + i * 4096, 4096, F32) for i in range(2)]; o += 8192
    zt = [carve(o + i * 4096, 4096, F32) for i in range(3)]; o += 12288
    x1t = [carve(o + i * 4096, 4096, F32) for i in range(2)]; o += 8192
    tt = [carve(o + i * 4096, 4096, F32) for i in range(2)]; o += 8192
    tb = [carve(o + i * 2048, 2048, BF16) for i in range(2)]; o += 4096
    tT = carve(o, 4096, F32, "p (k n) -> p k n", k=8); o += 4096
    lgall = carve(o, NT * 36 * 4, F32, "p (n c) -> p n c", n=NT); o += NT * 36 * 4
    st6d = [carve(o + i * 64, 48, F32, "p (j s) -> p j s", j=2) for i in range(3)]; o += 192
    mvd = [carve(o + i * 64, 64, F32) for i in range(3)]; o += 192
    assert o <= O_A, o
    tbl = [mixT[:, c_, 0:1024] for c_ in range(4)]
    o = 0
    wgS = [carve(o + i * 16384, 16384, BF16, "p (k n) -> p k n", k=8) for i in range(2)]; o += 32768
    wuS = [carve(o + i * 16384, 16384, BF16, "p (k n) -> p k n", k=8) for i in range(2)]; o += 32768
    wdS = [carve(o + i * 16384, 16384, BF16, "p (k n) -> p k n", k=8) for i in range(2)]; o += 32768
    O_WOV = o
    wov = [carve(o, 49152, BF16, "p (k n) -> p k n", k=24)]; o += 49152
    XTm = [carve(o + i * 8192, 8192, BF16, "p (k n) -> p k n", k=8) for i in range(2)]; o += 16384
    XTo = carve(o, 4096, BF16, "p (k n) -> p k n", k=8); o += 4096
    hidT = carve(o, 8192, BF16, "p (k n) -> p k n", k=8); o += 8192
    xg = [carve(o + i * 2048, 2048, BF16) for i in range(4)]; o += 8192
    sgt = [carve(o + i * 2048, 2048, F32) for i in range(2)]; o += 4096
    ysb = [carve(o + i * 4096, 4096, F32) for i in range(2)]; o += 8192
    tbo = [carve(o + i * 2048, 2048, BF16) for i in range(2)]; o += 4096
    assert o <= ARENA_B, o
    o = 0
    g2bt = [carve(o + i * 4096, 4096, F32) for i in range(2)]; o += 8192
    ln2g = carve(o, 4096, F32); o += 4096
    ln2b = carve(o, 4096, F32); o += 4096
    Y1 = [carve(o + i * 4096, 4096, F32) for i in range(4)]; o += 16384
    Y2 = [carve(o + i * 4096, 4096, F32) for i in range(4)]; o += 16384
    x1r = [carve(o + i * 4096, 4096, F32) for i in range(4)]; o += 16384
    fst = [carve(o + i * 128, 128, F32) for i in range(2)]; o += 256
    assert o <= 98304, o

    B_wi, B_rope, B_sgc, B_ln1, B_bc, B_hcT = Buf("wi"), Buf("rope"), Buf("sgc"), Buf("ln1"), Buf("bc"), Buf("hcT")
    S.dma("sp", lambda e: e.dma_start(out=ident_f[:], in_=ident_d), writes=[B_const])
    S.dma("sp", lambda e: e.dma_start(out=cT_sb[:], in_=cT_d), writes=[B_const])
    S.dma("sp", lambda e: e.dma_start(out=rw_sb[:], in_=rw_d.rearrange("(k p) n -> p k n", p=128)), writes=[B_const])
    S.dma("sp", lambda e: e.dma_start(out=rb_b[:], in_=rb_d.partition_broadcast(128)), writes=[B_const])
    S.dma("sp", lambda e: e.dma_start(out=econst[:].rearrange("p a e -> p (a e)"), in_=econst_d.partition_broadcast(128)), writes=[B_const])
    S.dma("sp", lambda e: e.dma_start(out=gvec[:], in_=subg_d.partition_broadcast(128)), writes=[B_const])
    S.dma("sp", lambda e: e.dma_start(out=lamt[:], in_=lamv_d.partition_broadcast(128)), writes=[B_const])
    S.dma("pool", lambda e: e.dma_start(out=ustr_b[:], in_=ustr_d), writes=[B_const])
    S.op("dve", lambda e: e.tensor_copy(ident_b[:], ident_f[:]), reads=[B_const], writes=[B_const])
    S.op("dve", lambda e: e.memset(ones_b[:], 1.0), writes=[B_const])
    S.op("dve", lambda e: e.tensor_scalar_mul(gvec[:], gvec[:], 1.0 - LAM_INIT), reads=[B_const], writes=[B_const])
    S.op("dve", lambda e: e.tensor_tensor(lamt[:, 0:64], lamt[:, 0:64], lamt[:, 64:128], ALU.mult), reads=[B_const], writes=[B_lam])
    S.op("dve", lambda e: e.tensor_tensor(lamt[:, 128:192], lamt[:, 128:192], lamt[:, 192:256], ALU.mult), reads=[B_const, B_lam], writes=[B_lam])
    S.op("dve", lambda e: e.memset(lams[:], 0.0), writes=[B_lam])
    S.op("dve", lambda e: e.reduce_sum(lams[:, 0:1], lamt[:, 0:64], axis=AX.X), reads=[B_lam], writes=[B_lam])
    S.op("dve", lambda e: e.reduce_sum(lams[:, 1:2], lamt[:, 128:192], axis=AX.X), reads=[B_lam], writes=[B_lam])
    S.op("act", lambda e: e.activation(out=lams[:, 2:4], in_=lams[:, 0:2], func=AF.Exp), reads=[B_lam], writes=[B_lam])
    S.op("dve", lambda e: e.tensor_tensor(lams[:, 4:5], lams[:, 2:3], lams[:, 3:4], ALU.subtract), reads=[B_lam], writes=[B_lam])
    S.op("dve", lambda e: e.tensor_scalar(lams[:, 5:6], lams[:, 4:5], LAM_INIT, -1.0, ALU.add, ALU.mult), reads=[B_lam], writes=[B_lam])
    for hh in range(4):
        S.dma("pool", lambda e, hh=hh: e.dma_start(out=wi[:, :, hh * 640:(hh + 1) * 640], in_=win_d[:, hh * 640:(hh + 1) * 640].rearrange("(k p) n -> p k n", p=128)), writes=[B_wi])
    S.dma("sp", lambda e: e.dma_start(out=ropec, in_=ropec_d.rearrange("(n p) d -> p n d", p=128)), writes=[B_rope])
    S.dma("sp", lambda e: e.dma_start(out=ropes, in_=ropes_d.rearrange("(n p) d -> p n d", p=128)), writes=[B_rope])
    S.dma("sp", lambda e: e.dma_start(out=sglg, in_=sglg_d.partition_broadcast(128)), writes=[B_sgc])
    S.dma("sp", lambda e: e.dma_start(out=sglb, in_=sglb_d.partition_broadcast(128)), writes=[B_sgc])
    S.dma("pool", lambda e: e.dma_start(out=sgwT, in_=sgwT_d.rearrange("g q p -> q g p")), writes=[B_sgc])
    S.dma("pool", lambda e: e.dma_start(out=sgb[0:1, :], in_=sgb_d), writes=[B_sgc])
    S.dma("sp", lambda e: e.dma_start(out=ln1g, in_=ln1g_d.partition_broadcast(128)), writes=[B_ln1])
    S.dma("sp", lambda e: e.dma_start(out=ln1b, in_=ln1b_d.partition_broadcast(128)), writes=[B_ln1])

    S.op("act", lambda e: e.activation(out=silT[:], in_=cT_sb[:], func=AF.Silu), reads=[B_const], writes=[B_const])
    wm = [xl[0].bitcast(BF16).rearrange("p (k n) -> p k n", k=8)[:, :, 0:256],
          xl[1].bitcast(BF16).rearrange("p (k n) -> p k n", k=8)[:, :, 0:256]]
    B_wm = [Buf("wm0"), Buf("wm1")]
    bm3 = [gv[0:3, 0, 0:256], gv[0:3, 1, 0:256]]
    mrow = [gv[0:3, 2, 0:256], gv[0:3, 3, 0:256]]
    B_bm3 = [Buf("bm0"), Buf("bm1")]
    B_mrow = [Buf("mr0"), Buf("mr1")]
    def mod_dma(nb, wmv, bmv, Bw, Bb):
        cs = slice(nb * 256, (nb + 1) * 256)
        S.dma("pool", lambda e: e.dma_start(out=wmv, in_=wmod_d[:, cs].rearrange("(k p) n -> p k n", p=128)), writes=[Bw])
        S.dma("sp", lambda e: e.dma_start(out=bmv, in_=bmod_d[:, cs].partition_broadcast(3)), writes=[Bb])

    def mod_compute(nb, wmv, bmv, mrv, Bw, Bb, Bm, bank):
        cs = slice(nb * 256, (nb + 1) * 256)
        for k in range(8):
            S.op("pe", lambda e, k=k: e.matmul(P[bank][0:3, 0:256], lhsT=silT[:, k, :], rhs=wmv[:, k, :], start=(k == 0), stop=(k == 7)),
                 reads=[B_const, Bw], writes=[PB[bank]])
        S.op("dve", lambda e: e.tensor_tensor(mrv, P[bank][0:3, 0:256], bmv, ALU.add), reads=[PB[bank], Bb], writes=[Bm])
        S.dma("sp", lambda e: e.dma_start(out=modd[:, cs], in_=mrv), reads=[Bm], writes=[b_modd])

    def mod_block(nb, wmv, bmv, mrv, Bw, Bb, Bm, bank):
        mod_dma(nb, wmv, bmv, Bw, Bb)
        mod_compute(nb, wmv, bmv, mrv, Bw, Bb, Bm, bank)

    for nb in range(8):
        i = nb % 2
        mod_block(nb, wm[i], bm3[i], mrow[i], B_wm[i], B_bm3[i], B_mrow[i], i)
    B_xl = [Buf("xl%d" % i) for i in range(2)]
    for i in range(2):
        Sched.alias(B_xl[i], [B_wm[i]])
    B_hTg, B_uT, B_gv = Buf("hTg"), Buf("uT"), Buf("gv")
    Sched.alias(B_gv, B_bm3 + B_mrow)
    gvflat = gv.rearrange("p j n -> p (j n)")
    S.dma("sp", lambda e: e.dma_start(out=gvflat[0:3, :], in_=modd[:, 0:2048]), reads=[b_modd], writes=[B_gv])
    for j in range(16):
        S.op("pe", lambda e, j=j: e.transpose(P[0][:, j * 3:(j + 1) * 3], gvflat[0:3, j * 128:(j + 1) * 128], ident_f[0:3, 0:3]), reads=[B_gv, B_const], writes=[PB[0]])
    S.op("dve", lambda e: e.tensor_copy(modT[:], P[0][:, 0:48].rearrange("p (j v) -> p j v", j=16)), reads=[PB[0]], writes=[B_modT])
    S.op("dve", lambda e: e.tensor_scalar_add(modT[:, 8:16, :], modT[:, 8:16, :], 1.0), reads=[B_modT], writes=[B_modT])
    B_r1, B_r2, B_qtm, B_ktm, B_vln, B_st = Buf("r1"), Buf("r2"), Buf("qtm"), Buf("ktm"), Buf("vln"), Buf("st")
    B_mixT = [Buf("mixT%d" % i) for i in range(NT)]
    B_qT = [Buf("qT%d" % i) for i in range(NT)]
    B_kT = [Buf("kT%d" % i) for i in range(NKT)]
    B_V = [Buf("V%d" % i) for i in range(NKT)]
    B_qtm2 = [B_qtm, Buf("qtm2")]
    B_ktm2 = [B_ktm, Buf("ktm2")]
    B_vln2 = [B_vln, Buf("vln2")]
    B_regA = [B_xl[0], B_xl[1], B_hTg, B_uT, B_gv, B_r1, B_r2, B_qtm, B_ktm, B_vln, B_st]
    xl_ctr = [0]
    pb_ctr = [0]

    def next_bank(lo=2, n=6):
        i = lo + pb_ctr[0] % n
        pb_ctr[0] += 1
        return i

    P_bf = [P[i][:].bitcast(BF16) for i in range(8)]
    tile_ctr = [0]

    B_tbd_all = []
    B_wg, B_wu, B_wd = [Buf("wg0"), Buf("wg1")], [Buf("wu0"), Buf("wu1")], [Buf("wd0"), Buf("wd1")]
    early = set()
    for b in range(NB):
        def vcol(v):
            return (lambda k: modT[:, 8 + k, v:v + 1]), (lambda k: modT[:, k, v:v + 1])

        S.op("pool", lambda e: e.memset(Vaug[:, :, :, 128:130], 1.0), reads=[], writes=B_V)

        def proj_block(lhs_fn, lhs_bufs, cols, bank):
            for k in range(8):
                S.op("pe", lambda e, k=k: e.matmul(P[bank][:, :], lhsT=lhs_fn(k), rhs=wi[:, k, cols], start=(k == 0), stop=(k == 7)),
                     reads=lhs_bufs + [B_wi], writes=[PB[bank]])

        def rope_block(bank, n, dst, dstbuf):
            pv = P[bank][:, :]
            cosb = ropec[:, n, :].unsqueeze(1).to_broadcast([128, 8, 64])
            S.op("dve", lambda e: e.tensor_tensor(r1.rearrange("p (a d) -> p a d", a=8), pv.rearrange("p (a d) -> p a d", a=8), cosb, ALU.mult),
                 reads=[PB[bank], B_rope], writes=[B_r1])
            p4 = pv.rearrange("p (a x h) -> p a x h", a=8, x=2)
            r24 = r2.rearrange("p (a x h) -> p a x h", a=8, x=2)
            s4 = ropes[:, n, :].rearrange("p (x h) -> p x h", x=2).unsqueeze(1).to_broadcast([128, 8, 2, 32])
            S.op("dve", lambda e: e.tensor_tensor(r24[:, :, :, 0:16], p4[:, :, :, 16:32], s4[:, :, :, 0:16], ALU.mult),
                 reads=[PB[bank], B_rope], writes=[B_r2])
            S.op("dve", lambda e: e.tensor_tensor(r24[:, :, :, 16:32], p4[:, :, :, 0:16], s4[:, :, :, 16:32], ALU.mult),
                 reads=[PB[bank], B_rope], writes=[B_r2])
            S.op("dve", lambda e: e.tensor_tensor(dst, r1, r2, ALU.add), reads=[B_r1, B_r2], writes=[dstbuf])

        def to_featmajor(src, srcbuf, dstT, col0, dstbuf, scale):
            tb_ = next_bank()
            for c in range(4):
                S.op("pe", lambda e, c=c: e.transpose(P_bf[tb_][:, c * 128:(c + 1) * 128], src[:, c * 128:(c + 1) * 128], ident_b[:]),
                     reads=[srcbuf, B_const], writes=[PB[tb_]])
            S.op("act", lambda e: e.activation(out=dstT[:, :, col0:col0 + 128], in_=P_bf[tb_][:, 0:512].rearrange("p (c n) -> p c n", c=4),
                                               func=AF.Copy, scale=scale), reads=[PB[tb_]], writes=[dstbuf])

        Sched.alias(B_hcT, [B_qtm2[1], B_ktm2[1], B_vln2[1]])
        scf, shf = vcol(2)
        for j in range(2):
            xi = xl_ctr[0] % 2; xl_ctr[0] += 1
            S.dma("sp", lambda e, j=j, xi=xi: e.dma_start(out=xl[xi], in_=ctx_d[b, j * 128:(j + 1) * 128, :]), writes=[B_xl[xi]])
            for k in range(8):
                bank = k % 2
                S.op("pe", lambda e, k=k, xi=xi, bank=bank: e.transpose(P[bank][:, 0:128], xl[xi][:, k * 128:(k + 1) * 128], ident_f[:]),
                     reads=[B_xl[xi], B_const], writes=[PB[bank]])
                S.op("act", lambda e, k=k, j=j, bank=bank: e.activation(out=hcT[:, k, j * 128:(j + 1) * 128], in_=P[bank][:, 0:128], func=AF.Identity,
                                                                         scale=scf(k), bias=shf(k)), reads=[PB[bank], B_modT], writes=[B_hcT])
        for j in range(2):
            bank = next_bank()
            proj_block(lambda k, j=j: hcT[:, k, j * 128:(j + 1) * 128], [B_hcT], slice(512, 1024), bank)
            S.op("act", lambda e, bank=bank: e.activation(out=ktm, in_=P[bank][:, :], func=AF.Copy), reads=[PB[bank]], writes=[B_ktm])
            to_featmajor(ktm, B_ktm, kT, L + j * 128, B_kT[16 + j], 1.0)
            bank = next_bank()
            proj_block(lambda k, j=j: hcT[:, k, j * 128:(j + 1) * 128], [B_hcT], slice(1024, 1536), bank)
            S.op("act", lambda e, bank=bank, j=j: e.activation(out=Vaug[:, 16 + j, :, 0:128], in_=P[bank][:, :].rearrange("p (h e) -> p h e", h=4), func=AF.Copy),
                 reads=[PB[bank]], writes=[B_V[16 + j]])

        if b == 0:
            wm_l = [mixT[:, i, :].rearrange("p (k n) -> p k n", k=8) for i in range(3)]
            sm_l = [mixT[0:3, 3, i * 512:(i + 1) * 512].bitcast(F32) for i in range(3)]
            B_wml = [Buf("wml%d" % i) for i in range(3)]
            B_sml = [Buf("sml%d" % i) for i in range(3)]
            late = list(range(8, 24))
            late_dma = list(range(8, 24))

            def late_dma_next():
                if late_dma:
                    nb2 = late_dma.pop(0)
                    i2 = nb2 % 3
                    mod_dma(nb2, wm_l[i2], sm_l[i2], B_wml[i2], B_sml[i2])
            late_dma_next()
            late_dma_next()
        for nb_ in (B_qtm2[1], B_ktm2[1], B_vln2[1]):
            Sched.alias(nb_, [B_hcT])
        scf, shf = vcol(b)
        for g in range(4):
            xis = []
            for j in range(4):
                n = g * 4 + j
                xi = xl_ctr[0] % 2; xl_ctr[0] += 1
                xis.append(xi)
                S.dma("sp", lambda e, n=n, xi=xi: e.dma_start(out=xl[xi], in_=x_d[b, n * 128:(n + 1) * 128, :]), writes=[B_xl[xi]])
                for k in range(8):
                    bank = k // 4
                    S.op("pe", lambda e, k=k, xi=xi, bank=bank: e.transpose(P[bank][:, (k % 4) * 128:(k % 4 + 1) * 128], xl[xi][:, k * 128:(k + 1) * 128], ident_f[:]),
                         reads=[B_xl[xi], B_const], writes=[PB[bank]])
                    if k % 4 == 3:
                        for kk in range(k - 3, k + 1):
                            S.op("act", lambda e, kk=kk, j=j, bank=bank: e.activation(out=hTg[:, kk, j * 128:(j + 1) * 128], in_=P[bank][:, (kk % 4) * 128:(kk % 4 + 1) * 128],
                                                                                      func=AF.Identity, scale=scf(kk), bias=shf(kk)),
                                 reads=[PB[bank], B_modT], writes=[B_hTg])
            for c in range(4):
                bank = next_bank()
                for k in range(8):
                    S.op("pe", lambda e, k=k, c=c, bank=bank: e.matmul(P[bank][:, :], lhsT=wi[:, k, 1536 + c * 128:1536 + (c + 1) * 128], rhs=hTg[:, k, :],
                                                                       start=(k == 0), stop=(k == 7)), reads=[B_wi, B_hTg], writes=[PB[bank]])
                S.op("act", lambda e, c=c, bank=bank: e.activation(out=uT[:, c, :], in_=P[bank][:, :], func=AF.Gelu), reads=[PB[bank]], writes=[B_uT])
            for j in range(4):
                bank = next_bank()
                proj_block(lambda k, j=j: hTg[:, k, j * 128:(j + 1) * 128], [B_hTg], slice(2048, 2560), bank)
                S.op("act", lambda e, j=j, bank=bank: e.activation(out=gv[:, j, :], in_=P[bank][:, :], func=AF.Gelu), reads=[PB[bank]], writes=[B_gv])
                S.op("dve", lambda e, j=j: e.bn_stats(st6[:, j, :], gv[:, j, :]), reads=[B_gv], writes=[B_st])
                S.op("dve", lambda e, j=j: e.bn_aggr(mvs[:, j, :], st6[:, j, :]), reads=[B_st], writes=[B_st])
            S.op("act", lambda e: e.activation(out=rst[:, 0:4], in_=mvs[:, 0:4, 1], func=AF.Ln, bias=EPS), reads=[B_st], writes=[B_st])
            S.op("act", lambda e: e.activation(out=rst[:, 4:8], in_=rst[:, 0:4], func=AF.Exp, scale=-0.5), reads=[B_st], writes=[B_st])
            qtms, ktms, vlns = [qtm, qtm2], [ktm, ktm2], [vln, vln2]

            def stage1(j):
                n = g * 4 + j
                d = n % 2
                lhs = lambda k: hTg[:, k, j * 128:(j + 1) * 128]
                bq = next_bank()
                proj_block(lhs, [B_hTg], slice(0, 512), bq)
                bk = next_bank()
                proj_block(lhs, [B_hTg], slice(512, 1024), bk)
                bv = next_bank()
                proj_block(lhs, [B_hTg], slice(1024, 1536), bv)
                rope_block(bq, n, qtms[d], B_qtm2[d])
                rope_block(bk, n, ktms[d], B_ktm2[d])
                S.op("act", lambda e: e.activation(out=Vaug[:, n, :, 0:128], in_=P[bv][:, :].rearrange("p (h e) -> p h e", h=4), func=AF.Copy),
                     reads=[PB[bv]], writes=[B_V[n]])
                S.op("dve", lambda e: e.tensor_scalar(gv[:, j, :], gv[:, j, :], mvs[:, j, 0:1], rst[:, 4 + j:5 + j], ALU.subtract, ALU.mult),
                     reads=[B_gv, B_st], writes=[B_gv])
                S.op("pool", lambda e: e.tensor_tensor(gv[:, j, :], gv[:, j, :], sglg, ALU.mult), reads=[B_gv, B_sgc], writes=[B_gv])
                S.op("pool", lambda e: e.tensor_tensor(vlns[d], gv[:, j, :], sglb, ALU.add), reads=[B_gv, B_sgc], writes=[B_vln2[d]])

            def stage2(j):
                n = g * 4 + j
                d = n % 2
                to_featmajor(qtms[d], B_qtm2[d], qT, n * 128, B_qT[n], 0.125)
                to_featmajor(ktms[d], B_ktm2[d], kT, n * 128, B_kT[n], 1.0)
                bank = next_bank()
                for gg in range(4):
                    S.op("pe", lambda e, gg=gg: e.matmul(P[bank][:, gg * 128:(gg + 1) * 128], lhsT=vlns[d][:, gg * 128:(gg + 1) * 128], rhs=sgwT[:, gg, :],
                                                         start=True, stop=False), reads=[B_vln2[d], B_sgc], writes=[PB[bank]])
                    S.op("pe", lambda e, gg=gg: e.matmul(P[bank][:, gg * 128:(gg + 1) * 128], lhsT=ones_b[0:1, :], rhs=sgb[0:1, gg * 128:(gg + 1) * 128],
                                                         start=False, stop=True), reads=[B_const, B_sgc], writes=[PB[bank]])
                S.op("dve", lambda e: e.tensor_tensor(mixT[:, 4:8, n * 128:(n + 1) * 128], P[bank][:, :].rearrange("p (g q) -> p g q", g=4),
                                                      uT[:, :, j * 128:(j + 1) * 128], ALU.mult),
                     reads=[PB[bank], B_uT], writes=[B_mixT[n]])

            for j in range(4):
                stage1(j)
                if j > 0:
                    stage2(j - 1)
                if b == 0 and late:
                    nb = late.pop(0)
                    i = nb % 3
                    mod_compute(nb, wm_l[i], sm_l[i], sm_l[i], B_wml[i], B_sml[i], B_sml[i], next_bank())
                    late_dma_next()
            stage2(3)
            if b == 0 and g == 3:
                for mb_ in B_mixT:
                    Sched.alias(mb_, B_wml + B_sml)

        B_PT = [Buf("PT%d" % i) for i in range(4)]
        B_osb = [Buf("osb0"), Buf("osb1")]
        B_ob, B_sq, B_da, B_rs = Buf("obuf"), Buf("sqb"), Buf("datm"), Buf("rsb")
        for nb_ in B_PT + B_osb + [B_ob, B_sq, B_da, B_rs]:
            Sched.alias(nb_, B_regA)
        tmpA = carve(O_A + 4096 + 8448, 4096, F32)
        B_tmpA = Buf("tmpA")
        Sched.alias(B_tmpA, B_regA)
        S.dma("sp", lambda e, b=b: e.dma_start(out=g1b, in_=modd[b:b + 1, 2048:3072].partition_broadcast(128)), reads=[b_modd], writes=[B_bc])
        S.dma("sp", lambda e, b=b: e.dma_start(out=sh2b, in_=modd[b:b + 1, 3072:4096].partition_broadcast(128)), reads=[b_modd], writes=[B_bc])
        S.dma("sp", lambda e, b=b: e.dma_start(out=opsc2, in_=modd[b:b + 1, 4096:5120].partition_broadcast(128)), reads=[b_modd], writes=[B_bc])
        S.op("pool", lambda e: e.tensor_scalar_add(opsc2, opsc2, 1.0), reads=[B_bc], writes=[B_bc])

        S.op("pool", lambda e: e.tensor_tensor(tmpA, ln1b, opsc2, ALU.mult), reads=[B_ln1, B_bc], writes=[B_tmpA])
        S.op("pool", lambda e: e.tensor_tensor(sh2b, sh2b, tmpA, ALU.add), reads=[B_tmpA, B_bc], writes=[B_bc])
        S.op("pool", lambda e: e.tensor_tensor(opsc2, opsc2, ln1g, ALU.mult), reads=[B_ln1, B_bc], writes=[B_bc])

        Sched.alias(B_ob, [B_tmpA])
        Sched.alias(B_sq, [B_tmpA])
        if b == NB - 1:
            Sched.alias(B_wg[0], [B_wi])
            Sched.alias(B_wg[1], [B_wi])
            Sched.alias(B_wu[0], [B_wi, B_rope])
            S.dma("pool", lambda e: e.dma_start(out=wgS[0], in_=wall_d[0, 0].rearrange("(k p) n -> p k n", p=128)), writes=[B_wg[0]])
            S.dma("pool", lambda e: e.dma_start(out=wuS[0], in_=wall_d[0, 1].rearrange("(k p) n -> p k n", p=128)), writes=[B_wu[0]])
            S.dma("pool", lambda e: e.dma_start(out=wgS[1], in_=wall_d[1, 0].rearrange("(k p) n -> p k n", p=128)), writes=[B_wg[1]])
            early.update([(0, 0), (0, 1), (1, 0)])
        B_wout = Buf("wout")
        Sched.alias(B_wout, B_regA)
        S.dma("pool", lambda e: e.dma_start(out=wout, in_=wout_d.rearrange("(k p) n -> p k n", p=128)), writes=[B_wout])
        pairs = [(hp, qb, m, kt) for hp in range(2) for qb in range(4) for m in range(2) for kt in range(NKT)]
        NI = len(pairs)
        SBK = [(0, 1), (6, 7)]

        def emit_S(i):
            hp, qb, m, kt = pairs[i]
            ch = m * 2 + hp
            qs0 = qb * 512
            for hh in range(2):
                r0 = hh * 64
                sb_ = SBK[i % 2][hh]
                S.op("pe", lambda e: e.matmul(P[sb_][:, :], lhsT=kT[r0:r0 + 64, ch, kt * 128:(kt + 1) * 128],
                                              rhs=qT[r0:r0 + 64, ch, qs0:qs0 + 512], start=True, stop=True),
                     reads=[B_kT[kt]] + B_qT[qb * 4:qb * 4 + 4], writes=[PB[sb_]])
            for hh in range(2):
                sb_ = SBK[i % 2][hh]
                pt = (i % 2) * 2 + hh
                S.op("act", lambda e: e.activation(out=PT[pt], in_=P[sb_][:, :], func=AF.Exp), reads=[PB[sb_]], writes=[B_PT[pt]])

        def emit_AV(i):
            hp, qb, m, kt = pairs[i]
            for hh in range(2):
                pt = (i % 2) * 2 + hh
                for qs in range(4):
                    ob = 2 + hh * 2 + qs // 2
                    S.op("pe", lambda e, qs=qs, ob=ob: e.matmul(P[ob][:, (qs % 2) * 129:(qs % 2) * 129 + 129], lhsT=PT[pt][:, qs * 128:(qs + 1) * 128],
                                                                rhs=Vaug[:, kt, 2 * hp + hh, 0:129], start=(kt == 0), stop=(kt == NKT - 1)),
                         reads=[B_PT[pt], B_V[kt]], writes=[PB[ob]])

        def evac(m):
            for hh in range(2):
                for hb in range(2):
                    ob = 2 + hh * 2 + hb
                    S.op("dve", lambda e, hh=hh, hb=hb, ob=ob: e.tensor_copy(osb[hh][:, m, hb * 2:hb * 2 + 2, :], P[ob][:, 0:258].rearrange("p (q e) -> p q e", q=2)),
                         reads=[PB[ob]], writes=[B_osb[hh]])

        def post_A(h, qb):
            oi = h % 2
            osv = osb[oi]
            S.op("dve", lambda e: e.reciprocal(rsb[:, 0:8].rearrange("p (m q) -> p m q", m=2), osv[:, :, :, 128]), reads=[B_osb[oi]], writes=[B_rs])
            S.op("dve", lambda e: e.tensor_scalar_mul(rsb[:, 8:12], rsb[:, 4:8], lams[:, 5:6]), reads=[B_rs, B_lam], writes=[B_rs])
            for qs in range(4):
                S.op("dve", lambda e, qs=qs: e.tensor_scalar_mul(obuf[:, qs, :], osv[:, 0, qs, 0:128], rsb[:, qs:qs + 1]), reads=[B_osb[oi], B_rs], writes=[B_ob])
                S.op("dve", lambda e, qs=qs: e.scalar_tensor_tensor(obuf[:, qs, :], osv[:, 1, qs, 0:128], rsb[:, 8 + qs:9 + qs], obuf[:, qs, :], ALU.mult, ALU.add),
                     reads=[B_osb[oi], B_rs, B_ob], writes=[B_ob])
            S.op("pool", lambda e: e.tensor_tensor(sqb, obuf, obuf, ALU.mult), reads=[B_ob], writes=[B_sq])
            S.op("dve", lambda e: e.reduce_sum(rsb[:, 12:16], sqb, axis=AX.X), reads=[B_sq], writes=[B_rs])

        def post_B(h, qb):
            S.op("act", lambda e: e.activation(out=lnt[:, 0:4], in_=rsb[:, 12:16], func=AF.Ln, scale=1.0 / 128.0, bias=EPS), reads=[B_rs], writes=[B_rs])
            S.op("act", lambda e: e.activation(out=lnt[:, 4:8], in_=lnt[:, 0:4], func=AF.Exp, scale=-0.5), reads=[B_rs], writes=[B_rs])
            for qs in range(4):
                S.op("dve", lambda e, qs=qs: e.scalar_tensor_tensor(datm[:, qs, :], obuf[:, qs, :], lnt[:, 4 + qs:5 + qs], gvec[:], ALU.mult, ALU.mult),
                     reads=[B_ob, B_rs, B_const], writes=[B_da])

        def post_C(h, qb):
            qs0 = qb * 512
            for qs in range(4):
                S.op("pe", lambda e, qs=qs: e.transpose(P_bf[6][:, qs * 128:(qs + 1) * 128], datm[:, qs, :], ident_b[:]), reads=[B_da, B_const], writes=[PB[6]])
            S.op("dve", lambda e: e.tensor_copy(mixT[:, h, qs0:qs0 + 512], P_bf[6][:, 0:512]), reads=[PB[6]], writes=B_mixT[qb * 4:qb * 4 + 4])

        pending = []
        emit_S(0)
        for i in range(NI):
            if i + 1 < NI:
                emit_S(i + 1)
            emit_AV(i)
            hp, qb, m, kt = pairs[i]
            if kt == NKT - 1:
                evac(m)
                if m == 1:
                    pending.append((i + 1, post_A, 2 * hp, qb))
                    pending.append((i + 4, post_B, 2 * hp, qb))
                    pending.append((i + 7, post_C, 2 * hp, qb))
                    pending.append((i + 8, post_A, 2 * hp + 1, qb))
                    pending.append((i + 11, post_B, 2 * hp + 1, qb))
                    pending.append((i + 14, post_C, 2 * hp + 1, qb))
            while pending and pending[0][0] <= i:
                _, fn_, h_, qb_ = pending.pop(0)
                fn_(h_, qb_)
        for _, fn_, h_, qb_ in pending:
            fn_(h_, qb_)

        B_tT, B_lg, B_rtb = Buf("tT"), Buf("lg"), Buf("rtb")
        B_std = [Buf("std%d" % i) for i in range(3)]
        B_xr, B_zt, B_x1t, B_tt, B_tb = ([Buf("xr%d" % i) for i in range(2)], [Buf("zt%d" % i) for i in range(3)],
                                         [Buf("x1t%d" % i) for i in range(2)], [Buf("tt%d" % i) for i in range(2)], [Buf("tb%d" % i) for i in range(2)])
        B_tbd = [Buf("tbd%d_%d" % (b, i)) for i in range(NT)]
        B_tbd_all.extend(B_tbd)
        B_tbl = [Buf("tbl%d" % i) for i in range(4)]
        old = B_qT + B_kT + B_V
        for nb_ in [B_tT, B_lg] + B_std + B_xr + B_zt + B_x1t + B_tt + B_tb:
            Sched.alias(nb_, old)
        Sched.alias(B_rtb, B_PT + B_osb + [B_ob, B_sq, B_da, B_rs])
        obank = {}

        def stageA1pe(n):
            w = n % 2
            S.dma("sp", lambda e: e.dma_start(out=xr[w], in_=x_d[b, n * 128:(n + 1) * 128, :]), writes=[B_xr[w]])
            obank[n] = []
            for hf in range(2):
                bank = next_bank(0, 4)
                obank[n].append(bank)
                for c in range(8):
                    S.op("pe", lambda e, c=c: e.matmul(P[bank][:, :], lhsT=mixT[:, c, n * 128:(n + 1) * 128], rhs=wout[:, c, hf * 512:(hf + 1) * 512],
                                                       start=(c == 0), stop=(c == 7)), reads=[B_mixT[n], B_wout], writes=[PB[bank]])

        def stageA1dve(n):
            w = n % 2
            z = n % 3
            for hf in range(2):
                bank = obank[n][hf]
                S.op("dve", lambda e: e.tensor_tensor(zt[z][:, hf * 512:(hf + 1) * 512], P[bank][:, :], g1b[:, hf * 512:(hf + 1) * 512], ALU.mult),
                     reads=[PB[bank], B_bc], writes=[B_zt[z]])
            S.op("dve", lambda e: e.scalar_tensor_tensor(zt[z], xr[w], ALPHA, zt[z], ALU.mult, ALU.add), reads=[B_xr[w], B_zt[z]], writes=[B_zt[z]])
            for hf in range(2):
                S.op("dve", lambda e, hf=hf: e.bn_stats(st6d[z][:, hf, :], zt[z][:, hf * 512:(hf + 1) * 512]), reads=[B_zt[z]], writes=[B_std[z]])
            S.op("dve", lambda e: e.bn_aggr(mvd[z][:, 0:2], st6d[z][:, :, :]), reads=[B_std[z]], writes=[B_std[z]])
            S.op("act", lambda e: e.activation(out=mvd[z][:, 2:3], in_=mvd[z][:, 1:2], func=AF.Ln, bias=EPS), reads=[B_std[z]], writes=[B_std[z]])
            S.op("act", lambda e: e.activation(out=mvd[z][:, 3:4], in_=mvd[z][:, 2:3], func=AF.Exp, scale=-0.5), reads=[B_std[z]], writes=[B_std[z]])

        def stageA2(n):
            gi = b * NT + n
            w = n % 2
            z = n % 3
            S.op("dve", lambda e: e.tensor_scalar(mvd[z][:, 4:5], mvd[z][:, 0:1], mvd[z][:, 3:4], -1.0, ALU.mult, ALU.mult), reads=[B_std[z]], writes=[B_std[z]])
            S.op("act", lambda e: e.activation(out=zt[z], in_=zt[z], func=AF.Identity, scale=mvd[z][:, 3:4], bias=mvd[z][:, 4:5]), reads=[B_zt[z], B_std[z]], writes=[B_zt[z]])
            S.op("dve", lambda e: e.tensor_tensor(x1t[w], zt[z], ln1g, ALU.mult), reads=[B_zt[z], B_ln1], writes=[B_x1t[w]])
            S.op("dve", lambda e: e.tensor_tensor(x1t[w], x1t[w], ln1b, ALU.add), reads=[B_x1t[w], B_ln1], writes=[B_x1t[w]])
            S.dma("sp", lambda e: e.dma_start(out=x1d[gi * 128:(gi + 1) * 128, :], in_=x1t[w]), reads=[B_x1t[w]], writes=[B_x1d[gi]])
            S.op("dve", lambda e: e.tensor_tensor(tt[w], zt[z], opsc2, ALU.mult), reads=[B_zt[z], B_bc], writes=[B_tt[w]])
            S.op("dve", lambda e: e.tensor_tensor(tt[w], tt[w], sh2b, ALU.add), reads=[B_tt[w], B_bc], writes=[B_tt[w]])
            S.op("act", lambda e: e.activation(out=tb[w], in_=tt[w], func=AF.Copy), reads=[B_tt[w]], writes=[B_tb[w]])
            S.dma("act", lambda e: e.dma_start(out=tbd[gi * 128:(gi + 1) * 128, :], in_=tb[w]), reads=[B_tb[w]], writes=[B_tbd[n]])
            if debug:
                S.dma("sp", lambda e: e.dma_start(out=dbg_d[gi * 128:(gi + 1) * 128, :], in_=tt[w]), reads=[B_tt[w]], writes=[B_dbg[gi]])

        def stageB(n):
            w = n % 2
            for hf in range(2):
                bank = 4 + hf
                for k4 in range(4):
                    k = hf * 4 + k4
                    S.op("pe", lambda e, k=k, k4=k4: e.transpose(P[bank][:, k4 * 128:(k4 + 1) * 128], tt[w][:, k * 128:(k + 1) * 128], ident_f[:]),
                         reads=[B_tt[w], B_const], writes=[PB[bank]])
                S.op("act", lambda e: e.activation(out=tT[:, hf * 4:hf * 4 + 4, :], in_=P[bank][:, :].rearrange("p (k n) -> p k n", k=4), func=AF.Copy),
                     reads=[PB[bank]], writes=[B_tT])
            for k in range(8):
                S.op("pe", lambda e, k=k: e.matmul(P[6][:, (n % 2) * 64:(n % 2) * 64 + 36], lhsT=tT[:, k, :], rhs=rw_sb[:, k, :], start=(k == 0), stop=(k == 7)),
                     reads=[B_tT, B_const], writes=[PB[6]])

        def lgadd(n):
            S.op("dve", lambda e: e.tensor_tensor(lgall[:, n, :], P[6][:, (n % 2) * 64:(n % 2) * 64 + 36], rb_b[:], ALU.add), reads=[PB[6], B_const], writes=[B_lg])

        def route_chunk(c):
            G = RG
            t0 = c * G
            ts = slice(t0, t0 + G)
            gis = slice(b * NT + t0, b * NT + t0 + G)
            off = [0]

            def alloc(wd):
                v = rtb[:, off[0]:off[0] + G * wd].rearrange("p (g x) -> p g x", g=G)
                off[0] += G * wd
                return v
            gmax, gone, gex, gsum, gw, pen = alloc(1), alloc(4), alloc(4), alloc(1), alloc(1), alloc(4)
            elm, m1, mk1, elm2, m2, mk2 = alloc(32), alloc(1), alloc(32), alloc(32), alloc(1), alloc(32)
            dm, ex_, den, rr, ovf, dst, prod, dsum = alloc(1), alloc(1), alloc(1), alloc(1), alloc(32), alloc(32), alloc(64), alloc(2)
            assert off[0] <= 2240
            R = [B_rtb]
            gl = lgall[:, ts, 0:4]
            el = lgall[:, ts, 4:36]
            bc = lambda v, k: v.to_broadcast([128, G, k])
            S.op("dve", lambda e: e.reduce_max(gmax[:, :, 0], gl, axis=AX.X), reads=[B_lg], writes=R)
            S.op("dve", lambda e: e.tensor_tensor(gone, gl, bc(gmax, 4), ALU.is_ge), reads=[B_lg] + R, writes=R)
            S.op("dve", lambda e: e.tensor_tensor(gex, gl, bc(gmax, 4), ALU.subtract), reads=[B_lg] + R, writes=R)
            S.op("act", lambda e: e.activation(out=gex, in_=gex, func=AF.Exp), reads=R, writes=R)
            S.op("dve", lambda e: e.reduce_sum(gsum[:, :, 0], gex, axis=AX.X), reads=R, writes=R)
            S.op("dve", lambda e: e.reciprocal(gw, gsum), reads=R, writes=R)
            S.op("dve", lambda e: e.tensor_scalar(pen, gone, BIG, -BIG, ALU.mult, ALU.add), reads=R, writes=R)
            S.op("dve", lambda e: e.tensor_tensor(elm.rearrange("p g (a x) -> p g a x", a=4), el.rearrange("p g (a x) -> p g a x", a=4),
                                                  pen.unsqueeze(3).to_broadcast([128, G, 4, 8]), ALU.add), reads=[B_lg] + R, writes=R)
            S.op("dve", lambda e: e.reduce_max(m1[:, :, 0], elm, axis=AX.X), reads=R, writes=R)
            S.op("dve", lambda e: e.tensor_tensor(mk1, elm, bc(m1, 32), ALU.is_ge), reads=R, writes=R)
            S.op("dve", lambda e: e.scalar_tensor_tensor(elm2, mk1, -BIG, elm, ALU.mult, ALU.add), reads=R, writes=R)
            S.op("dve", lambda e: e.reduce_max(m2[:, :, 0], elm2, axis=AX.X), reads=R, writes=R)
            S.op("dve", lambda e: e.tensor_tensor(mk2, elm2, bc(m2, 32), ALU.is_ge), reads=R, writes=R)
            S.op("dve", lambda e: e.tensor_tensor(dm, m2, m1, ALU.subtract), reads=R, writes=R)
            S.op("act", lambda e: e.activation(out=ex_, in_=dm, func=AF.Exp), reads=R, writes=R)
            S.op("dve", lambda e: e.tensor_scalar_add(den, ex_, 1.0), reads=R, writes=R)
            S.op("dve", lambda e: e.reciprocal(rr, den), reads=R, writes=R)
            Wb = [B_wts[gi] for gi in range(b * NT + t0, b * NT + t0 + G)]
            S.op("dve", lambda e: e.tensor_tensor(wts_sb[:, gis, 0:1], rr, gw, ALU.mult), reads=R, writes=Wb)
            S.op("dve", lambda e: e.tensor_tensor(wts_sb[:, gis, 1:2], gw, wts_sb[:, gis, 0:1], ALU.subtract), reads=R + Wb, writes=Wb)
            Mbb = [B_Mb[gi] for gi in range(b * NT + t0, b * NT + t0 + G)]
            S.op("dve", lambda e: e.tensor_tensor(Mb[:, gis, :], mk1, mk2, ALU.add), reads=R, writes=Mbb)
            for il in range(G):
                gi = b * NT + t0 + il
                for jj in range(gi):
                    S.op("pe", lambda e, jj=jj, il=il: e.matmul(P[7][:, il * NE:(il + 1) * NE], lhsT=ones_b[:], rhs=Mb[:, jj, :], start=(jj == 0), stop=False),
                         reads=[B_Mb[jj], B_const], writes=[PB[7]])
                S.op("pe", lambda e, gi=gi, il=il: e.matmul(P[7][:, il * NE:(il + 1) * NE], lhsT=ustr_b[:], rhs=Mb[:, gi, :], start=(gi == 0), stop=True),
                     reads=[B_Mb[gi], B_const], writes=[PB[7]])
            pos = P[7][:, 0:G * NE].rearrange("p (g x) -> p g x", g=G)
            eb1024 = econst[:, 0, :].unsqueeze(1).to_broadcast([128, G, NE])
            ebcap = econst[:, 1, :].unsqueeze(1).to_broadcast([128, G, NE])
            S.op("dve", lambda e: e.tensor_scalar_min(dst, pos, 1023.0), reads=[PB[7]], writes=R)
            S.op("dve", lambda e: e.tensor_scalar(ovf, dst, float(CAP), 1.0e6, ALU.is_ge, ALU.mult), reads=R, writes=R)
            S.op("dve", lambda e: e.tensor_tensor(ovf, ovf, ebcap, ALU.add), reads=R + [B_const], writes=R)
            S.op("dve", lambda e: e.tensor_tensor(ovf, ovf, dst, ALU.add), reads=R, writes=R)
            Ib = [B_idx[gi] for gi in range(b * NT + t0, b * NT + t0 + G)]
            Vb = [B_vs[gi] for gi in range(b * NT + t0, b * NT + t0 + G)]
            S.op("dve", lambda e: e.tensor_tensor(prod[:, :, 0:32], ovf, mk1, ALU.mult), reads=R, writes=R)
            S.op("dve", lambda e: e.tensor_tensor(prod[:, :, 32:64], ovf, mk2, ALU.mult), reads=R, writes=R)
            S.op("dve", lambda e: e.reduce_sum(dsum.rearrange("p g a -> p (g a)"), prod.rearrange("p g (a x) -> p (g a) x", a=2), axis=AX.X), reads=R, writes=R)
            S.op("dve", lambda e: e.tensor_copy(idx_sb[:, gis, :], dsum), reads=R, writes=Ib)
            S.op("dve", lambda e: e.tensor_tensor(prod[:, :, 0:32], dst, mk1, ALU.mult), reads=R + Ib, writes=R)
            S.op("dve", lambda e: e.tensor_tensor(prod[:, :, 32:64], dst, mk2, ALU.mult), reads=R, writes=R)
            S.op("dve", lambda e: e.reduce_sum(pslot[:, gis, :].rearrange("p g a -> p (g a)"), prod.rearrange("p g (a x) -> p (g a) x", a=2), axis=AX.X), reads=R, writes=Vb)
            S.op("dve", lambda e: e.tensor_tensor(prod[:, :, 0:32], mk1, eb1024, ALU.mult), reads=R + Vb + [B_const], writes=R)
            S.op("dve", lambda e: e.tensor_tensor(prod[:, :, 32:64], mk2, eb1024, ALU.mult), reads=R + [B_const], writes=R)
            S.op("dve", lambda e: e.reduce_sum(eslot[:, gis, :].rearrange("p g a -> p (g a)"), prod.rearrange("p g (a x) -> p (g a) x", a=2), axis=AX.X), reads=R, writes=Vb)
            for nb_ in B_tbl:
                Sched.alias(nb_, B_mixT[0:8])
            for il in range(G):
                n = t0 + il
                gi = b * NT + n
                w = n % 4
                S.dma("sp", lambda e: e.dma_start(out=tbl[w], in_=tbd[gi * 128:(gi + 1) * 128, :]), reads=[B_tbd[n]], writes=[B_tbl[w]])
                for s_ in range(2):
                    S.dma("pool", lambda e, s_=s_: e.indirect_dma_start(
                        out=Xd, out_offset=bass.IndirectOffsetOnAxis(ap=idx_sb[:, gi, s_:s_ + 1], axis=0), in_=tbl[w], in_offset=None,
                        bounds_check="BCREG", oob_is_err=False), reads=[B_tbl[w], B_idx[gi]], writes=[B_Xd[gi * 2 + s_]])

        stageA1pe(0)
        stageA1pe(1)
        stageA1dve(0)
        for n in range(NT):
            if n >= 1:
                stageB(n - 1)
            if n + 2 < NT:
                stageA1pe(n + 2)
            stageA2(n)
            if n + 1 < NT:
                stageA1dve(n + 1)
            if n >= 2:
                lgadd(n - 2)
            if n == RG + 1:
                route_chunk(0)
        stageB(NT - 1)
        lgadd(NT - 2)
        lgadd(NT - 1)
        route_chunk(1)
        for mb_ in B_mixT:
            Sched.alias(mb_, B_tbl)
        prev_1d = [B_wout, B_tT, B_lg, B_rtb] + B_std + B_xr + B_zt + B_x1t + B_tt + B_tb + B_tbl
        for nb_ in B_regA + B_qT + B_kT + B_V:
            Sched.alias(nb_, prev_1d)

    mixer_all = prev_1d + B_regA + B_qT + B_kT + B_V + B_mixT + [B_qtm2[1], B_ktm2[1], B_vln2[1], B_wi, B_rope, B_sgc, B_ln1, B_bc, B_hcT]
    B_rk = Buf("rank")
    Sched.alias(B_rk, mixer_all)
    rk = carve(O_WOV, 2240 * 4, F32)
    T1 = carve(O_WOV + 8960, 4096, F32, "p (a c) -> p a c", a=32)
    T2 = carve(O_WOV + 8960 + 4096, 4096, F32, "p (a c) -> p a c", a=32)
    RK = [B_rk]
    cnt, cntu, rank_, permf = rk[:, 0:32], rk[:, 32:64], rk[:, 64:96], rk[:, 96:104]
    sv = lambda i: rk[:, 128 + i * 64:128 + (i + 1) * 64]
    pos_s, e1k, qs_s, ovp, vm, vo, mainf, ovi, t_a, t_b = [sv(i) for i in range(10)]
    pflat = pslot[:].rearrange("p g a -> p (g a)")
    eflat = eslot[:].rearrange("p g a -> p (g a)")
    def emit_ranking():
        for j in range(NB * NT):
            S.op("pe", lambda e, j=j: e.matmul(P[7][:, 0:NE], lhsT=ones_b[:], rhs=Mb[:, j, :], start=(j == 0), stop=(j == NB * NT - 1)),
                 reads=[B_Mb[j], B_const], writes=[PB[7]])
        S.op("dve", lambda e: e.tensor_tensor(cntu, P[7][:, 0:NE], econst[:, 2, :], ALU.add), reads=[PB[7], B_const], writes=RK)
        S.op("dve", lambda e: e.tensor_tensor(T1, cntu.unsqueeze(1).to_broadcast([128, NE, NE]), cntu.unsqueeze(2).to_broadcast([128, NE, NE]), ALU.is_gt), reads=RK, writes=RK)
        S.op("dve", lambda e: e.reduce_sum(rank_, T1, axis=AX.X), reads=RK, writes=RK)
        T2v = T2[:, 0:R_OV, :]
        S.op("dve", lambda e: e.tensor_tensor(T2v, rank_.unsqueeze(1).to_broadcast([128, R_OV, NE]), econst[:, 4, 0:R_OV].unsqueeze(2).to_broadcast([128, R_OV, NE]), ALU.is_equal),
             reads=RK + [B_const], writes=RK)
        S.op("dve", lambda e: e.tensor_tensor(T2v, T2v, econst[:, 3, :].unsqueeze(1).to_broadcast([128, R_OV, NE]), ALU.mult), reads=RK + [B_const], writes=RK)
        S.op("dve", lambda e: e.reduce_sum(permf, T2v, axis=AX.X), reads=RK, writes=RK)
        S.op("dve", lambda e: e.tensor_copy(perm_i[:], permf), reads=RK, writes=[B_perm])
        S.op("dve", lambda e: e.tensor_copy(pos_s, pflat), reads=B_vs, writes=RK)
        S.op("dve", lambda e: e.tensor_copy(e1k, eflat), reads=B_vs, writes=RK)
        for hh in range(2):
            S.op("dve", lambda e, hh=hh: e.tensor_tensor(T1, e1k[:, hh * 32:(hh + 1) * 32].unsqueeze(2).to_broadcast([128, 32, NE]),
                                                         econst[:, 0, :].unsqueeze(1).to_broadcast([128, 32, NE]), ALU.is_equal), reads=RK + [B_const], writes=RK)
            S.op("dve", lambda e: e.tensor_tensor(T1, T1, rank_.unsqueeze(1).to_broadcast([128, 32, NE]), ALU.mult), reads=RK, writes=RK)
            S.op("dve", lambda e, hh=hh: e.reduce_sum(qs_s[:, hh * 32:(hh + 1) * 32], T1, axis=AX.X), reads=RK, writes=RK)
        S.op("dve", lambda e: e.tensor_single_scalar(vm, pos_s, float(CAP), ALU.is_lt), reads=RK, writes=RK)
        S.op("dve", lambda e: e.tensor_scalar_add(ovp, pos_s, -float(CAP)), reads=RK, writes=RK)
        S.op("dve", lambda e: e.tensor_single_scalar(vo, ovp, float(CAPO), ALU.is_lt), reads=RK, writes=RK)
        S.op("dve", lambda e: e.tensor_single_scalar(t_a, ovp, 0.0, ALU.is_ge), reads=RK, writes=RK)
        S.op("dve", lambda e: e.tensor_tensor(vo, vo, t_a, ALU.mult), reads=RK, writes=RK)
        S.op("dve", lambda e: e.tensor_single_scalar(t_a, qs_s, float(R_OV), ALU.is_lt), reads=RK, writes=RK)
        S.op("dve", lambda e: e.tensor_tensor(vo, vo, t_a, ALU.mult), reads=RK, writes=RK)
        S.op("dve", lambda e: e.scalar_tensor_tensor(mainf, e1k, float(CAP) / 1024.0, pos_s, ALU.mult, ALU.add), reads=RK, writes=RK)
        S.op("dve", lambda e: e.scalar_tensor_tensor(ovi, qs_s, float(CAPO), ovp, ALU.mult, ALU.add), reads=RK, writes=RK)
        S.op("dve", lambda e: e.tensor_scalar_add(ovi, ovi, float(NMAIN)), reads=RK, writes=RK)
        S.op("dve", lambda e: e.tensor_scalar_add(t_a, mainf, -float(ZROW)), reads=RK, writes=RK)
        S.op("dve", lambda e: e.tensor_tensor(t_a, t_a, vm, ALU.mult), reads=RK, writes=RK)
        S.op("dve", lambda e: e.tensor_scalar_add(t_b, ovi, -float(ZROW)), reads=RK, writes=RK)
        S.op("dve", lambda e: e.tensor_tensor(t_b, t_b, vo, ALU.mult), reads=RK, writes=RK)
        S.op("dve", lambda e: e.tensor_tensor(t_a, t_a, t_b, ALU.add), reads=RK, writes=RK)
        S.op("dve", lambda e: e.tensor_scalar_add(t_a, t_a, float(ZROW)), reads=RK, writes=RK)
        S.op("dve", lambda e: e.tensor_copy(idxg_sb[:].rearrange("p g a -> p (g a)"), t_a), reads=RK, writes=B_idx)
        S.op("dve", lambda e: e.tensor_scalar_add(t_b, ovi, -1.0e6), reads=RK, writes=RK)
        S.op("dve", lambda e: e.tensor_tensor(t_b, t_b, vo, ALU.mult), reads=RK, writes=RK)
        S.op("dve", lambda e: e.tensor_scalar_add(t_b, t_b, 1.0e6), reads=RK, writes=RK)
        S.op("dve", lambda e: e.tensor_copy(idxo_sb[:].rearrange("p g a -> p (g a)"), t_b), reads=RK, writes=[B_io])


    B_XT, B_hid = [Buf("XT0"), Buf("XT1")], Buf("hid")
    B_xg, B_sgt, B_ysb = [Buf("xg%d" % i) for i in range(4)], [Buf("sgt0"), Buf("sgt1")], [Buf("ysb%d" % i) for i in range(2)]
    B_tbo = [Buf("tbo0"), Buf("tbo1")]
    moe_bufs = B_wg + B_wu + B_wd + B_XT + [B_hid] + B_xg + B_sgt + B_ysb + B_tbo
    for nb_ in moe_bufs:
        if nb_ in (B_wg[0], B_wg[1], B_wu[0]):
            continue
        Sched.alias(nb_, mixer_all)

    def load_expert(ex):
        i = ex % 2
        if (ex, 0) not in early:
            S.dma("pool", lambda e: e.dma_start(out=wgS[i], in_=wall_d[ex, 0].rearrange("(k p) n -> p k n", p=128)), writes=[B_wg[i]])
        if (ex, 1) not in early:
            S.dma("pool", lambda e: e.dma_start(out=wuS[i], in_=wall_d[ex, 1].rearrange("(k p) n -> p k n", p=128)), writes=[B_wu[i]])
        S.dma("pool", lambda e: e.dma_start(out=wdS[i], in_=wall_d[ex, 2].rearrange("(k p) n -> p k n", p=128)), writes=[B_wd[i]])

    ctr = {"xg": 0, "y": 0, "g": 0, "xt": 0}

    def prep_X(row0, ntile, xi_t, xreads, xt, Bxt):
        for s_ in range(ntile):
            xi = ctr["xg"] % 4
            ctr["xg"] += 1
            S.dma("sp", lambda e, s_=s_, xi=xi: e.dma_start(out=xg[xi], in_=Xd[row0 + s_ * 128: row0 + (s_ + 1) * 128, :]), reads=xreads, writes=[B_xg[xi]])
            for hf in range(2):
                bank = next_bank(0, 2)
                for k4 in range(4):
                    k = hf * 4 + k4
                    S.op("pe", lambda e, k=k, k4=k4, xi=xi, bank=bank: e.transpose(P_bf[bank][:, k4 * 128:(k4 + 1) * 128], xg[xi][:, k * 128:(k + 1) * 128], ident_b[:]),
                         reads=[B_xg[xi], B_const], writes=[PB[bank]])
                S.op("dve", lambda e, hf=hf, s_=s_, bank=bank: e.tensor_copy(xt[:, hf * 4:hf * 4 + 4, s_ * 128:(s_ + 1) * 128], P_bf[bank][:, 0:512].rearrange("p (k n) -> p k n", k=4)),
                     reads=[PB[bank]], writes=[Bxt])

    def compute(wgv, wuv, wdv, Bw, xt, Bxt, ncap, row0, yd0):
        for f in range(8):
            pg = 2 + (ctr["g"] % 2)
            pu = 4 + (ctr["g"] % 2)
            sg_i = ctr["g"] % 2
            ctr["g"] += 1
            for k in range(8):
                S.op("pe", lambda e, k=k: e.matmul(P[pg][:, 0:ncap], lhsT=wgv[:, k, f * 128:(f + 1) * 128], rhs=xt[:, k, 0:ncap],
                                                   start=(k == 0), stop=(k == 7)), reads=Bw + [Bxt], writes=[PB[pg]])
            for k in range(8):
                S.op("pe", lambda e, k=k: e.matmul(P[pu][:, 0:ncap], lhsT=wuv[:, k, f * 128:(f + 1) * 128], rhs=xt[:, k, 0:ncap],
                                                   start=(k == 0), stop=(k == 7)), reads=Bw + [Bxt], writes=[PB[pu]])
            S.op("act", lambda e: e.activation(out=sgt[sg_i][:, 0:ncap], in_=P[pg][:, 0:ncap], func=AF.Silu), reads=[PB[pg]], writes=[B_sgt[sg_i]])
            S.op("dve", lambda e: e.tensor_tensor(hidT[:, f, 0:ncap], P[pu][:, 0:ncap], sgt[sg_i][:, 0:ncap], ALU.mult),
                 reads=[PB[pu], B_sgt[sg_i]], writes=[B_hid])
        for s_ in range(ncap // 128):
            yi = ctr["y"] % 2
            ctr["y"] += 1
            for dh in range(2):
                pd = 6 + dh
                for f in range(8):
                    S.op("pe", lambda e, f=f: e.matmul(P[pd][:, :], lhsT=hidT[:, f, s_ * 128:(s_ + 1) * 128], rhs=wdv[:, f, dh * 512:(dh + 1) * 512],
                                                       start=(f == 0), stop=(f == 7)), reads=[B_hid] + Bw, writes=[PB[pd]])
                if dh == 0:
                    S.op("act", lambda e: e.activation(out=ysb[yi][:, 0:512], in_=P[pd][:, :], func=AF.Copy), reads=[PB[pd]], writes=[B_ysb[yi]])
                else:
                    S.op("dve", lambda e: e.tensor_copy(ysb[yi][:, 512:1024], P[pd][:, :]), reads=[PB[pd]], writes=[B_ysb[yi]])
            S.dma("act", lambda e: e.dma_start(out=Yd[row0 + s_ * 128: row0 + (s_ + 1) * 128, :], in_=ysb[yi]), reads=[B_ysb[yi]], writes=[B_Yd[yd0 + s_]])

    ov_tiles = list(range(NB * NT))

    def overflow_scatter(k):
        for _ in range(k):
            if not ov_tiles:
                return
            gi = ov_tiles.pop(0)
            w = gi % 2
            S.dma("sp", lambda e: e.dma_start(out=tbo[w], in_=tbd[gi * 128:(gi + 1) * 128, :]), reads=[B_tbd_all[gi]], writes=[B_tbo[w]])
            for s_ in range(2):
                S.dma("pool", lambda e, s_=s_: e.indirect_dma_start(
                    out=Xd, out_offset=bass.IndirectOffsetOnAxis(ap=idxo_sb[:, gi, s_:s_ + 1], axis=0), in_=tbo[w], in_offset=None,
                    bounds_check="BCREG", oob_is_err=False), reads=[B_tbo[w], B_io], writes=[B_Xo[gi * 2 + s_]])

    NS = CAP // 128
    NSO = CAPO // 128
    B_wov = [Buf("wov0")]
    B_XTo = Buf("XTo")
    for nb_ in B_wov + [B_XTo]:
        Sched.alias(nb_, mixer_all + [B_rk])

    def load_overflow(r):
        dst = wov[0]

        def bld(eng):
            reg = eng.alloc_register("pr%d" % r)
            eng.reg_load(reg, perm_i[0:1, r:r + 1])
            v = eng.snap(reg, donate=True, min_val=0, max_val=NE - 1)
            return eng.dma_start(out=dst, in_=wall_d[bass.ds(v, 1)].rearrange("o w (k p) n -> p (o w k) n", p=128))
        S.dma("pool", Late(bld), reads=[B_perm], writes=[B_wov[0]])

    ov_slot = {17 + 2 * r: r for r in range(R_OV)}
    load_expert(0)
    prep_X(0, NS, 0, B_Xd, XTm[0], B_XT[0])
    for ex in range(NE):
        i = ex % 2
        if ex + 1 < NE:
            load_expert(ex + 1)
            prep_X((ex + 1) * CAP, NS, (ex + 1) % 2, B_Xd, XTm[(ex + 1) % 2], B_XT[(ex + 1) % 2])
        if ex + 1 in ov_slot:
            load_overflow(ov_slot[ex + 1])
        if ex >= 1:
            overflow_scatter(2)
        compute(wgS[i], wuS[i], wdS[i], [B_wg[i], B_wu[i], B_wd[i]], XTm[i], B_XT[i], CAP, ex * CAP, ex * NS)
        if ex == 0:
            emit_ranking()
        if ex in ov_slot:
            r = ov_slot[ex]
            prep_X(NMAIN + r * CAPO, NSO, 0, B_Xo, XTo, B_XTo)
            wv = wov[0]
            compute(wv[:, 0:8, :], wv[:, 8:16, :], wv[:, 16:24, :], [B_wov[0]], XTo, B_XTo, CAPO, NMAIN + r * CAPO, NE * NS + r * NSO)
    assert not ov_tiles
    moe_bufs = moe_bufs + B_wov + [B_XTo]

    B_fin = Buf("fin")
    B_fs = [Buf("fst0"), Buf("fst1")]
    NBUF = 4
    B_Y1, B_Y2, B_x1r = [Buf("Y1%d" % i) for i in range(NBUF)], [Buf("Y2%d" % i) for i in range(NBUF)], [Buf("x1r%d" % i) for i in range(NBUF)]
    for nb_ in [B_fin] + B_fs + B_Y1 + B_Y2 + B_x1r:
        Sched.alias(nb_, moe_bufs)
    for b in range(NB):
        S.dma("sp", lambda e, b=b: e.dma_start(out=g2bt[b], in_=modd[b:b + 1, 5120:6144].partition_broadcast(128)), reads=[b_modd], writes=[B_fin])
    S.dma("sp", lambda e: e.dma_start(out=ln2g, in_=ln2g_d.partition_broadcast(128)), writes=[B_fin])
    S.dma("sp", lambda e: e.dma_start(out=ln2b, in_=ln2b_d.partition_broadcast(128)), writes=[B_fin])
    B_zrow = Buf("zrow")
    S.op("dve", lambda e: e.memset(x1r[0][0:1, :], 0.0), writes=[B_x1r[0]])
    S.dma("sp", lambda e: e.dma_start(out=Yd[ZROW:ZROW + 1, :], in_=x1r[0][0:1, :]), reads=[B_x1r[0]], writes=[B_zrow])
    YdAll = B_Yd + [B_zrow]

    def fetch(gi):
        w = gi % NBUF
        S.dma("pool", lambda e: e.indirect_dma_start(out=Y1[w], out_offset=None, in_=Yd, in_offset=bass.IndirectOffsetOnAxis(ap=idxg_sb[:, gi, 0:1], axis=0),
                                                   bounds_check="BCREG2", oob_is_err=False), reads=YdAll + [B_idx[gi]], writes=[B_Y1[w]])
        S.dma("pool", lambda e: e.indirect_dma_start(out=Y2[w], out_offset=None, in_=Yd, in_offset=bass.IndirectOffsetOnAxis(ap=idxg_sb[:, gi, 1:2], axis=0),
                                                   bounds_check="BCREG2", oob_is_err=False), reads=YdAll + [B_idx[gi]], writes=[B_Y2[w]])
        S.dma("sp", lambda e: e.dma_start(out=x1r[w], in_=x1d[gi * 128:(gi + 1) * 128, :]), reads=[B_x1d[gi]], writes=[B_x1r[w]])

    def comb1(gi):
        b = gi // NT
        w = gi % NBUF
        f_ = fst[gi % 2]
        Bf = B_fs[gi % 2]
        S.op("act", lambda e: e.activation(out=Y1[w], in_=Y1[w], func=AF.Identity, scale=wts_sb[:, gi, 0:1]), reads=[B_Y1[w], B_wts[gi]], writes=[B_Y1[w]])
        S.op("dve", lambda e: e.scalar_tensor_tensor(Y1[w], Y2[w], wts_sb[:, gi, 1:2], Y1[w], ALU.mult, ALU.add), reads=[B_Y1[w], B_Y2[w], B_wts[gi]], writes=[B_Y1[w]])
        S.op("dve", lambda e: e.tensor_tensor(Y1[w], Y1[w], g2bt[b], ALU.mult), reads=[B_Y1[w], B_fin], writes=[B_Y1[w]])
        S.op("dve", lambda e: e.scalar_tensor_tensor(Y1[w], x1r[w], ALPHA, Y1[w], ALU.mult, ALU.add), reads=[B_Y1[w], B_x1r[w]], writes=[B_Y1[w]])
        for hf in range(2):
            S.op("dve", lambda e, hf=hf: e.bn_stats(f_[:, hf * 6:hf * 6 + 6], Y1[w][:, hf * 512:(hf + 1) * 512]), reads=[B_Y1[w]], writes=[Bf])
        S.op("dve", lambda e: e.bn_aggr(f_[:, 12:14], f_[:, 0:12].rearrange("p (a s) -> p a s", a=2)), reads=[Bf], writes=[Bf])
        S.op("act", lambda e: e.activation(out=f_[:, 14:15], in_=f_[:, 13:14], func=AF.Ln, bias=EPS), reads=[Bf], writes=[Bf])
        S.op("act", lambda e: e.activation(out=f_[:, 15:16], in_=f_[:, 14:15], func=AF.Exp, scale=-0.5), reads=[Bf], writes=[Bf])

    def comb2a(gi):
        w = gi % NBUF
        f_ = fst[gi % 2]
        Bf = B_fs[gi % 2]
        S.op("dve", lambda e: e.tensor_scalar(f_[:, 16:17], f_[:, 12:13], f_[:, 15:16], -1.0, ALU.mult, ALU.mult), reads=[Bf], writes=[Bf])
        S.op("act", lambda e: e.activation(out=Y1[w], in_=Y1[w], func=AF.Identity, scale=f_[:, 15:16], bias=f_[:, 16:17]), reads=[B_Y1[w], Bf], writes=[B_Y1[w]])

    def comb2b(gi):
        b = gi // NT
        n = gi % NT
        w = gi % NBUF
        S.op("dve", lambda e: e.tensor_tensor(Y1[w], Y1[w], ln2g, ALU.mult), reads=[B_Y1[w], B_fin], writes=[B_Y1[w]])
        S.op("dve", lambda e: e.tensor_tensor(Y2[w], Y1[w], ln2b, ALU.add), reads=[B_Y1[w], B_fin], writes=[B_Y2[w]])
        S.dma("sp", lambda e: e.dma_start(out=out_d[b, n * 128:(n + 1) * 128, :], in_=Y2[w]), reads=[B_Y2[w]], writes=[B_out[gi]])

    NTOT = NB * NT
    for gi in range(min(3, NTOT)):
        fetch(gi)
    comb1(0)
    for gi in range(NTOT):
        if gi + 3 < NTOT:
            fetch(gi + 3)
        comb2a(gi)
        if gi + 1 < NTOT:
            comb1(gi + 1)
        comb2b(gi)
    S.wait_all("sp", B_out + (B_dbg if debug else []))
    S.emit(nc, st)
    st.close()
    return nc


def _rope_tables():
    half = 16
    inv = (10000.0 ** (-np.arange(half, dtype=np.float32) / half)).astype(np.float32)
    tok = np.arange(L)
    rows = (tok // 64).astype(np.float32)
    cols = (tok % 64).astype(np.float32)
    cos = np.zeros((L, 64), np.float32)
    sin = np.zeros((L, 64), np.float32)
    for base, pos in ((0, rows), (32, cols)):
        ang = (pos[:, None] * inv[None, :]).astype(np.float32)
        c, s = np.cos(ang).astype(np.float32), np.sin(ang).astype(np.float32)
        cos[:, base:base + 16] = c
        cos[:, base + 16:base + 32] = c
        sin[:, base:base + 16] = -s
        sin[:, base + 16:base + 32] = s
    return cos, sin


def make_in_maps(inputs, ncores=NCORES):
    f = lambda a: np.ascontiguousarray(np.asarray(a, dtype=np.float32))
    cos, sin = _rope_tables()
    shared = {
        "w_mod": f(inputs["w_mod"][0]), "b_mod": f(inputs["b_mod"][0]).reshape(1, -1),
        "w_in": f(inputs["w_in"][0]), "w_out": f(inputs["w_out"][0]),
        "lamv": np.concatenate([f(inputs[k][0]) for k in ("lam_q1", "lam_k1", "lam_q2", "lam_k2")]).reshape(1, 256),
        "subln_g": f(inputs["subln_g"][0]).reshape(1, -1),
        "sg_ln_g": f(inputs["sg_ln_g"][0]).reshape(1, -1), "sg_ln_b": f(inputs["sg_ln_b"][0]).reshape(1, -1),
        "sg_wT": np.ascontiguousarray(f(inputs["sg_w"][0]).transpose(0, 2, 1)),
        "sg_b": f(inputs["sg_b"][0]).reshape(1, -1),
        "ln1_g": f(inputs["ln1_g"][0]).reshape(1, -1), "ln1_b": f(inputs["ln1_b"][0]).reshape(1, -1),
        "ln2_g": f(inputs["ln2_g"][0]).reshape(1, -1), "ln2_b": f(inputs["ln2_b"][0]).reshape(1, -1),
        "router_w": np.ascontiguousarray(np.concatenate([f(inputs["router_group_w"][0]), f(inputs["router_expert_w"][0])], axis=1)),
        "router_b": np.concatenate([f(inputs["router_group_b"][0]), f(inputs["router_expert_b"][0])]).reshape(1, 36),
        "exp_w": np.ascontiguousarray(np.stack([f(inputs["exp_w_gate"][0]), f(inputs["exp_w_up"][0]), f(inputs["exp_w_down"][0])], axis=1)),
        "ident": np.eye(128, dtype=np.float32),
        "ustrict": np.triu(np.ones((128, 128), np.float32), 1),
        "rope_cos": cos, "rope_sin": sin,
        "econst": np.concatenate([np.arange(NE, dtype=np.float32) * 1024.0, np.arange(NE, dtype=np.float32) * CAP,
                                  np.arange(NE, dtype=np.float32) / 64.0, np.arange(NE, dtype=np.float32),
                                  np.arange(NE, dtype=np.float32)]).reshape(1, 5 * NE),
    }
    x = f(inputs["x"]); c = f(inputs["c"]); ctx = f(inputs["ctx"]); cc = f(inputs["c_ctx"])
    maps = []
    for i in range(ncores):
        sl = slice(i * NB, (i + 1) * NB)
        cv = np.stack([c[i * NB], c[i * NB + 1], cc], axis=1)
        cT = np.ascontiguousarray(cv.reshape(8, 128, 3).transpose(1, 0, 2))
        m = dict(shared)
        m.update({"x": np.ascontiguousarray(x[sl]), "ctx": np.ascontiguousarray(ctx[sl]), "cT": cT})
        maps.append(m)
    return maps


_NC_CACHE = {}


def kernel(**inputs):
    if "nc" not in _NC_CACHE:
        _NC_CACHE["nc"] = build_nc()
    nc = _NC_CACHE["nc"]
    in_maps = make_in_maps(inputs)
    res = run_bass_kernel_spmd(nc, in_maps, core_ids=list(range(NCORES)))
    out = np.concatenate([np.asarray(r["out"]) for r in res.results], axis=0)
    return out.astype(np.float32)
```

```python
import math
from contextlib import ExitStack

import numpy as np
import concourse.bass as bass
import concourse.mybir as mybir
from concourse.bass_utils import run_bass_kernel_spmd

F32 = mybir.dt.float32
BF16 = mybir.dt.bfloat16
I32 = mybir.dt.int32
AF = mybir.ActivationFunctionType
ALU = mybir.AluOpType
AX = mybir.AxisListType

NCORES = 8
D = 1024
L = 2048
CTXL = 256
NKT = 18
NB = 2
NT = 16
NE = 32
CAP = 512
R_OV = 8
CAPO = 256
NMAIN = NE * CAP
ZROW = NMAIN + R_OV * CAPO
NSLOT = ZROW
ALPHA = 2.0 ** 0.25
LAM_INIT = 0.8 - 0.6 * math.exp(0.0)
EPS = 1e-5
BIG = 1.0e4

COMPUTE = ("pe", "act", "dve", "pool")
QUEUES = ("sp", "act", "pool")


class Buf:
    __slots__ = ("name", "lw", "rd")

    def __init__(self, name):
        self.name = name
        self.lw = None
        self.rd = []


class Op:
    __slots__ = ("eng", "fn", "deps", "signal", "sigval", "is_dma", "slot", "use", "cidx")


class _Rec:
    def __init__(self):
        self.call = None

    def __getattr__(self, name):
        def f(*a, **k):
            self.call = (name, a, k)
            return self
        return f


class Late:
    def __init__(self, builder):
        self.builder = builder


class Sched:
    def __init__(self, nslots=12):
        self.ops = {e: [] for e in ("pe", "act", "dve", "pool", "sp")}
        self.ccount = {e: 0 for e in self.ops}
        self.dcount = {e: 0 for e in self.ops}
        self.nslots = nslots
        self.slot_last = {}

    def _add(self, eng, fn, reads, writes, is_dma):
        op = Op()
        if fn is not None and not isinstance(fn, Late):
            rec = _Rec()
            fn(rec)
            fn = rec.call
        op.eng, op.fn, op.is_dma = eng, fn, is_dma
        op.signal, op.sigval, op.slot, op.use = False, None, None, None
        deps = []
        for b in reads:
            if b.lw is not None:
                deps.append(b.lw)
        for b in writes:
            if b.lw is not None:
                deps.append(b.lw)
            deps.extend(b.rd)
        op.cidx = self.ccount[eng]
        if is_dma:
            j = self.dcount[eng]
            self.dcount[eng] += 1
            op.slot = j % self.nslots
            op.use = j // self.nslots + 1
            prev = self.slot_last.get((eng, op.slot))
            if prev is not None:
                deps.append(prev)
            self.slot_last[(eng, op.slot)] = op
        else:
            self.ccount[eng] += 1
        seen = set()
        op.deps = []
        for d in deps:
            if d is op or id(d) in seen:
                continue
            seen.add(id(d))
            op.deps.append(d)
        for b in reads:
            b.rd.append(op)
        for b in writes:
            b.lw = op
            b.rd = []
        self.ops[eng].append(op)
        return op

    def op(self, eng, fn, reads=(), writes=()):
        return self._add(eng, fn, list(reads), list(writes), False)

    def dma(self, eng, fn, reads=(), writes=()):
        return self._add(eng, fn, list(reads), list(writes), True)

    def wait_all(self, eng, bufs):
        return self._add(eng, None, list(bufs), [], False)

    @staticmethod
    def alias(new, olds):
        for o in olds:
            if o.lw is not None:
                new.rd.append(o.lw)
            new.rd.extend(o.rd)

    @staticmethod
    def _needs_wait(op, d):
        if d.is_dma or d.eng != op.eng:
            return True
        if op.eng == "pe":
            return False
        if op.eng == "pool" or op.is_dma:
            return True
        return (op.cidx - d.cidx) < 2

    def emit(self, nc, stack):
        for lst in self.ops.values():
            for op in lst:
                for d in op.deps:
                    if not d.is_dma and self._needs_wait(op, d):
                        d.signal = True
        for lst in self.ops.values():
            c = 0
            for op in lst:
                if not op.is_dma and op.fn is not None and op.signal:
                    c += 1
                    op.sigval = c
        csem = {e: stack.enter_context(nc.semaphore("c_" + e)) for e in COMPUTE}
        dsem = {}
        for q in QUEUES:
            for s in range(min(self.nslots, self.dcount[q])):
                dsem[(q, s)] = stack.enter_context(nc.semaphore("d_%s%d" % (q, s)))
        sched = self

        def run(eng_obj, e):
            known = {}
            bc_reg = eng_obj.to_reg(NSLOT - 1) if e == "pool" else None
            bc_reg2 = eng_obj.to_reg(NSLOT) if e == "pool" else None
            for op in sched.ops[e]:
                for d in op.deps:
                    if not sched._needs_wait(op, d):
                        continue
                    if d.is_dma:
                        sem, val, key = dsem[(d.eng, d.slot)], 16 * d.use, ("d", d.eng, d.slot)
                    else:
                        sem, val, key = csem[d.eng], d.sigval, ("c", d.eng)
                    if known.get(key, 0) >= val:
                        continue
                    known[key] = val
                    eng_obj.wait_ge(sem, val)
                if op.fn is None:
                    continue
                if isinstance(op.fn, Late):
                    inst = op.fn.builder(eng_obj)
                    inst.then_inc(dsem[(e, op.slot)], 16)
                    continue
                try:
                    kw = op.fn[2]
                    if kw.get("bounds_check") == "BCREG":
                        kw = dict(kw)
                        kw["bounds_check"] = bc_reg
                    elif kw.get("bounds_check") == "BCREG2":
                        kw = dict(kw)
                        kw["bounds_check"] = bc_reg2
                    inst = getattr(eng_obj, op.fn[0])(*op.fn[1], **kw)
                except Exception:
                    ii = sched.ops[e].index(op)
                    print("PREV", [(o_.fn[0] if o_.fn else None) for o_ in sched.ops[e][max(0, ii - 12):ii]], ii, flush=True)
                    print("FAILED OP", e, op.fn[0], [getattr(a, "shape", a) for a in op.fn[1]],
                          {k: getattr(v, "shape", v) for k, v in op.fn[2].items()}, flush=True)
                    raise
                if op.is_dma:
                    inst.then_inc(dsem[(e, op.slot)], 16)
                elif op.signal:
                    inst.then_inc(csem[e], 1)

        with nc.Block() as block:
            @block.sync
            def _(eng):
                run(eng, "sp")

            @block.tensor
            def _(eng):
                run(eng, "pe")

            @block.scalar
            def _(eng):
                run(eng, "act")

            @block.vector
            def _(eng):
                run(eng, "dve")

            @block.gpsimd
            def _(eng):
                run(eng, "pool")


def build_nc(debug=False):
    nc = bass.Bass("TRN2", target_bir_lowering=False)

    def din(name, shape, dt=F32):
        return nc.dram_tensor(name, list(shape), dt, kind="ExternalInput").ap()

    x_d = din("x", [NB, L, D])
    ctx_d = din("ctx", [NB, CTXL, D])
    cT_d = din("cT", [128, 8, 3])
    wmod_d = din("w_mod", [D, 6 * D])
    bmod_d = din("b_mod", [1, 6 * D])
    win_d = din("w_in", [D, 2560])
    wout_d = din("w_out", [D, D])
    lamv_d = din("lamv", [1, 256])
    subg_d = din("subln_g", [1, 128])
    sglg_d = din("sg_ln_g", [1, 512])
    sglb_d = din("sg_ln_b", [1, 512])
    sgwT_d = din("sg_wT", [4, 128, 128])
    sgb_d = din("sg_b", [1, 512])
    ln1g_d = din("ln1_g", [1, D])
    ln1b_d = din("ln1_b", [1, D])
    ln2g_d = din("ln2_g", [1, D])
    ln2b_d = din("ln2_b", [1, D])
    rw_d = din("router_w", [D, 36])
    rb_d = din("router_b", [1, 36])
    wall_d = din("exp_w", [NE, 3, D, D])
    ident_d = din("ident", [128, 128])
    ustr_d = din("ustrict", [128, 128])
    ropec_d = din("rope_cos", [L, 64])
    ropes_d = din("rope_sin", [L, 64])
    econst_d = din("econst", [1, 5 * NE])
    out_d = nc.dram_tensor("out", [NB, L, D], F32, kind="ExternalOutput").ap()
    dbg_d = nc.dram_tensor("dbg", [NB * L, D], F32, kind="ExternalOutput").ap() if debug else None

    modd = nc.dram_tensor("modd", [3, 6 * D], F32, kind="Internal").ap()
    x1d = nc.dram_tensor("x1d", [NB * L, D], F32, kind="Internal").ap()
    tbd = nc.dram_tensor("tbd", [NB * L, D], BF16, kind="Internal").ap()
    Xd = nc.dram_tensor("Xd", [NSLOT, D], BF16, kind="Internal").ap()
    Yd = nc.dram_tensor("Yd", [NSLOT + 1, D], F32, kind="Internal").ap()
    b_modd = Buf("modd")
    B_x1d = [Buf("x1d%d" % i) for i in range(NB * NT)]
    B_Xd = [Buf("Xd%d" % i) for i in range(NB * NT * 2)]
    B_Yd = [Buf("Yd%d" % i) for i in range(NE * (CAP // 128) + R_OV * (CAPO // 128))]
    B_Xo = [Buf("Xo%d" % i) for i in range(NB * NT * 2)]
    B_vs = [Buf("vs%d" % i) for i in range(NB * NT)]
    B_perm, B_io = Buf("perm"), Buf("idxo")
    B_out = [Buf("out%d" % i) for i in range(NB * NT)]
    B_dbg = [Buf("dbg%d" % i) for i in range(NB * NT)]

    S = Sched()
    st = ExitStack()
    st.enter_context(nc.allow_low_precision("bf16 matmul operands, fp32 accumulation"))
    st.enter_context(nc.allow_non_contiguous_dma(reason="tiny setup gathers"))

    def sb(name, shape, dt):
        return st.enter_context(nc.sbuf_tensor(name, list(shape), dt))

    ident_f = sb("ident_f", [128, 128], F32)
    ident_b = sb("ident_b", [128, 128], BF16)
    ones_b = sb("ones_b", [128, 128], BF16)
    ustr_b = sb("ustr_b", [128, 128], BF16)
    rw_sb = sb("rw_sb", [128, 8, 36], F32)
    rb_b = sb("rb_b", [128, 36], F32)
    econst = sb("econst_sb", [128, 5, NE], F32)
    pslot = sb("pslot", [128, NB * NT, 2], F32)
    eslot = sb("eslot", [128, NB * NT, 2], F32)
    idxo_sb = sb("idxo_sb", [128, NB * NT, 2], I32)
    perm_i = sb("perm_i", [128, R_OV], I32)
    gvec = sb("gvec", [128, 128], F32)
    lamt = sb("lamt", [128, 256], F32)
    lams = sb("lams", [128, 8], F32)
    cT_sb = sb("cT_sb", [128, 8, 3], F32)
    silT = sb("silT", [128, 8, 3], BF16)
    modT = sb("modT", [128, 16, 3], F32)
    Mb = sb("Mb", [128, NB * NT, NE], BF16)
    idx_sb = sb("idx_sb", [128, NB * NT, 2], I32)
    wts_sb = sb("wts_sb", [128, NB * NT, 2], F32)
    idxg_sb = sb("idxg_sb", [128, NB * NT, 2], I32)
    B_const = Buf("const")
    B_lam = Buf("lam")
    B_modT = Buf("modT")
    B_Mb = [Buf("Mb%d" % i) for i in range(NB * NT)]
    B_idx = [Buf("idx%d" % i) for i in range(NB * NT)]
    B_wts = [Buf("wts%d" % i) for i in range(NB * NT)]

    P = [st.enter_context(nc.psum_tensor("P%d" % i, [128, 512], F32)) for i in range(8)]
    PB = [Buf("P%d" % i) for i in range(8)]

    ARENA_B = 199 * 1024
    arena = sb("arena", [128, ARENA_B // 2], BF16)

    def carve(off, nbytes, dt, pattern=None, **kw):
        assert off % 4 == 0 and off + nbytes <= ARENA_B, (off, nbytes)
        v = arena[:, off // 2:(off + nbytes) // 2]
        if dt == F32:
            v = v.bitcast(F32)
        elif dt == I32:
            v = v.bitcast(I32)
        if pattern:
            v = v.rearrange(pattern, **kw)
        return v

    o = 0
    wi = carve(o, 40960, BF16, "p (k n) -> p k n", k=8); o += 40960
    ropec = carve(o, 4096, F32, "p (n d) -> p n d", n=16); o += 4096
    ropes = carve(o, 4096, F32, "p (n d) -> p n d", n=16); o += 4096
    sglg = carve(o, 2048, F32); o += 2048
    sglb = carve(o, 2048, F32); o += 2048
    sgwT = carve(o, 1024, BF16, "p (g q) -> p g q", g=4); o += 1024
    sgb = carve(o, 1024, BF16); o += 1024
    ln1g = carve(o, 4096, F32); o += 4096
    ln1b = carve(o, 4096, F32); o += 4096
    g1b = carve(o, 4096, F32); o += 4096
    opsc2 = carve(o, 4096, F32); o += 4096
    sh2b = carve(o, 4096, F32); o += 4096
    hcT = carve(o, 4096, BF16, "p (k n) -> p k n", k=8)
    qtm2, ktm2, vln2 = carve(o, 1024, BF16), carve(o + 1024, 1024, BF16), carve(o + 2048, 1024, BF16)
    o += 4096
    mixT = carve(o, 32768, BF16, "p (k n) -> p k n", k=8); o += 32768
    O_ATT = o
    qT = carve(o, 16384, BF16, "p (k n) -> p k n", k=4); o += 16384
    kT = carve(o, 18432, BF16, "p (k n) -> p k n", k=4); o += 18432
    Vaug = carve(o, 18720, BF16, "p (t h e) -> p t h e", t=NKT, h=4); o += 18944
    O_A = o
    xl = [carve(o + i * 4096, 4096, F32) for i in range(2)]; o += 8192
    hTg = carve(o, 8192, BF16, "p (k n) -> p k n", k=8); o += 8192
    uT = carve(o, 4096, BF16, "p (k n) -> p k n", k=4); o += 4096
    gv = carve(o, 8192, F32, "p (j n) -> p j n", j=4); o += 8192
    r1 = carve(o, 2048, F32); o += 2048
    r2 = carve(o, 2048, F32); o += 2048
    qtm = carve(o, 1024, BF16); o += 1024
    ktm = carve(o, 1024, BF16); o += 1024
    vln = carve(o, 1024, BF16); o += 1024
    st6 = carve(o, 192, F32, "p (j s) -> p j s", j=8); o += 192
    mvs = carve(o, 64, F32, "p (j s) -> p j s", j=8); o += 64
    rst = carve(o, 64, F32); o += 64
    MIX_END = o
    assert MIX_END <= ARENA_B, MIX_END
    o = O_A
    PT = [carve(o + i * 1024, 1024, BF16) for i in range(4)]; o += 4096
    osb = [carve(o + i * 4224, 4128, F32, "p (m q e) -> p m q e", m=2, q=4) for i in range(2)]; o += 8448
    obuf = carve(o, 2048, F32, "p (q e) -> p q e", q=4); o += 2048
    sqb = carve(o, 2048, F32, "p (q e) -> p q e", q=4); o += 2048
    datm = carve(o, 1024, BF16, "p (q e) -> p q e", q=4); o += 1024
    rsb = carve(o, 64, F32); o += 64
    lnt = carve(o, 64, F32); o += 64
    assert o <= MIX_END
    ATT_TMP = 17792
    wout = carve(O_A + ATT_TMP, 16384, BF16, "p (k n) -> p k n", k=8)
    assert O_A + ATT_TMP + 16384 <= MIX_END
    RG = 8
    rtb = carve(O_A, 2240 * 4, F32)
    assert 2240 * 4 <= ATT_TMP
    o = O_ATT
    xr = [carve(o + i * 4096, 4096, F32) for i in range(2)]; o += 8192
    zt = [carve(o + i * 4096, 4096, F32) for i in range(3)]; o += 12288
    x1t = [carve(o + i * 4096, 4096, F32) for i in range(2)]; o += 8192
    tt = [carve(o + i * 4096, 4096, F32) for i in range(2)]; o += 8192
    tb = [carve(o + i * 2048, 2048, BF16) for i in range(2)]; o += 4096
    tT = carve(o, 4096, F32, "p (k n) -> p k n", k=8); o += 4096
    lgall = carve(o, NT * 36 * 4, F32, "p (n c) -> p n c", n=NT); o += NT * 36 * 4
    st6d = [carve(o + i * 64, 48, F32, "p (j s) -> p j s", j=2) for i in range(3)]; o += 192
    mvd = [carve(o + i * 64, 64, F32) for i in range(3)]; o += 192
    assert o <= O_A, o
    tbl = [mixT[:, c_, 0:1024] for c_ in range(4)]
    o = 0
    wgS = [carve(o + i * 16384, 16384, BF16, "p (k n) -> p k n", k=8) for i in range(2)]; o += 32768
    wuS = [carve(o + i * 16384, 16384, BF16, "p (k n) -> p k n", k=8) for i in range(2)]; o += 32768
    wdS = [carve(o + i * 16384, 16384, BF16, "p (k n) -> p k n", k=8) for i in range(2)]; o += 32768
    O_WOV = o
    wov = [carve(o, 49152, BF16, "p (k n) -> p k n", k=24)]; o += 49152
    XTm = [carve(o + i * 8192, 8192, BF16, "p (k n) -> p k n", k=8) for i in range(2)]; o += 16384
    XTo = carve(o, 4096, BF16, "p (k n) -> p k n", k=8); o += 4096
    hidT = carve(o, 8192, BF16, "p (k n) -> p k n", k=8); o += 8192
    xg = [carve(o + i * 2048, 2048, BF16) for i in range(4)]; o += 8192
    sgt = [carve(o + i * 2048, 2048, F32) for i in range(2)]; o += 4096
    ysb = [carve(o + i * 4096, 4096, F32) for i in range(2)]; o += 8192
    tbo = [carve(o + i * 2048, 2048, BF16) for i in range(2)]; o += 4096
    assert o <= ARENA_B, o
    o = 0
    g2bt = [carve(o + i * 4096, 4096, F32) for i in range(2)]; o += 8192
    ln2g = carve(o, 4096, F32); o += 4096
    ln2b = carve(o, 4096, F32); o += 4096
    Y1 = [carve(o + i * 4096, 4096, F32) for i in range(4)]; o += 16384
    Y2 = [carve(o + i * 4096, 4096, F32) for i in range(4)]; o += 16384
    x1r = [carve(o + i * 4096, 4096, F32) for i in range(4)]; o += 16384
    fst = [carve(o + i * 128, 128, F32) for i in range(2)]; o += 256
    assert o <= 98304, o

    B_wi, B_rope, B_sgc, B_ln1, B_bc, B_hcT = Buf("wi"), Buf("rope"), Buf("sgc"), Buf("ln1"), Buf("bc"), Buf("hcT")
    S.dma("sp", lambda e: e.dma_start(out=ident_f[:], in_=ident_d), writes=[B_const])
    S.dma("sp", lambda e: e.dma_start(out=cT_sb[:], in_=cT_d), writes=[B_const])
    S.dma("sp", lambda e: e.dma_start(out=rw_sb[:], in_=rw_d.rearrange("(k p) n -> p k n", p=128)), writes=[B_const])
    S.dma("sp", lambda e: e.dma_start(out=rb_b[:], in_=rb_d.partition_broadcast(128)), writes=[B_const])
    S.dma("sp", lambda e: e.dma_start(out=econst[:].rearrange("p a e -> p (a e)"), in_=econst_d.partition_broadcast(128)), writes=[B_const])
    S.dma("sp", lambda e: e.dma_start(out=gvec[:], in_=subg_d.partition_broadcast(128)), writes=[B_const])
    S.dma("sp", lambda e: e.dma_start(out=lamt[:], in_=lamv_d.partition_broadcast(128)), writes=[B_const])
    S.dma("pool", lambda e: e.dma_start(out=ustr_b[:], in_=ustr_d), writes=[B_const])
    S.op("dve", lambda e: e.tensor_copy(ident_b[:], ident_f[:]), reads=[B_const], writes=[B_const])
    S.op("dve", lambda e: e.memset(ones_b[:], 1.0), writes=[B_const])
    S.op("dve", lambda e: e.tensor_scalar_mul(gvec[:], gvec[:], 1.0 - LAM_INIT), reads=[B_const], writes=[B_const])
    S.op("dve", lambda e: e.tensor_tensor(lamt[:, 0:64], lamt[:, 0:64], lamt[:, 64:128], ALU.mult), reads=[B_const], writes=[B_lam])
    S.op("dve", lambda e: e.tensor_tensor(lamt[:, 128:192], lamt[:, 128:192], lamt[:, 192:256], ALU.mult), reads=[B_const, B_lam], writes=[B_lam])
    S.op("dve", lambda e: e.memset(lams[:], 0.0), writes=[B_lam])
    S.op("dve", lambda e: e.reduce_sum(lams[:, 0:1], lamt[:, 0:64], axis=AX.X), reads=[B_lam], writes=[B_lam])
    S.op("dve", lambda e: e.reduce_sum(lams[:, 1:2], lamt[:, 128:192], axis=AX.X), reads=[B_lam], writes=[B_lam])
    S.op("act", lambda e: e.activation(out=lams[:, 2:4], in_=lams[:, 0:2], func=AF.Exp), reads=[B_lam], writes=[B_lam])
    S.op("dve", lambda e: e.tensor_tensor(lams[:, 4:5], lams[:, 2:3], lams[:, 3:4], ALU.subtract), reads=[B_lam], writes=[B_lam])
    S.op("dve", lambda e: e.tensor_scalar(lams[:, 5:6], lams[:, 4:5], LAM_INIT, -1.0, ALU.add, ALU.mult), reads=[B_lam], writes=[B_lam])
    S.dma("sp", lambda e: e.dma_start(out=ropec, in_=ropec_d.rearrange("(n p) d -> p n d", p=128)), writes=[B_rope])
    S.dma("sp", lambda e: e.dma_start(out=ropes, in_=ropes_d.rearrange("(n p) d -> p n d", p=128)), writes=[B_rope])
    S.dma("sp", lambda e: e.dma_start(out=sglg, in_=sglg_d.partition_broadcast(128)), writes=[B_sgc])
    S.dma("sp", lambda e: e.dma_start(out=sglb, in_=sglb_d.partition_broadcast(128)), writes=[B_sgc])
    S.dma("pool", lambda e: e.dma_start(out=sgwT, in_=sgwT_d.rearrange("g q p -> q g p")), writes=[B_sgc])
    S.dma("pool", lambda e: e.dma_start(out=sgb[0:1, :], in_=sgb_d), writes=[B_sgc])
    S.dma("sp", lambda e: e.dma_start(out=ln1g, in_=ln1g_d.partition_broadcast(128)), writes=[B_ln1])
    S.dma("sp", lambda e: e.dma_start(out=ln1b, in_=ln1b_d.partition_broadcast(128)), writes=[B_ln1])

    S.op("act", lambda e: e.activation(out=silT[:], in_=cT_sb[:], func=AF.Silu), reads=[B_const], writes=[B_const])
    wm = [xl[0].bitcast(BF16).rearrange("p (k n) -> p k n", k=8)[:, :, 0:256],
          xl[1].bitcast(BF16).rearrange("p (k n) -> p k n", k=8)[:, :, 0:256]]
    B_wm = [Buf("wm0"), Buf("wm1")]
    bm3 = [gv[0:3, 0, 0:256], gv[0:3, 1, 0:256]]
    mrow = [gv[0:3, 2, 0:256], gv[0:3, 3, 0:256]]
    B_bm3 = [Buf("bm0"), Buf("bm1")]
    B_mrow = [Buf("mr0"), Buf("mr1")]
    def mod_dma(nb, wmv, bmv, Bw, Bb):
        cs = slice(nb * 256, (nb + 1) * 256)
        S.dma("pool", lambda e: e.dma_start(out=wmv, in_=wmod_d[:, cs].rearrange("(k p) n -> p k n", p=128)), writes=[Bw])
        S.dma("sp", lambda e: e.dma_start(out=bmv, in_=bmod_d[:, cs].partition_broadcast(3)), writes=[Bb])

    def mod_compute(nb, wmv, bmv, mrv, Bw, Bb, Bm, bank):
        cs = slice(nb * 256, (nb + 1) * 256)
        for k in range(8):
            S.op("pe", lambda e, k=k: e.matmul(P[bank][0:3, 0:256], lhsT=silT[:, k, :], rhs=wmv[:, k, :], start=(k == 0), stop=(k == 7)),
                 reads=[B_const, Bw], writes=[PB[bank]])
        S.op("dve", lambda e: e.tensor_tensor(mrv, P[bank][0:3, 0:256], bmv, ALU.add), reads=[PB[bank], Bb], writes=[Bm])
        S.dma("sp", lambda e: e.dma_start(out=modd[:, cs], in_=mrv), reads=[Bm], writes=[b_modd])

    def mod_block(nb, wmv, bmv, mrv, Bw, Bb, Bm, bank):
        mod_dma(nb, wmv, bmv, Bw, Bb)
        mod_compute(nb, wmv, bmv, mrv, Bw, Bb, Bm, bank)

    wm4 = wm + [hTg[:, 0:4, :].rearrange("p k n -> p (k n)").rearrange("p (k n) -> p k n", k=8), hTg[:, 4:8, :].rearrange("p k n -> p (k n)").rearrange("p (k n) -> p k n", k=8)]
    bm4 = bm3 + [r1[0:3, 0:256], r1[0:3, 256:512]]
    mr4 = mrow + [r2[0:3, 0:256], r2[0:3, 256:512]]
    B_wm4 = B_wm + [Buf("wm2"), Buf("wm3")]
    B_bm4 = B_bm3 + [Buf("bm2"), Buf("bm3")]
    B_mr4 = B_mrow + [Buf("mr2"), Buf("mr3")]
    for nb in range(4):
        mod_dma(nb, wm4[nb], bm4[nb], B_wm4[nb], B_bm4[nb])
    for nb in range(8):
        i = nb % 4
        mod_compute(nb, wm4[i], bm4[i], mr4[i], B_wm4[i], B_bm4[i], B_mr4[i], i)
        if nb + 4 < 8:
            mod_dma(nb + 4, wm4[i], bm4[i], B_wm4[i], B_bm4[i])
    for hh in range(4):
        S.dma("pool", lambda e, hh=hh: e.dma_start(out=wi[:, :, hh * 640:(hh + 1) * 640], in_=win_d[:, hh * 640:(hh + 1) * 640].rearrange("(k p) n -> p k n", p=128)), writes=[B_wi])
    B_xl = [Buf("xl%d" % i) for i in range(2)]
    for i in range(2):
        Sched.alias(B_xl[i], [B_wm[i]])
    B_hTg, B_uT, B_gv = Buf("hTg"), Buf("uT"), Buf("gv")
    Sched.alias(B_gv, B_bm3 + B_mrow)
    gvflat = gv.rearrange("p j n -> p (j n)")
    S.dma("sp", lambda e: e.dma_start(out=gvflat[0:3, :], in_=modd[:, 0:2048]), reads=[b_modd], writes=[B_gv])
    for j in range(16):
        S.op("pe", lambda e, j=j: e.transpose(P[0][:, j * 3:(j + 1) * 3], gvflat[0:3, j * 128:(j + 1) * 128], ident_f[0:3, 0:3]), reads=[B_gv, B_const], writes=[PB[0]])
    S.op("dve", lambda e: e.tensor_copy(modT[:], P[0][:, 0:48].rearrange("p (j v) -> p j v", j=16)), reads=[PB[0]], writes=[B_modT])
    S.op("dve", lambda e: e.tensor_scalar_add(modT[:, 8:16, :], modT[:, 8:16, :], 1.0), reads=[B_modT], writes=[B_modT])
    B_r1, B_r2, B_qtm, B_ktm, B_vln, B_st = Buf("r1"), Buf("r2"), Buf("qtm"), Buf("ktm"), Buf("vln"), Buf("st")
    B_mixT = [Buf("mixT%d" % i) for i in range(NT)]
    B_qT = [Buf("qT%d" % i) for i in range(NT)]
    B_kT = [Buf("kT%d" % i) for i in range(NKT)]
    B_V = [Buf("V%d" % i) for i in range(NKT)]
    B_qtm2 = [B_qtm, Buf("qtm2")]
    B_ktm2 = [B_ktm, Buf("ktm2")]
    B_vln2 = [B_vln, Buf("vln2")]
    Sched.alias(B_hTg, [B_wm4[2], B_wm4[3]])
    Sched.alias(B_r1, [B_bm4[2], B_bm4[3]])
    Sched.alias(B_r2, [B_mr4[2], B_mr4[3]])
    B_regA = [B_xl[0], B_xl[1], B_hTg, B_uT, B_gv, B_r1, B_r2, B_qtm, B_ktm, B_vln, B_st]
    xl_ctr = [0]
    pb_ctr = [0]

    def next_bank(lo=2, n=6):
        i = lo + pb_ctr[0] % n
        pb_ctr[0] += 1
        return i

    P_bf = [P[i][:].bitcast(BF16) for i in range(8)]
    tile_ctr = [0]

    B_tbd_all = []
    B_wg, B_wu, B_wd = [Buf("wg0"), Buf("wg1")], [Buf("wu0"), Buf("wu1")], [Buf("wd0"), Buf("wd1")]
    early = set()
    for b in range(NB):
        def vcol(v):
            return (lambda k: modT[:, 8 + k, v:v + 1]), (lambda k: modT[:, k, v:v + 1])

        S.op("pool", lambda e: e.memset(Vaug[:, :, :, 128:130], 1.0), reads=[], writes=B_V)

        def proj_block(lhs_fn, lhs_bufs, cols, bank):
            for k in range(8):
                S.op("pe", lambda e, k=k: e.matmul(P[bank][:, :], lhsT=lhs_fn(k), rhs=wi[:, k, cols], start=(k == 0), stop=(k == 7)),
                     reads=lhs_bufs + [B_wi], writes=[PB[bank]])

        def rope_block(bank, n, dst, dstbuf):
            pv = P[bank][:, :]
            cosb = ropec[:, n, :].unsqueeze(1).to_broadcast([128, 8, 64])
            S.op("dve", lambda e: e.tensor_tensor(r1.rearrange("p (a d) -> p a d", a=8), pv.rearrange("p (a d) -> p a d", a=8), cosb, ALU.mult),
                 reads=[PB[bank], B_rope], writes=[B_r1])
            p4 = pv.rearrange("p (a x h) -> p a x h", a=8, x=2)
            r24 = r2.rearrange("p (a x h) -> p a x h", a=8, x=2)
            s4 = ropes[:, n, :].rearrange("p (x h) -> p x h", x=2).unsqueeze(1).to_broadcast([128, 8, 2, 32])
            S.op("dve", lambda e: e.tensor_tensor(r24[:, :, :, 0:16], p4[:, :, :, 16:32], s4[:, :, :, 0:16], ALU.mult),
                 reads=[PB[bank], B_rope], writes=[B_r2])
            S.op("dve", lambda e: e.tensor_tensor(r24[:, :, :, 16:32], p4[:, :, :, 0:16], s4[:, :, :, 16:32], ALU.mult),
                 reads=[PB[bank], B_rope], writes=[B_r2])
            S.op("dve", lambda e: e.tensor_tensor(dst, r1, r2, ALU.add), reads=[B_r1, B_r2], writes=[dstbuf])

        def to_featmajor(src, srcbuf, dstT, col0, dstbuf, scale):
            tb_ = next_bank()
            for c in range(4):
                S.op("pe", lambda e, c=c: e.transpose(P_bf[tb_][:, c * 128:(c + 1) * 128], src[:, c * 128:(c + 1) * 128], ident_b[:]),
                     reads=[srcbuf, B_const], writes=[PB[tb_]])
            S.op("act", lambda e: e.activation(out=dstT[:, :, col0:col0 + 128], in_=P_bf[tb_][:, 0:512].rearrange("p (c n) -> p c n", c=4),
                                               func=AF.Copy, scale=scale), reads=[PB[tb_]], writes=[dstbuf])

        Sched.alias(B_hcT, [B_qtm2[1], B_ktm2[1], B_vln2[1]])
        scf, shf = vcol(2)
        for j in range(2):
            xi = xl_ctr[0] % 2; xl_ctr[0] += 1
            S.dma("sp", lambda e, j=j, xi=xi: e.dma_start(out=xl[xi], in_=ctx_d[b, j * 128:(j + 1) * 128, :]), writes=[B_xl[xi]])
            for k in range(8):
                bank = k % 2
                S.op("pe", lambda e, k=k, xi=xi, bank=bank: e.transpose(P[bank][:, 0:128], xl[xi][:, k * 128:(k + 1) * 128], ident_f[:]),
                     reads=[B_xl[xi], B_const], writes=[PB[bank]])
                S.op("act", lambda e, k=k, j=j, bank=bank: e.activation(out=hcT[:, k, j * 128:(j + 1) * 128], in_=P[bank][:, 0:128], func=AF.Identity,
                                                                         scale=scf(k), bias=shf(k)), reads=[PB[bank], B_modT], writes=[B_hcT])
        for j in range(2):
            bank = next_bank()
            proj_block(lambda k, j=j: hcT[:, k, j * 128:(j + 1) * 128], [B_hcT], slice(512, 1024), bank)
            S.op("act", lambda e, bank=bank: e.activation(out=ktm, in_=P[bank][:, :], func=AF.Copy), reads=[PB[bank]], writes=[B_ktm])
            to_featmajor(ktm, B_ktm, kT, L + j * 128, B_kT[16 + j], 1.0)
            bank = next_bank()
            proj_block(lambda k, j=j: hcT[:, k, j * 128:(j + 1) * 128], [B_hcT], slice(1024, 1536), bank)
            S.op("act", lambda e, bank=bank, j=j: e.activation(out=Vaug[:, 16 + j, :, 0:128], in_=P[bank][:, :].rearrange("p (h e) -> p h e", h=4), func=AF.Copy),
                 reads=[PB[bank]], writes=[B_V[16 + j]])

        if b == 0:
            wm_l = [mixT[:, i, :].rearrange("p (k n) -> p k n", k=8) for i in range(3)]
            sm_l = [mixT[0:3, 3, i * 512:(i + 1) * 512].bitcast(F32) for i in range(3)]
            B_wml = [Buf("wml%d" % i) for i in range(3)]
            B_sml = [Buf("sml%d" % i) for i in range(3)]
            late = list(range(8, 24))
            late_dma = list(range(8, 24))

            def late_dma_next():
                if late_dma:
                    nb2 = late_dma.pop(0)
                    i2 = nb2 % 3
                    mod_dma(nb2, wm_l[i2], sm_l[i2], B_wml[i2], B_sml[i2])
            late_dma_next()
            late_dma_next()
        for nb_ in (B_qtm2[1], B_ktm2[1], B_vln2[1]):
            Sched.alias(nb_, [B_hcT])
        scf, shf = vcol(b)
        for g in range(4):
            xis = []
            for j in range(4):
                n = g * 4 + j
                xi = xl_ctr[0] % 2; xl_ctr[0] += 1
                xis.append(xi)
                S.dma("sp", lambda e, n=n, xi=xi: e.dma_start(out=xl[xi], in_=x_d[b, n * 128:(n + 1) * 128, :]), writes=[B_xl[xi]])
                for k in range(8):
                    bank = k // 4
                    S.op("pe", lambda e, k=k, xi=xi, bank=bank: e.transpose(P[bank][:, (k % 4) * 128:(k % 4 + 1) * 128], xl[xi][:, k * 128:(k + 1) * 128], ident_f[:]),
                         reads=[B_xl[xi], B_const], writes=[PB[bank]])
                    if k % 4 == 3:
                        for kk in range(k - 3, k + 1):
                            S.op("act", lambda e, kk=kk, j=j, bank=bank: e.activation(out=hTg[:, kk, j * 128:(j + 1) * 128], in_=P[bank][:, (kk % 4) * 128:(kk % 4 + 1) * 128],
                                                                                      func=AF.Identity, scale=scf(kk), bias=shf(kk)),
                                 reads=[PB[bank], B_modT], writes=[B_hTg])
            for c in range(4):
                bank = next_bank()
                for k in range(8):
                    S.op("pe", lambda e, k=k, c=c, bank=bank: e.matmul(P[bank][:, :], lhsT=wi[:, k, 1536 + c * 128:1536 + (c + 1) * 128], rhs=hTg[:, k, :],
                                                                       start=(k == 0), stop=(k == 7)), reads=[B_wi, B_hTg], writes=[PB[bank]])
                S.op("act", lambda e, c=c, bank=bank: e.activation(out=uT[:, c, :], in_=P[bank][:, :], func=AF.Gelu), reads=[PB[bank]], writes=[B_uT])
            for j in range(4):
                bank = next_bank()
                proj_block(lambda k, j=j: hTg[:, k, j * 128:(j + 1) * 128], [B_hTg], slice(2048, 2560), bank)
                S.op("act", lambda e, j=j, bank=bank: e.activation(out=gv[:, j, :], in_=P[bank][:, :], func=AF.Gelu), reads=[PB[bank]], writes=[B_gv])
                S.op("dve", lambda e, j=j: e.bn_stats(st6[:, j, :], gv[:, j, :]), reads=[B_gv], writes=[B_st])
                S.op("dve", lambda e, j=j: e.bn_aggr(mvs[:, j, :], st6[:, j, :]), reads=[B_st], writes=[B_st])
            S.op("act", lambda e: e.activation(out=rst[:, 0:4], in_=mvs[:, 0:4, 1], func=AF.Ln, bias=EPS), reads=[B_st], writes=[B_st])
            S.op("act", lambda e: e.activation(out=rst[:, 4:8], in_=rst[:, 0:4], func=AF.Exp, scale=-0.5), reads=[B_st], writes=[B_st])
            qtms, ktms, vlns = [qtm, qtm2], [ktm, ktm2], [vln, vln2]

            def stage1(j):
                n = g * 4 + j
                d = n % 2
                lhs = lambda k: hTg[:, k, j * 128:(j + 1) * 128]
                bq = next_bank()
                proj_block(lhs, [B_hTg], slice(0, 512), bq)
                bk = next_bank()
                proj_block(lhs, [B_hTg], slice(512, 1024), bk)
                bv = next_bank()
                proj_block(lhs, [B_hTg], slice(1024, 1536), bv)
                rope_block(bq, n, qtms[d], B_qtm2[d])
                rope_block(bk, n, ktms[d], B_ktm2[d])
                S.op("act", lambda e: e.activation(out=Vaug[:, n, :, 0:128], in_=P[bv][:, :].rearrange("p (h e) -> p h e", h=4), func=AF.Copy),
                     reads=[PB[bv]], writes=[B_V[n]])
                S.op("dve", lambda e: e.tensor_scalar(gv[:, j, :], gv[:, j, :], mvs[:, j, 0:1], rst[:, 4 + j:5 + j], ALU.subtract, ALU.mult),
                     reads=[B_gv, B_st], writes=[B_gv])
                S.op("pool", lambda e: e.tensor_tensor(gv[:, j, :], gv[:, j, :], sglg, ALU.mult), reads=[B_gv, B_sgc], writes=[B_gv])
                S.op("pool", lambda e: e.tensor_tensor(vlns[d], gv[:, j, :], sglb, ALU.add), reads=[B_gv, B_sgc], writes=[B_vln2[d]])

            def stage2(j):
                n = g * 4 + j
                d = n % 2
                to_featmajor(qtms[d], B_qtm2[d], qT, n * 128, B_qT[n], 0.125)
                to_featmajor(ktms[d], B_ktm2[d], kT, n * 128, B_kT[n], 1.0)
                bank = next_bank()
                for gg in range(4):
                    S.op("pe", lambda e, gg=gg: e.matmul(P[bank][:, gg * 128:(gg + 1) * 128], lhsT=vlns[d][:, gg * 128:(gg + 1) * 128], rhs=sgwT[:, gg, :],
                                                         start=True, stop=False), reads=[B_vln2[d], B_sgc], writes=[PB[bank]])
                    S.op("pe", lambda e, gg=gg: e.matmul(P[bank][:, gg * 128:(gg + 1) * 128], lhsT=ones_b[0:1, :], rhs=sgb[0:1, gg * 128:(gg + 1) * 128],
                                                         start=False, stop=True), reads=[B_const, B_sgc], writes=[PB[bank]])
                S.op("dve", lambda e: e.tensor_tensor(mixT[:, 4:8, n * 128:(n + 1) * 128], P[bank][:, :].rearrange("p (g q) -> p g q", g=4),
                                                      uT[:, :, j * 128:(j + 1) * 128], ALU.mult),
                     reads=[PB[bank], B_uT], writes=[B_mixT[n]])

            for j in range(4):
                stage1(j)
                if j > 0:
                    stage2(j - 1)
                if b == 0 and late:
                    nb = late.pop(0)
                    i = nb % 3
                    mod_compute(nb, wm_l[i], sm_l[i], sm_l[i], B_wml[i], B_sml[i], B_sml[i], next_bank())
                    late_dma_next()
            stage2(3)
            if b == 0 and g == 3:
                for mb_ in B_mixT:
                    Sched.alias(mb_, B_wml + B_sml)

        B_PT = [Buf("PT%d" % i) for i in range(4)]
        B_osb = [Buf("osb0"), Buf("osb1")]
        B_ob, B_sq, B_da, B_rs = Buf("obuf"), Buf("sqb"), Buf("datm"), Buf("rsb")
        for nb_ in B_PT + B_osb + [B_ob, B_sq, B_da, B_rs]:
            Sched.alias(nb_, B_regA)
        tmpA = carve(O_A + 4096 + 8448, 4096, F32)
        B_tmpA = Buf("tmpA")
        Sched.alias(B_tmpA, B_regA)
        S.dma("sp", lambda e, b=b: e.dma_start(out=g1b, in_=modd[b:b + 1, 2048:3072].partition_broadcast(128)), reads=[b_modd], writes=[B_bc])
        S.dma("sp", lambda e, b=b: e.dma_start(out=sh2b, in_=modd[b:b + 1, 3072:4096].partition_broadcast(128)), reads=[b_modd], writes=[B_bc])
        S.dma("sp", lambda e, b=b: e.dma_start(out=opsc2, in_=modd[b:b + 1, 4096:5120].partition_broadcast(128)), reads=[b_modd], writes=[B_bc])
        S.op("pool", lambda e: e.tensor_scalar_add(opsc2, opsc2, 1.0), reads=[B_bc], writes=[B_bc])

        S.op("pool", lambda e: e.tensor_tensor(tmpA, ln1b, opsc2, ALU.mult), reads=[B_ln1, B_bc], writes=[B_tmpA])
        S.op("pool", lambda e: e.tensor_tensor(sh2b, sh2b, tmpA, ALU.add), reads=[B_tmpA, B_bc], writes=[B_bc])
        S.op("pool", lambda e: e.tensor_tensor(opsc2, opsc2, ln1g, ALU.mult), reads=[B_ln1, B_bc], writes=[B_bc])

        Sched.alias(B_ob, [B_tmpA])
        Sched.alias(B_sq, [B_tmpA])
        if b == NB - 1:
            Sched.alias(B_wg[0], [B_wi])
            Sched.alias(B_wg[1], [B_wi])
            Sched.alias(B_wu[0], [B_wi, B_rope])
            S.dma("pool", lambda e: e.dma_start(out=wgS[0], in_=wall_d[0, 0].rearrange("(k p) n -> p k n", p=128)), writes=[B_wg[0]])
            S.dma("pool", lambda e: e.dma_start(out=wuS[0], in_=wall_d[0, 1].rearrange("(k p) n -> p k n", p=128)), writes=[B_wu[0]])
            S.dma("pool", lambda e: e.dma_start(out=wgS[1], in_=wall_d[1, 0].rearrange("(k p) n -> p k n", p=128)), writes=[B_wg[1]])
            early.update([(0, 0), (0, 1), (1, 0)])
        B_wout = Buf("wout")
        Sched.alias(B_wout, B_regA)
        S.dma("pool", lambda e: e.dma_start(out=wout, in_=wout_d.rearrange("(k p) n -> p k n", p=128)), writes=[B_wout])
        pairs = [(hp, qb, m, kt) for hp in range(2) for qb in range(4) for m in range(2) for kt in range(NKT)]
        NI = len(pairs)
        SBK = [(0, 1), (6, 7)]

        def emit_S(i):
            hp, qb, m, kt = pairs[i]
            ch = m * 2 + hp
            qs0 = qb * 512
            for hh in range(2):
                r0 = hh * 64
                sb_ = SBK[i % 2][hh]
                S.op("pe", lambda e: e.matmul(P[sb_][:, :], lhsT=kT[r0:r0 + 64, ch, kt * 128:(kt + 1) * 128],
                                              rhs=qT[r0:r0 + 64, ch, qs0:qs0 + 512], start=True, stop=True),
                     reads=[B_kT[kt]] + B_qT[qb * 4:qb * 4 + 4], writes=[PB[sb_]])
            for hh in range(2):
                sb_ = SBK[i % 2][hh]
                pt = (i % 2) * 2 + hh
                S.op("act", lambda e: e.activation(out=PT[pt], in_=P[sb_][:, :], func=AF.Exp), reads=[PB[sb_]], writes=[B_PT[pt]])

        def emit_AV(i):
            hp, qb, m, kt = pairs[i]
            for hh in range(2):
                pt = (i % 2) * 2 + hh
                for qs in range(4):
                    ob = 2 + hh * 2 + qs // 2
                    S.op("pe", lambda e, qs=qs, ob=ob: e.matmul(P[ob][:, (qs % 2) * 129:(qs % 2) * 129 + 129], lhsT=PT[pt][:, qs * 128:(qs + 1) * 128],
                                                                rhs=Vaug[:, kt, 2 * hp + hh, 0:129], start=(kt == 0), stop=(kt == NKT - 1)),
                         reads=[B_PT[pt], B_V[kt]], writes=[PB[ob]])

        def evac(m):
            for hh in range(2):
                for hb in range(2):
                    ob = 2 + hh * 2 + hb
                    S.op("dve", lambda e, hh=hh, hb=hb, ob=ob: e.tensor_copy(osb[hh][:, m, hb * 2:hb * 2 + 2, :], P[ob][:, 0:258].rearrange("p (q e) -> p q e", q=2)),
                         reads=[PB[ob]], writes=[B_osb[hh]])

        def post_A(h, qb):
            oi = h % 2
            osv = osb[oi]
            S.op("dve", lambda e: e.reciprocal(rsb[:, 0:8].rearrange("p (m q) -> p m q", m=2), osv[:, :, :, 128]), reads=[B_osb[oi]], writes=[B_rs])
            S.op("dve", lambda e: e.tensor_scalar_mul(rsb[:, 8:12], rsb[:, 4:8], lams[:, 5:6]), reads=[B_rs, B_lam], writes=[B_rs])
            for qs in range(4):
                S.op("dve", lambda e, qs=qs: e.tensor_scalar_mul(obuf[:, qs, :], osv[:, 0, qs, 0:128], rsb[:, qs:qs + 1]), reads=[B_osb[oi], B_rs], writes=[B_ob])
                S.op("dve", lambda e, qs=qs: e.scalar_tensor_tensor(obuf[:, qs, :], osv[:, 1, qs, 0:128], rsb[:, 8 + qs:9 + qs], obuf[:, qs, :], ALU.mult, ALU.add),
                     reads=[B_osb[oi], B_rs, B_ob], writes=[B_ob])
            S.op("pool", lambda e: e.tensor_tensor(sqb, obuf, obuf, ALU.mult), reads=[B_ob], writes=[B_sq])
            S.op("dve", lambda e: e.reduce_sum(rsb[:, 12:16], sqb, axis=AX.X), reads=[B_sq], writes=[B_rs])

        def post_B(h, qb):
            S.op("act", lambda e: e.activation(out=lnt[:, 0:4], in_=rsb[:, 12:16], func=AF.Ln, scale=1.0 / 128.0, bias=EPS), reads=[B_rs], writes=[B_rs])
            S.op("act", lambda e: e.activation(out=lnt[:, 4:8], in_=lnt[:, 0:4], func=AF.Exp, scale=-0.5), reads=[B_rs], writes=[B_rs])
            for qs in range(4):
                S.op("dve", lambda e, qs=qs: e.scalar_tensor_tensor(datm[:, qs, :], obuf[:, qs, :], lnt[:, 4 + qs:5 + qs], gvec[:], ALU.mult, ALU.mult),
                     reads=[B_ob, B_rs, B_const], writes=[B_da])

        def post_C(h, qb):
            qs0 = qb * 512
            for qs in range(4):
                S.op("pe", lambda e, qs=qs: e.transpose(P_bf[6][:, qs * 128:(qs + 1) * 128], datm[:, qs, :], ident_b[:]), reads=[B_da, B_const], writes=[PB[6]])
            S.op("dve", lambda e: e.tensor_copy(mixT[:, h, qs0:qs0 + 512], P_bf[6][:, 0:512]), reads=[PB[6]], writes=B_mixT[qb * 4:qb * 4 + 4])

        pending = []
        emit_S(0)
        for i in range(NI):
            if i + 1 < NI:
                emit_S(i + 1)
            emit_AV(i)
            hp, qb, m, kt = pairs[i]
            if kt == NKT - 1:
                evac(m)
                if m == 1:
                    pending.append((i + 1, post_A, 2 * hp, qb))
                    pending.append((i + 4, post_B, 2 * hp, qb))
                    pending.append((i + 7, post_C, 2 * hp, qb))
                    pending.append((i + 8, post_A, 2 * hp + 1, qb))
                    pending.append((i + 11, post_B, 2 * hp + 1, qb))
                    pending.append((i + 14, post_C, 2 * hp + 1, qb))
            while pending and pending[0][0] <= i:
                _, fn_, h_, qb_ = pending.pop(0)
                fn_(h_, qb_)
        for _, fn_, h_, qb_ in pending:
            fn_(h_, qb_)

        B_tT, B_lg, B_rtb = Buf("tT"), Buf("lg"), Buf("rtb")
        B_std = [Buf("std%d" % i) for i in range(3)]
        B_xr, B_zt, B_x1t, B_tt, B_tb = ([Buf("xr%d" % i) for i in range(2)], [Buf("zt%d" % i) for i in range(3)],
                                         [Buf("x1t%d" % i) for i in range(2)], [Buf("tt%d" % i) for i in range(2)], [Buf("tb%d" % i) for i in range(2)])
        B_tbd = [Buf("tbd%d_%d" % (b, i)) for i in range(NT)]
        B_tbd_all.extend(B_tbd)
        B_tbl = [Buf("tbl%d" % i) for i in range(4)]
        old = B_qT + B_kT + B_V
        for nb_ in [B_tT, B_lg] + B_std + B_xr + B_zt + B_x1t + B_tt + B_tb:
            Sched.alias(nb_, old)
        Sched.alias(B_rtb, B_PT + B_osb + [B_ob, B_sq, B_da, B_rs])
        obank = {}

        def stageA1pe(n):
            w = n % 2
            S.dma("sp", lambda e: e.dma_start(out=xr[w], in_=x_d[b, n * 128:(n + 1) * 128, :]), writes=[B_xr[w]])
            obank[n] = []
            for hf in range(2):
                bank = next_bank(0, 4)
                obank[n].append(bank)
                for c in range(8):
                    S.op("pe", lambda e, c=c: e.matmul(P[bank][:, :], lhsT=mixT[:, c, n * 128:(n + 1) * 128], rhs=wout[:, c, hf * 512:(hf + 1) * 512],
                                                       start=(c == 0), stop=(c == 7)), reads=[B_mixT[n], B_wout], writes=[PB[bank]])

        def stageA1dve(n):
            w = n % 2
            z = n % 3
            for hf in range(2):
                bank = obank[n][hf]
                S.op("dve", lambda e: e.tensor_tensor(zt[z][:, hf * 512:(hf + 1) * 512], P[bank][:, :], g1b[:, hf * 512:(hf + 1) * 512], ALU.mult),
                     reads=[PB[bank], B_bc], writes=[B_zt[z]])
            S.op("dve", lambda e: e.scalar_tensor_tensor(zt[z], xr[w], ALPHA, zt[z], ALU.mult, ALU.add), reads=[B_xr[w], B_zt[z]], writes=[B_zt[z]])
            for hf in range(2):
                S.op("dve", lambda e, hf=hf: e.bn_stats(st6d[z][:, hf, :], zt[z][:, hf * 512:(hf + 1) * 512]), reads=[B_zt[z]], writes=[B_std[z]])
            S.op("dve", lambda e: e.bn_aggr(mvd[z][:, 0:2], st6d[z][:, :, :]), reads=[B_std[z]], writes=[B_std[z]])
            S.op("act", lambda e: e.activation(out=mvd[z][:, 2:3], in_=mvd[z][:, 1:2], func=AF.Ln, bias=EPS), reads=[B_std[z]], writes=[B_std[z]])
            S.op("act", lambda e: e.activation(out=mvd[z][:, 3:4], in_=mvd[z][:, 2:3], func=AF.Exp, scale=-0.5), reads=[B_std[z]], writes=[B_std[z]])

        def stageA2a(n):
            z = n % 3
            S.op("dve", lambda e: e.tensor_scalar(mvd[z][:, 4:5], mvd[z][:, 0:1], mvd[z][:, 3:4], -1.0, ALU.mult, ALU.mult), reads=[B_std[z]], writes=[B_std[z]])
            S.op("act", lambda e: e.activation(out=zt[z], in_=zt[z], func=AF.Identity, scale=mvd[z][:, 3:4], bias=mvd[z][:, 4:5]), reads=[B_zt[z], B_std[z]], writes=[B_zt[z]])

        def stageA2b(n):
            gi = b * NT + n
            w = n % 2
            z = n % 3
            S.op("dve", lambda e: e.tensor_tensor(x1t[w], zt[z], ln1g, ALU.mult), reads=[B_zt[z], B_ln1], writes=[B_x1t[w]])
            S.op("dve", lambda e: e.tensor_tensor(x1t[w], x1t[w], ln1b, ALU.add), reads=[B_x1t[w], B_ln1], writes=[B_x1t[w]])
            S.dma("sp", lambda e: e.dma_start(out=x1d[gi * 128:(gi + 1) * 128, :], in_=x1t[w]), reads=[B_x1t[w]], writes=[B_x1d[gi]])
            S.op("dve", lambda e: e.tensor_tensor(tt[w], zt[z], opsc2, ALU.mult), reads=[B_zt[z], B_bc], writes=[B_tt[w]])
            S.op("dve", lambda e: e.tensor_tensor(tt[w], tt[w], sh2b, ALU.add), reads=[B_tt[w], B_bc], writes=[B_tt[w]])
            S.op("act", lambda e: e.activation(out=tb[w], in_=tt[w], func=AF.Copy), reads=[B_tt[w]], writes=[B_tb[w]])
            S.dma("act", lambda e: e.dma_start(out=tbd[gi * 128:(gi + 1) * 128, :], in_=tb[w]), reads=[B_tb[w]], writes=[B_tbd[n]])
            if debug:
                S.dma("sp", lambda e: e.dma_start(out=dbg_d[gi * 128:(gi + 1) * 128, :], in_=tt[w]), reads=[B_tt[w]], writes=[B_dbg[gi]])

        def stageB(n):
            w = n % 2
            for hf in range(2):
                bank = 4 + hf
                for k4 in range(4):
                    k = hf * 4 + k4
                    S.op("pe", lambda e, k=k, k4=k4: e.transpose(P[bank][:, k4 * 128:(k4 + 1) * 128], tt[w][:, k * 128:(k + 1) * 128], ident_f[:]),
                         reads=[B_tt[w], B_const], writes=[PB[bank]])
                S.op("act", lambda e: e.activation(out=tT[:, hf * 4:hf * 4 + 4, :], in_=P[bank][:, :].rearrange("p (k n) -> p k n", k=4), func=AF.Copy),
                     reads=[PB[bank]], writes=[B_tT])
            for k in range(8):
                S.op("pe", lambda e, k=k: e.matmul(P[6][:, (n % 2) * 64:(n % 2) * 64 + 36], lhsT=tT[:, k, :], rhs=rw_sb[:, k, :], start=(k == 0), stop=(k == 7)),
                     reads=[B_tT, B_const], writes=[PB[6]])

        def lgadd(n):
            S.op("dve", lambda e: e.tensor_tensor(lgall[:, n, :], P[6][:, (n % 2) * 64:(n % 2) * 64 + 36], rb_b[:], ALU.add), reads=[PB[6], B_const], writes=[B_lg])

        def route_chunk(c):
            G = RG
            t0 = c * G
            ts = slice(t0, t0 + G)
            gis = slice(b * NT + t0, b * NT + t0 + G)
            off = [0]

            def alloc(wd):
                v = rtb[:, off[0]:off[0] + G * wd].rearrange("p (g x) -> p g x", g=G)
                off[0] += G * wd
                return v
            gmax, gone, gex, gsum, gw, pen = alloc(1), alloc(4), alloc(4), alloc(1), alloc(1), alloc(4)
            elm, m1, mk1, elm2, m2, mk2 = alloc(32), alloc(1), alloc(32), alloc(32), alloc(1), alloc(32)
            dm, ex_, den, rr, ovf, dst, prod, dsum = alloc(1), alloc(1), alloc(1), alloc(1), alloc(32), alloc(32), alloc(64), alloc(2)
            assert off[0] <= 2240
            R = [B_rtb]
            gl = lgall[:, ts, 0:4]
            el = lgall[:, ts, 4:36]
            bc = lambda v, k: v.to_broadcast([128, G, k])
            S.op("dve", lambda e: e.reduce_max(gmax[:, :, 0], gl, axis=AX.X), reads=[B_lg], writes=R)
            S.op("dve", lambda e: e.tensor_tensor(gone, gl, bc(gmax, 4), ALU.is_ge), reads=[B_lg] + R, writes=R)
            S.op("dve", lambda e: e.tensor_tensor(gex, gl, bc(gmax, 4), ALU.subtract), reads=[B_lg] + R, writes=R)
            S.op("act", lambda e: e.activation(out=gex, in_=gex, func=AF.Exp), reads=R, writes=R)
            S.op("dve", lambda e: e.reduce_sum(gsum[:, :, 0], gex, axis=AX.X), reads=R, writes=R)
            S.op("dve", lambda e: e.reciprocal(gw, gsum), reads=R, writes=R)
            S.op("dve", lambda e: e.tensor_scalar(pen, gone, BIG, -BIG, ALU.mult, ALU.add), reads=R, writes=R)
            S.op("dve", lambda e: e.tensor_tensor(elm.rearrange("p g (a x) -> p g a x", a=4), el.rearrange("p g (a x) -> p g a x", a=4),
                                                  pen.unsqueeze(3).to_broadcast([128, G, 4, 8]), ALU.add), reads=[B_lg] + R, writes=R)
            S.op("dve", lambda e: e.reduce_max(m1[:, :, 0], elm, axis=AX.X), reads=R, writes=R)
            S.op("dve", lambda e: e.tensor_tensor(mk1, elm, bc(m1, 32), ALU.is_ge), reads=R, writes=R)
            S.op("dve", lambda e: e.scalar_tensor_tensor(elm2, mk1, -BIG, elm, ALU.mult, ALU.add), reads=R, writes=R)
            S.op("dve", lambda e: e.reduce_max(m2[:, :, 0], elm2, axis=AX.X), reads=R, writes=R)
            S.op("dve", lambda e: e.tensor_tensor(mk2, elm2, bc(m2, 32), ALU.is_ge), reads=R, writes=R)
            S.op("dve", lambda e: e.tensor_tensor(dm, m2, m1, ALU.subtract), reads=R, writes=R)
            S.op("act", lambda e: e.activation(out=ex_, in_=dm, func=AF.Exp), reads=R, writes=R)
            S.op("dve", lambda e: e.tensor_scalar_add(den, ex_, 1.0), reads=R, writes=R)
            S.op("dve", lambda e: e.reciprocal(rr, den), reads=R, writes=R)
            Wb = [B_wts[gi] for gi in range(b * NT + t0, b * NT + t0 + G)]
            S.op("dve", lambda e: e.tensor_tensor(wts_sb[:, gis, 0:1], rr, gw, ALU.mult), reads=R, writes=Wb)
            S.op("dve", lambda e: e.tensor_tensor(wts_sb[:, gis, 1:2], gw, wts_sb[:, gis, 0:1], ALU.subtract), reads=R + Wb, writes=Wb)
            Mbb = [B_Mb[gi] for gi in range(b * NT + t0, b * NT + t0 + G)]
            S.op("dve", lambda e: e.tensor_tensor(Mb[:, gis, :], mk1, mk2, ALU.add), reads=R, writes=Mbb)
            for il in range(G):
                gi = b * NT + t0 + il
                for jj in range(gi):
                    S.op("pe", lambda e, jj=jj, il=il: e.matmul(P[7][:, il * NE:(il + 1) * NE], lhsT=ones_b[:], rhs=Mb[:, jj, :], start=(jj == 0), stop=False),
                         reads=[B_Mb[jj], B_const], writes=[PB[7]])
                S.op("pe", lambda e, gi=gi, il=il: e.matmul(P[7][:, il * NE:(il + 1) * NE], lhsT=ustr_b[:], rhs=Mb[:, gi, :], start=(gi == 0), stop=True),
                     reads=[B_Mb[gi], B_const], writes=[PB[7]])
            pos = P[7][:, 0:G * NE].rearrange("p (g x) -> p g x", g=G)
            eb1024 = econst[:, 0, :].unsqueeze(1).to_broadcast([128, G, NE])
            ebcap = econst[:, 1, :].unsqueeze(1).to_broadcast([128, G, NE])
            S.op("dve", lambda e: e.tensor_scalar_min(dst, pos, 1023.0), reads=[PB[7]], writes=R)
            S.op("dve", lambda e: e.tensor_scalar(ovf, dst, float(CAP), 1.0e6, ALU.is_ge, ALU.mult), reads=R, writes=R)
            S.op("dve", lambda e: e.tensor_tensor(ovf, ovf, ebcap, ALU.add), reads=R + [B_const], writes=R)
            S.op("dve", lambda e: e.tensor_tensor(ovf, ovf, dst, ALU.add), reads=R, writes=R)
            Ib = [B_idx[gi] for gi in range(b * NT + t0, b * NT + t0 + G)]
            Vb = [B_vs[gi] for gi in range(b * NT + t0, b * NT + t0 + G)]
            S.op("dve", lambda e: e.tensor_tensor(prod[:, :, 0:32], ovf, mk1, ALU.mult), reads=R, writes=R)
            S.op("dve", lambda e: e.tensor_tensor(prod[:, :, 32:64], ovf, mk2, ALU.mult), reads=R, writes=R)
            S.op("dve", lambda e: e.reduce_sum(dsum.rearrange("p g a -> p (g a)"), prod.rearrange("p g (a x) -> p (g a) x", a=2), axis=AX.X), reads=R, writes=R)
            S.op("dve", lambda e: e.tensor_copy(idx_sb[:, gis, :], dsum), reads=R, writes=Ib)
            S.op("dve", lambda e: e.tensor_tensor(prod[:, :, 0:32], dst, mk1, ALU.mult), reads=R + Ib, writes=R)
            S.op("dve", lambda e: e.tensor_tensor(prod[:, :, 32:64], dst, mk2, ALU.mult), reads=R, writes=R)
            S.op("dve", lambda e: e.reduce_sum(pslot[:, gis, :].rearrange("p g a -> p (g a)"), prod.rearrange("p g (a x) -> p (g a) x", a=2), axis=AX.X), reads=R, writes=Vb)
            S.op("dve", lambda e: e.tensor_tensor(prod[:, :, 0:32], mk1, eb1024, ALU.mult), reads=R + Vb + [B_const], writes=R)
            S.op("dve", lambda e: e.tensor_tensor(prod[:, :, 32:64], mk2, eb1024, ALU.mult), reads=R + [B_const], writes=R)
            S.op("dve", lambda e: e.reduce_sum(eslot[:, gis, :].rearrange("p g a -> p (g a)"), prod.rearrange("p g (a x) -> p (g a) x", a=2), axis=AX.X), reads=R, writes=Vb)
            for nb_ in B_tbl:
                Sched.alias(nb_, B_mixT[0:8])
            for il in range(G):
                n = t0 + il
                gi = b * NT + n
                w = n % 4
                S.dma("sp", lambda e: e.dma_start(out=tbl[w], in_=tbd[gi * 128:(gi + 1) * 128, :]), reads=[B_tbd[n]], writes=[B_tbl[w]])
                for s_ in range(2):
                    S.dma("pool", lambda e, s_=s_: e.indirect_dma_start(
                        out=Xd, out_offset=bass.IndirectOffsetOnAxis(ap=idx_sb[:, gi, s_:s_ + 1], axis=0), in_=tbl[w], in_offset=None,
                        bounds_check="BCREG", oob_is_err=False), reads=[B_tbl[w], B_idx[gi]], writes=[B_Xd[gi * 2 + s_]])

        stageA1pe(0)
        stageA1pe(1)
        stageA1dve(0)
        for n in range(NT):
            if n >= 1:
                stageB(n - 1)
            if n + 2 < NT:
                stageA1pe(n + 2)
            stageA2a(n)
            if n + 1 < NT:
                stageA1dve(n + 1)
            stageA2b(n)
            if n >= 2:
                lgadd(n - 2)
            if n == RG + 1:
                route_chunk(0)
        stageB(NT - 1)
        lgadd(NT - 2)
        lgadd(NT - 1)
        route_chunk(1)
        for mb_ in B_mixT:
            Sched.alias(mb_, B_tbl)
        prev_1d = [B_wout, B_tT, B_lg, B_rtb] + B_std + B_xr + B_zt + B_x1t + B_tt + B_tb + B_tbl
        for nb_ in B_regA + B_qT + B_kT + B_V:
            Sched.alias(nb_, prev_1d)

    mixer_all = prev_1d + B_regA + B_qT + B_kT + B_V + B_mixT + [B_qtm2[1], B_ktm2[1], B_vln2[1], B_wi, B_rope, B_sgc, B_ln1, B_bc, B_hcT]
    B_rk = Buf("rank")
    Sched.alias(B_rk, mixer_all)
    rk = carve(O_WOV, 2240 * 4, F32)
    T1 = carve(O_WOV + 8960, 4096, F32, "p (a c) -> p a c", a=32)
    T2 = carve(O_WOV + 8960 + 4096, 4096, F32, "p (a c) -> p a c", a=32)
    RK = [B_rk]
    cnt, cntu, rank_, permf = rk[:, 0:32], rk[:, 32:64], rk[:, 64:96], rk[:, 96:104]
    sv = lambda i: rk[:, 128 + i * 64:128 + (i + 1) * 64]
    pos_s, e1k, qs_s, ovp, vm, vo, mainf, ovi, t_a, t_b = [sv(i) for i in range(10)]
    pflat = pslot[:].rearrange("p g a -> p (g a)")
    eflat = eslot[:].rearrange("p g a -> p (g a)")
    def emit_ranking():
        for j in range(NB * NT):
            S.op("pe", lambda e, j=j: e.matmul(P[7][:, 0:NE], lhsT=ones_b[:], rhs=Mb[:, j, :], start=(j == 0), stop=(j == NB * NT - 1)),
                 reads=[B_Mb[j], B_const], writes=[PB[7]])
        S.op("dve", lambda e: e.tensor_tensor(cntu, P[7][:, 0:NE], econst[:, 2, :], ALU.add), reads=[PB[7], B_const], writes=RK)
        S.op("dve", lambda e: e.tensor_tensor(T1, cntu.unsqueeze(1).to_broadcast([128, NE, NE]), cntu.unsqueeze(2).to_broadcast([128, NE, NE]), ALU.is_gt), reads=RK, writes=RK)
        S.op("dve", lambda e: e.reduce_sum(rank_, T1, axis=AX.X), reads=RK, writes=RK)
        T2v = T2[:, 0:R_OV, :]
        S.op("dve", lambda e: e.tensor_tensor(T2v, rank_.unsqueeze(1).to_broadcast([128, R_OV, NE]), econst[:, 4, 0:R_OV].unsqueeze(2).to_broadcast([128, R_OV, NE]), ALU.is_equal),
             reads=RK + [B_const], writes=RK)
        S.op("dve", lambda e: e.tensor_tensor(T2v, T2v, econst[:, 3, :].unsqueeze(1).to_broadcast([128, R_OV, NE]), ALU.mult), reads=RK + [B_const], writes=RK)
        S.op("dve", lambda e: e.reduce_sum(permf, T2v, axis=AX.X), reads=RK, writes=RK)
        S.op("dve", lambda e: e.tensor_copy(perm_i[:], permf), reads=RK, writes=[B_perm])
        S.op("dve", lambda e: e.tensor_copy(pos_s, pflat), reads=B_vs, writes=RK)
        S.op("dve", lambda e: e.tensor_copy(e1k, eflat), reads=B_vs, writes=RK)
        for hh in range(2):
            S.op("dve", lambda e, hh=hh: e.tensor_tensor(T1, e1k[:, hh * 32:(hh + 1) * 32].unsqueeze(2).to_broadcast([128, 32, NE]),
                                                         econst[:, 0, :].unsqueeze(1).to_broadcast([128, 32, NE]), ALU.is_equal), reads=RK + [B_const], writes=RK)
            S.op("dve", lambda e: e.tensor_tensor(T1, T1, rank_.unsqueeze(1).to_broadcast([128, 32, NE]), ALU.mult), reads=RK, writes=RK)
            S.op("dve", lambda e, hh=hh: e.reduce_sum(qs_s[:, hh * 32:(hh + 1) * 32], T1, axis=AX.X), reads=RK, writes=RK)
        S.op("dve", lambda e: e.tensor_single_scalar(vm, pos_s, float(CAP), ALU.is_lt), reads=RK, writes=RK)
        S.op("dve", lambda e: e.tensor_scalar_add(ovp, pos_s, -float(CAP)), reads=RK, writes=RK)
        S.op("dve", lambda e: e.tensor_single_scalar(vo, ovp, float(CAPO), ALU.is_lt), reads=RK, writes=RK)
        S.op("dve", lambda e: e.tensor_single_scalar(t_a, ovp, 0.0, ALU.is_ge), reads=RK, writes=RK)
        S.op("dve", lambda e: e.tensor_tensor(vo, vo, t_a, ALU.mult), reads=RK, writes=RK)
        S.op("dve", lambda e: e.tensor_single_scalar(t_a, qs_s, float(R_OV), ALU.is_lt), reads=RK, writes=RK)
        S.op("dve", lambda e: e.tensor_tensor(vo, vo, t_a, ALU.mult), reads=RK, writes=RK)
        S.op("dve", lambda e: e.scalar_tensor_tensor(mainf, e1k, float(CAP) / 1024.0, pos_s, ALU.mult, ALU.add), reads=RK, writes=RK)
        S.op("dve", lambda e: e.scalar_tensor_tensor(ovi, qs_s, float(CAPO), ovp, ALU.mult, ALU.add), reads=RK, writes=RK)
        S.op("dve", lambda e: e.tensor_scalar_add(ovi, ovi, float(NMAIN)), reads=RK, writes=RK)
        S.op("dve", lambda e: e.tensor_scalar_add(t_a, mainf, -float(ZROW)), reads=RK, writes=RK)
        S.op("dve", lambda e: e.tensor_tensor(t_a, t_a, vm, ALU.mult), reads=RK, writes=RK)
        S.op("dve", lambda e: e.tensor_scalar_add(t_b, ovi, -float(ZROW)), reads=RK, writes=RK)
        S.op("dve", lambda e: e.tensor_tensor(t_b, t_b, vo, ALU.mult), reads=RK, writes=RK)
        S.op("dve", lambda e: e.tensor_tensor(t_a, t_a, t_b, ALU.add), reads=RK, writes=RK)
        S.op("dve", lambda e: e.tensor_scalar_add(t_a, t_a, float(ZROW)), reads=RK, writes=RK)
        S.op("dve", lambda e: e.tensor_copy(idxg_sb[:].rearrange("p g a -> p (g a)"), t_a), reads=RK, writes=B_idx)
        S.op("dve", lambda e: e.tensor_scalar_add(t_b, ovi, -1.0e6), reads=RK, writes=RK)
        S.op("dve", lambda e: e.tensor_tensor(t_b, t_b, vo, ALU.mult), reads=RK, writes=RK)
        S.op("dve", lambda e: e.tensor_scalar_add(t_b, t_b, 1.0e6), reads=RK, writes=RK)
        S.op("dve", lambda e: e.tensor_copy(idxo_sb[:].rearrange("p g a -> p (g a)"), t_b), reads=RK, writes=[B_io])


    B_XT, B_hid = [Buf("XT0"), Buf("XT1")], Buf("hid")
    B_xg, B_sgt, B_ysb = [Buf("xg%d" % i) for i in range(4)], [Buf("sgt0"), Buf("sgt1")], [Buf("ysb%d" % i) for i in range(2)]
    B_tbo = [Buf("tbo0"), Buf("tbo1")]
    moe_bufs = B_wg + B_wu + B_wd + B_XT + [B_hid] + B_xg + B_sgt + B_ysb + B_tbo
    for nb_ in moe_bufs:
        if nb_ in (B_wg[0], B_wg[1], B_wu[0]):
            continue
        Sched.alias(nb_, mixer_all)

    def load_expert(ex):
        i = ex % 2
        if (ex, 0) not in early:
            S.dma("pool", lambda e: e.dma_start(out=wgS[i], in_=wall_d[ex, 0].rearrange("(k p) n -> p k n", p=128)), writes=[B_wg[i]])
        if (ex, 1) not in early:
            S.dma("pool", lambda e: e.dma_start(out=wuS[i], in_=wall_d[ex, 1].rearrange("(k p) n -> p k n", p=128)), writes=[B_wu[i]])
        S.dma("pool", lambda e: e.dma_start(out=wdS[i], in_=wall_d[ex, 2].rearrange("(k p) n -> p k n", p=128)), writes=[B_wd[i]])

    ctr = {"xg": 0, "y": 0, "g": 0, "xt": 0}

    def prep_X(row0, ntile, xi_t, xreads, xt, Bxt):
        for s_ in range(ntile):
            xi = ctr["xg"] % 4
            ctr["xg"] += 1
            S.dma("sp", lambda e, s_=s_, xi=xi: e.dma_start(out=xg[xi], in_=Xd[row0 + s_ * 128: row0 + (s_ + 1) * 128, :]), reads=xreads, writes=[B_xg[xi]])
            for hf in range(2):
                bank = next_bank(0, 2)
                for k4 in range(4):
                    k = hf * 4 + k4
                    S.op("pe", lambda e, k=k, k4=k4, xi=xi, bank=bank: e.transpose(P_bf[bank][:, k4 * 128:(k4 + 1) * 128], xg[xi][:, k * 128:(k + 1) * 128], ident_b[:]),
                         reads=[B_xg[xi], B_const], writes=[PB[bank]])
                S.op("dve", lambda e, hf=hf, s_=s_, bank=bank: e.tensor_copy(xt[:, hf * 4:hf * 4 + 4, s_ * 128:(s_ + 1) * 128], P_bf[bank][:, 0:512].rearrange("p (k n) -> p k n", k=4)),
                     reads=[PB[bank]], writes=[Bxt])

    def compute(wgv, wuv, wdv, Bw, xt, Bxt, ncap, row0, yd0):
        for f in range(8):
            pg = 2 + (ctr["g"] % 2)
            pu = 4 + (ctr["g"] % 2)
            sg_i = ctr["g"] % 2
            ctr["g"] += 1
            for k in range(8):
                S.op("pe", lambda e, k=k: e.matmul(P[pg][:, 0:ncap], lhsT=wgv[:, k, f * 128:(f + 1) * 128], rhs=xt[:, k, 0:ncap],
                                                   start=(k == 0), stop=(k == 7)), reads=Bw + [Bxt], writes=[PB[pg]])
            for k in range(8):
                S.op("pe", lambda e, k=k: e.matmul(P[pu][:, 0:ncap], lhsT=wuv[:, k, f * 128:(f + 1) * 128], rhs=xt[:, k, 0:ncap],
                                                   start=(k == 0), stop=(k == 7)), reads=Bw + [Bxt], writes=[PB[pu]])
            S.op("act", lambda e: e.activation(out=sgt[sg_i][:, 0:ncap], in_=P[pg][:, 0:ncap], func=AF.Silu), reads=[PB[pg]], writes=[B_sgt[sg_i]])
            S.op("dve", lambda e: e.tensor_tensor(hidT[:, f, 0:ncap], P[pu][:, 0:ncap], sgt[sg_i][:, 0:ncap], ALU.mult),
                 reads=[PB[pu], B_sgt[sg_i]], writes=[B_hid])
        for s_ in range(ncap // 128):
            yi = ctr["y"] % 2
            ctr["y"] += 1
            for dh in range(2):
                pd = 6 + dh
                for f in range(8):
                    S.op("pe", lambda e, f=f: e.matmul(P[pd][:, :], lhsT=hidT[:, f, s_ * 128:(s_ + 1) * 128], rhs=wdv[:, f, dh * 512:(dh + 1) * 512],
                                                       start=(f == 0), stop=(f == 7)), reads=[B_hid] + Bw, writes=[PB[pd]])
                if dh == 0:
                    S.op("act", lambda e: e.activation(out=ysb[yi][:, 0:512], in_=P[pd][:, :], func=AF.Copy), reads=[PB[pd]], writes=[B_ysb[yi]])
                else:
                    S.op("dve", lambda e: e.tensor_copy(ysb[yi][:, 512:1024], P[pd][:, :]), reads=[PB[pd]], writes=[B_ysb[yi]])
            S.dma("act", lambda e: e.dma_start(out=Yd[row0 + s_ * 128: row0 + (s_ + 1) * 128, :], in_=ysb[yi]), reads=[B_ysb[yi]], writes=[B_Yd[yd0 + s_]])

    ov_tiles = list(range(NB * NT))

    def overflow_scatter(k):
        for _ in range(k):
            if not ov_tiles:
                return
            gi = ov_tiles.pop(0)
            w = gi % 2
            S.dma("sp", lambda e: e.dma_start(out=tbo[w], in_=tbd[gi * 128:(gi + 1) * 128, :]), reads=[B_tbd_all[gi]], writes=[B_tbo[w]])
            for s_ in range(2):
                S.dma("pool", lambda e, s_=s_: e.indirect_dma_start(
                    out=Xd, out_offset=bass.IndirectOffsetOnAxis(ap=idxo_sb[:, gi, s_:s_ + 1], axis=0), in_=tbo[w], in_offset=None,
                    bounds_check="BCREG", oob_is_err=False), reads=[B_tbo[w], B_io], writes=[B_Xo[gi * 2 + s_]])

    NS = CAP // 128
    NSO = CAPO // 128
    B_wov = [Buf("wov0")]
    B_XTo = Buf("XTo")
    for nb_ in B_wov + [B_XTo]:
        Sched.alias(nb_, mixer_all + [B_rk])

    def load_overflow(r):
        dst = wov[0]

        def bld(eng):
            reg = eng.alloc_register("pr%d" % r)
            eng.reg_load(reg, perm_i[0:1, r:r + 1])
            v = eng.snap(reg, donate=True, min_val=0, max_val=NE - 1)
            return eng.dma_start(out=dst, in_=wall_d[bass.ds(v, 1)].rearrange("o w (k p) n -> p (o w k) n", p=128))
        S.dma("pool", Late(bld), reads=[B_perm], writes=[B_wov[0]])

    ov_slot = {17 + 2 * r: r for r in range(R_OV)}
    load_expert(0)
    prep_X(0, NS, 0, B_Xd, XTm[0], B_XT[0])
    for ex in range(NE):
        i = ex % 2
        if ex + 1 < NE:
            load_expert(ex + 1)
            prep_X((ex + 1) * CAP, NS, (ex + 1) % 2, B_Xd, XTm[(ex + 1) % 2], B_XT[(ex + 1) % 2])
        if ex + 1 in ov_slot:
            load_overflow(ov_slot[ex + 1])
        if ex >= 1:
            overflow_scatter(2)
        compute(wgS[i], wuS[i], wdS[i], [B_wg[i], B_wu[i], B_wd[i]], XTm[i], B_XT[i], CAP, ex * CAP, ex * NS)
        if ex == 0:
            emit_ranking()
        if ex in ov_slot:
            r = ov_slot[ex]
            prep_X(NMAIN + r * CAPO, NSO, 0, B_Xo, XTo, B_XTo)
            wv = wov[0]
            compute(wv[:, 0:8, :], wv[:, 8:16, :], wv[:, 16:24, :], [B_wov[0]], XTo, B_XTo, CAPO, NMAIN + r * CAPO, NE * NS + r * NSO)
    assert not ov_tiles
    moe_bufs = moe_bufs + B_wov + [B_XTo]

    B_fin = Buf("fin")
    B_fs = [Buf("fst0"), Buf("fst1")]
    NBUF = 4
    B_Y1, B_Y2, B_x1r = [Buf("Y1%d" % i) for i in range(NBUF)], [Buf("Y2%d" % i) for i in range(NBUF)], [Buf("x1r%d" % i) for i in range(NBUF)]
    for nb_ in [B_fin] + B_fs + B_Y1 + B_Y2 + B_x1r:
        Sched.alias(nb_, moe_bufs)
    for b in range(NB):
        S.dma("sp", lambda e, b=b: e.dma_start(out=g2bt[b], in_=modd[b:b + 1, 5120:6144].partition_broadcast(128)), reads=[b_modd], writes=[B_fin])
    S.dma("sp", lambda e: e.dma_start(out=ln2g, in_=ln2g_d.partition_broadcast(128)), writes=[B_fin])
    S.dma("sp", lambda e: e.dma_start(out=ln2b, in_=ln2b_d.partition_broadcast(128)), writes=[B_fin])
    B_zrow = Buf("zrow")
    S.op("dve", lambda e: e.memset(x1r[0][0:1, :], 0.0), writes=[B_x1r[0]])
    S.dma("sp", lambda e: e.dma_start(out=Yd[ZROW:ZROW + 1, :], in_=x1r[0][0:1, :]), reads=[B_x1r[0]], writes=[B_zrow])
    YdAll = B_Yd + [B_zrow]

    def fetch(gi):
        w = gi % NBUF
        S.dma("pool", lambda e: e.indirect_dma_start(out=Y1[w], out_offset=None, in_=Yd, in_offset=bass.IndirectOffsetOnAxis(ap=idxg_sb[:, gi, 0:1], axis=0),
                                                   bounds_check="BCREG2", oob_is_err=False), reads=YdAll + [B_idx[gi]], writes=[B_Y1[w]])
        S.dma("pool", lambda e: e.indirect_dma_start(out=Y2[w], out_offset=None, in_=Yd, in_offset=bass.IndirectOffsetOnAxis(ap=idxg_sb[:, gi, 1:2], axis=0),
                                                   bounds_check="BCREG2", oob_is_err=False), reads=YdAll + [B_idx[gi]], writes=[B_Y2[w]])
        S.dma("sp", lambda e: e.dma_start(out=x1r[w], in_=x1d[gi * 128:(gi + 1) * 128, :]), reads=[B_x1d[gi]], writes=[B_x1r[w]])

    def comb0(gi):
        w = gi % NBUF
        S.op("act", lambda e: e.activation(out=Y1[w], in_=Y1[w], func=AF.Identity, scale=wts_sb[:, gi, 0:1]), reads=[B_Y1[w], B_wts[gi]], writes=[B_Y1[w]])

    def comb1(gi):
        b = gi // NT
        w = gi % NBUF
        f_ = fst[gi % 2]
        Bf = B_fs[gi % 2]
        S.op("dve", lambda e: e.scalar_tensor_tensor(Y1[w], Y2[w], wts_sb[:, gi, 1:2], Y1[w], ALU.mult, ALU.add), reads=[B_Y1[w], B_Y2[w], B_wts[gi]], writes=[B_Y1[w]])
        S.op("dve", lambda e: e.tensor_tensor(Y1[w], Y1[w], g2bt[b], ALU.mult), reads=[B_Y1[w], B_fin], writes=[B_Y1[w]])
        S.op("dve", lambda e: e.scalar_tensor_tensor(Y1[w], x1r[w], ALPHA, Y1[w], ALU.mult, ALU.add), reads=[B_Y1[w], B_x1r[w]], writes=[B_Y1[w]])
        for hf in range(2):
            S.op("dve", lambda e, hf=hf: e.bn_stats(f_[:, hf * 6:hf * 6 + 6], Y1[w][:, hf * 512:(hf + 1) * 512]), reads=[B_Y1[w]], writes=[Bf])
        S.op("dve", lambda e: e.bn_aggr(f_[:, 12:14], f_[:, 0:12].rearrange("p (a s) -> p a s", a=2)), reads=[Bf], writes=[Bf])
        S.op("act", lambda e: e.activation(out=f_[:, 14:15], in_=f_[:, 13:14], func=AF.Ln, bias=EPS), reads=[Bf], writes=[Bf])
        S.op("act", lambda e: e.activation(out=f_[:, 15:16], in_=f_[:, 14:15], func=AF.Exp, scale=-0.5), reads=[Bf], writes=[Bf])

    def comb2a(gi):
        w = gi % NBUF
        f_ = fst[gi % 2]
        Bf = B_fs[gi % 2]
        S.op("dve", lambda e: e.tensor_scalar(f_[:, 16:17], f_[:, 12:13], f_[:, 15:16], -1.0, ALU.mult, ALU.mult), reads=[Bf], writes=[Bf])
        S.op("act", lambda e: e.activation(out=Y1[w], in_=Y1[w], func=AF.Identity, scale=f_[:, 15:16], bias=f_[:, 16:17]), reads=[B_Y1[w], Bf], writes=[B_Y1[w]])

    def comb2b(gi):
        b = gi // NT
        n = gi % NT
        w = gi % NBUF
        S.op("dve", lambda e: e.tensor_tensor(Y1[w], Y1[w], ln2g, ALU.mult), reads=[B_Y1[w], B_fin], writes=[B_Y1[w]])
        S.op("dve", lambda e: e.tensor_tensor(Y2[w], Y1[w], ln2b, ALU.add), reads=[B_Y1[w], B_fin], writes=[B_Y2[w]])
        S.dma("sp", lambda e: e.dma_start(out=out_d[b, n * 128:(n + 1) * 128, :], in_=Y2[w]), reads=[B_Y2[w]], writes=[B_out[gi]])

    NTOT = NB * NT
    for gi in range(min(3, NTOT)):
        fetch(gi)
    comb0(0)
    comb0(1)
    comb1(0)
    for gi in range(NTOT):
        if gi + 3 < NTOT:
            fetch(gi + 3)
        comb2a(gi)
        if gi + 2 < NTOT:
            comb0(gi + 2)
        if gi + 1 < NTOT:
            comb1(gi + 1)
        comb2b(gi)
    S.wait_all("sp", B_out + (B_dbg if debug else []))
    S.emit(nc, st)
    st.close()
    return nc


def _rope_tables():
    half = 16
    inv = (10000.0 ** (-np.arange(half, dtype=np.float32) / half)).astype(np.float32)
    tok = np.arange(L)
    rows = (tok // 64).astype(np.float32)
    cols = (tok % 64).astype(np.float32)
    cos = np.zeros((L, 64), np.float32)
    sin = np.zeros((L, 64), np.float32)
    for base, pos in ((0, rows), (32, cols)):
        ang = (pos[:, None] * inv[None, :]).astype(np.float32)
        c, s = np.cos(ang).astype(np.float32), np.sin(ang).astype(np.float32)
        cos[:, base:base + 16] = c
        cos[:, base + 16:base + 32] = c
        sin[:, base:base + 16] = -s
        sin[:, base + 16:base + 32] = s
    return cos, sin


def make_in_maps(inputs, ncores=NCORES):
    f = lambda a: np.ascontiguousarray(np.asarray(a, dtype=np.float32))
    cos, sin = _rope_tables()
    shared = {
        "w_mod": f(inputs["w_mod"][0]), "b_mod": f(inputs["b_mod"][0]).reshape(1, -1),
        "w_in": f(inputs["w_in"][0]), "w_out": f(inputs["w_out"][0]),
        "lamv": np.concatenate([f(inputs[k][0]) for k in ("lam_q1", "lam_k1", "lam_q2", "lam_k2")]).reshape(1, 256),
        "subln_g": f(inputs["subln_g"][0]).reshape(1, -1),
        "sg_ln_g": f(inputs["sg_ln_g"][0]).reshape(1, -1), "sg_ln_b": f(inputs["sg_ln_b"][0]).reshape(1, -1),
        "sg_wT": np.ascontiguousarray(f(inputs["sg_w"][0]).transpose(0, 2, 1)),
        "sg_b": f(inputs["sg_b"][0]).reshape(1, -1),
        "ln1_g": f(inputs["ln1_g"][0]).reshape(1, -1), "ln1_b": f(inputs["ln1_b"][0]).reshape(1, -1),
        "ln2_g": f(inputs["ln2_g"][0]).reshape(1, -1), "ln2_b": f(inputs["ln2_b"][0]).reshape(1, -1),
        "router_w": np.ascontiguousarray(np.concatenate([f(inputs["router_group_w"][0]), f(inputs["router_expert_w"][0])], axis=1)),
        "router_b": np.concatenate([f(inputs["router_group_b"][0]), f(inputs["router_expert_b"][0])]).reshape(1, 36),
        "exp_w": np.ascontiguousarray(np.stack([f(inputs["exp_w_gate"][0]), f(inputs["exp_w_up"][0]), f(inputs["exp_w_down"][0])], axis=1)),
        "ident": np.eye(128, dtype=np.float32),
        "ustrict": np.triu(np.ones((128, 128), np.float32), 1),
        "rope_cos": cos, "rope_sin": sin,
        "econst": np.concatenate([np.arange(NE, dtype=np.float32) * 1024.0, np.arange(NE, dtype=np.float32) * CAP,
                                  np.arange(NE, dtype=np.float32) / 64.0, np.arange(NE, dtype=np.float32),
                                  np.arange(NE, dtype=np.float32)]).reshape(1, 5 * NE),
    }
    x = f(inputs["x"]); c = f(inputs["c"]); ctx = f(inputs["ctx"]); cc = f(inputs["c_ctx"])
    maps = []
    for i in range(ncores):
        sl = slice(i * NB, (i + 1) * NB)
        cv = np.stack([c[i * NB], c[i * NB + 1], cc], axis=1)
        cT = np.ascontiguousarray(cv.reshape(8, 128, 3).transpose(1, 0, 2))
        m = dict(shared)
        m.update({"x": np.ascontiguousarray(x[sl]), "ctx": np.ascontiguousarray(ctx[sl]), "cT": cT})
        maps.append(m)
    return maps


_NC_CACHE = {}


def kernel(**inputs):
    if "nc" not in _NC_CACHE:
        _NC_CACHE["nc"] = build_nc()
    nc = _NC_CACHE["nc"]
    in_maps = make_in_maps(inputs)
    res = run_bass_kernel_spmd(nc, in_maps, core_ids=list(range(NCORES)))
    out = np.concatenate([np.asarray(r["out"]) for r in res.results], axis=0)
    return out.astype(np.float32)
```
